# Optimizing a Trainium2 kernel written in Bass

```python
import jax
import jax.numpy as jnp
from jax import lax
import numpy as np


D_MODEL = 4096
BATCH = 4
SEQ = 2048
DEPTH = 2

HEAD_DIM = 128
N_BRANCH = 4
BRANCH_WIDTH = D_MODEL // N_BRANCH
SGU_CHUNK = 128
SGU_GROUP_WIDTH = 128
SGU_GROUPS = BRANCH_WIDTH // SGU_GROUP_WIDTH
POOL_WINDOWS = (2, 4, 8, 16)
POOL_GROUP_WIDTH = BRANCH_WIDTH // len(POOL_WINDOWS)
DIL_PAIRS = ((128, 1), (512, 4), (2048, 16))
DIL_HEADS = BRANCH_WIDTH // HEAD_DIM
DIL_BLOCK = 128
MOBA_HEADS = BRANCH_WIDTH // HEAD_DIM
MOBA_BLOCK = 256
MOBA_TOPK = 3
MOBA_QCHUNK = 32
ALIBI_SETS = len(DIL_PAIRS) + 1
N_ALIBI_HEADS = len(DIL_PAIRS) * DIL_HEADS + MOBA_HEADS
D_FF = 256 * (-(-8 * D_MODEL // (3 * 256)))
CONV_WIDTH = 3
N_MOD = 6
NORM_EPS = 1e-6
NEG_INF = -1e30
COLS_A = 2 * BRANCH_WIDTH
COLS_B = BRANCH_WIDTH
COLS_C = 3 * len(DIL_PAIRS) * DIL_HEADS * HEAD_DIM
COLS_D = 3 * MOBA_HEADS * HEAD_DIM
IN_COLS = COLS_A + COLS_B + COLS_C + COLS_D

kernel_name = 'hybrid_gated_multimixer_decoder'


def rmsnorm(x, g):
    xf = x.astype(jnp.float32)
    y = xf * lax.rsqrt(jnp.mean(xf * xf, axis=-1, keepdims=True) + NORM_EPS)
    return (y * g.astype(jnp.float32)).astype(x.dtype)


def alibi_slopes():
    n = N_ALIBI_HEADS
    return jnp.asarray(2.0 ** (-8.0 * np.arange(1, n + 1) / n), dtype=jnp.float32)


def softmax_stats(s, axes):
    m = jnp.max(s, axis=axes, keepdims=True)
    p = jnp.exp(s - m)
    den = jnp.sum(p, axis=axes, keepdims=True)
    return p, den, m


def spatial_gating(z, norm_g, w_s, b_s):
    bsz, seq, _ = z.shape
    u, v = jnp.split(jax.nn.gelu(z), 2, axis=-1)
    v = rmsnorm(v, norm_g)
    n_chunk = seq // SGU_CHUNK
    v = v.reshape(bsz, n_chunk, SGU_CHUNK, SGU_GROUPS, SGU_GROUP_WIDTH)
    causal = jnp.tril(jnp.ones((SGU_CHUNK, SGU_CHUNK), dtype=bool))
    w_c = jnp.where(causal[None], w_s, 0).astype(v.dtype)
    mixed = jnp.einsum('gts,bnsgc->bntgc', w_c, v) + b_s.T[:, :, None].astype(v.dtype)
    return u * mixed.reshape(bsz, seq, BRANCH_WIDTH)


def multiscale_pool(z, w_pool, scale):
    bsz, seq, _ = z.shape
    n_g = len(POOL_WINDOWS)
    zg = z.reshape(bsz, seq, n_g, POOL_GROUP_WIDTH).astype(jnp.float32)
    csum = jnp.cumsum(zg, axis=1)
    prev = jnp.stack([jnp.pad(csum[:, :, g], ((0, 0), (w, 0), (0, 0)))[:, :seq]
                      for g, w in enumerate(POOL_WINDOWS)], axis=2)
    t = jnp.arange(seq)[:, None]
    count = jnp.minimum(t + 1, jnp.asarray(POOL_WINDOWS)[None, :]).astype(jnp.float32)
    mixed = ((csum - prev) / count[None, :, :, None] - zg).astype(z.dtype)
    y = jnp.einsum('bsgc,gce->bsge', mixed, w_pool)
    return y.reshape(bsz, seq, BRANCH_WIDTH) * scale


def dilated_window_attention(q, k, v, window, dilation, slopes):
    bsz, seq, nh, hd = q.shape
    span = window // dilation
    sub = seq // dilation
    n_blk = -(-sub // DIL_BLOCK)
    pad = n_blk * DIL_BLOCK - sub

    def to_blocks(a):
        a = a.reshape(bsz, sub, dilation, nh, hd).transpose(0, 2, 1, 3, 4)
        a = jnp.pad(a, ((0, 0), (0, 0), (0, pad), (0, 0), (0, 0)))
        return a.reshape(bsz, dilation, n_blk, DIL_BLOCK, nh, hd)

    def band(a):
        prev = jnp.pad(a, ((0, 0), (0, 0), (1, 0), (0, 0), (0, 0), (0, 0)))[:, :, :-1]
        return jnp.concatenate([prev, a], axis=3)

    qb = to_blocks(q)
    kband = band(to_blocks(k))
    vband = band(to_blocks(v)).astype(jnp.float32)
    s = jnp.einsum('brnqhd,brnkhd->brhnqk', qb, kband).astype(jnp.float32) * (hd ** -0.5)
    qi = jnp.arange(DIL_BLOCK)[:, None]
    ki = jnp.arange(2 * DIL_BLOCK)[None, :]
    steps = qi - ki + DIL_BLOCK
    key_sub = jnp.arange(n_blk)[:, None, None] * DIL_BLOCK + ki[None] - DIL_BLOCK
    valid = ((steps >= 0) & (steps <= span))[None] & (key_sub >= 0)
    bias = -slopes[:, None, None] * (dilation * steps).astype(jnp.float32)
    s = jnp.where(valid[None, None, None], s + bias[None, None, :, None], NEG_INF)
    p, den, m = softmax_stats(s, -1)
    o = jnp.einsum('brhnqk,brnkhd->brhnqd', p, vband) / den
    lse = (m + jnp.log(den))[..., 0]
    o = o.reshape(bsz, dilation, nh, n_blk * DIL_BLOCK, hd)[:, :, :, :sub]
    o = o.transpose(0, 3, 1, 2, 4).reshape(bsz, seq, nh, hd)
    lse = lse.reshape(bsz, dilation, nh, n_blk * DIL_BLOCK)[..., :sub]
    lse = lse.transpose(0, 3, 1, 2).reshape(bsz, seq, nh)
    return o, lse


def dilated_mixer(zc, slopes):
    bsz, seq, _ = zc.shape
    qkv = zc.reshape(bsz, seq, 3, len(DIL_PAIRS), DIL_HEADS, HEAD_DIM)
    outs, lses = [], []
    for g, (window, dilation) in enumerate(DIL_PAIRS):
        o, l = dilated_window_attention(qkv[:, :, 0, g], qkv[:, :, 1, g], qkv[:, :, 2, g],
                                        window, dilation, slopes[g::ALIBI_SETS])
        outs.append(o)
        lses.append(l)
    wts = jax.nn.softmax(jnp.stack(lses), axis=0)
    o = jnp.sum(wts[..., None] * jnp.stack(outs), axis=0)
    return o.reshape(bsz, seq, BRANCH_WIDTH).astype(zc.dtype)


def moba_mixer(zd, slopes):
    bsz, seq, _ = zd.shape
    nh, hd, blk, qc = MOBA_HEADS, HEAD_DIM, MOBA_BLOCK, MOBA_QCHUNK
    n_blk = -(-seq // blk)
    seq_p = n_blk * blk
    qkv = jnp.pad(zd, ((0, 0), (0, seq_p - seq), (0, 0))).reshape(bsz, seq_p, 3, nh, hd)
    q, k, v = qkv[:, :, 0], qkv[:, :, 1], qkv[:, :, 2]
    scale = hd ** -0.5
    qb = q.reshape(bsz, n_blk, blk, nh, hd)
    kb = k.reshape(bsz, n_blk, blk, nh, hd)
    vb = v.reshape(bsz, n_blk, blk, nh, hd)

    ii = jnp.arange(blk)
    dist = ii[:, None] - ii[None, :]
    s_own = jnp.einsum('bnqhd,bnkhd->bhnqk', qb, kb).astype(jnp.float32) * scale
    s_own = jnp.where(dist >= 0, s_own - slopes[:, None, None, None] * dist.astype(jnp.float32), NEG_INF)
    p, den, m = softmax_stats(s_own, -1)
    o_own = jnp.einsum('bhnqk,bnkhd->bhnqd', p, vb.astype(jnp.float32)) / den
    o_own = o_own.reshape(bsz, nh, seq_p, hd)
    lse_own = (m + jnp.log(den))[..., 0].reshape(bsz, nh, seq_p)

    k_mean = jnp.mean(kb.astype(jnp.float32), axis=2)
    gate = jnp.einsum('bshd,bnhd->bhsn', q.astype(jnp.float32), k_mean)
    t = jnp.arange(seq_p)
    q_blk = t // blk
    fully_past = jnp.arange(n_blk)[None, :] < q_blk[:, None]
    gate = jnp.where(fully_past, gate, NEG_INF)
    top = min(MOBA_TOPK, n_blk)
    _, sel = lax.top_k(gate, top)
    sel_valid = jnp.arange(top)[None, :] < q_blk[:, None]

    k_t = kb.transpose(0, 3, 1, 2, 4)
    v_t = vb.transpose(0, 3, 1, 2, 4)
    n_qc = seq_p // qc
    q_c = q.transpose(0, 2, 1, 3).reshape(bsz, nh, n_qc, qc, hd).transpose(2, 0, 1, 3, 4)
    sel_c = sel.reshape(bsz, nh, n_qc, qc, top).transpose(2, 0, 1, 3, 4)
    valid_c = sel_valid.reshape(n_qc, qc, top)
    t_c = t.reshape(n_qc, qc)
    gather = jax.vmap(jax.vmap(lambda a, i: a[i]))

    def selected_blocks(args):
        qq, ss, vv, tt = args
        flat = ss.reshape(bsz, nh, qc * top)
        kg = gather(k_t, flat).reshape(bsz, nh, qc, top, blk, hd)
        vg = gather(v_t, flat).reshape(bsz, nh, qc, top, blk, hd).astype(jnp.float32)
        s = jnp.einsum('bhqd,bhqrkd->bhqrk', qq, kg).astype(jnp.float32) * scale
        key_pos = ss[..., None] * blk + jnp.arange(blk)
        d = (tt[None, None, :, None, None] - key_pos).astype(jnp.float32)
        s = jnp.where(vv[None, None, :, :, None], s - slopes[None, :, None, None, None] * d, NEG_INF)
        p, den, m = softmax_stats(s, (-2, -1))
        o = jnp.einsum('bhqrk,bhqrkd->bhqd', p, vg) / den[..., 0]
        return o, (m + jnp.log(den))[..., 0, 0]

    o_sel, lse_sel = lax.map(selected_blocks, (q_c, sel_c, valid_c, t_c))
    o_sel = o_sel.transpose(1, 2, 0, 3, 4).reshape(bsz, nh, seq_p, hd)
    lse_sel = lse_sel.transpose(1, 2, 0, 3).reshape(bsz, nh, seq_p)

    m = jnp.maximum(lse_own, lse_sel)
    w_own = jnp.exp(lse_own - m)
    w_sel = jnp.exp(lse_sel - m)
    o = (w_own[..., None] * o_own + w_sel[..., None] * o_sel) / (w_own + w_sel)[..., None]
    o = o.transpose(0, 2, 1, 3)[:, :seq].reshape(bsz, seq, BRANCH_WIDTH)
    return o.astype(zd.dtype)


def hybrid_layer(x, c, norm1_g, w_ada, b_ada, w_in, sgu_norm_g, sgu_w, sgu_b, pool_w, pool_scale,
                 merge_w, merge_b, branch_w, out_w, norm2_g, ffn_wg, ffn_wu, conv_w, conv_b, ffn_wd,
                 slopes):
    bsz = x.shape[0]
    mod = (c @ w_ada + b_ada).reshape(bsz, N_MOD, 1, D_MODEL)
    shift1, scale1, gate1 = mod[:, 0], mod[:, 1], mod[:, 2]
    shift2, scale2, gate2 = mod[:, 3], mod[:, 4], mod[:, 5]

    h = rmsnorm(x, norm1_g) * (1 + scale1) + shift1
    z = h @ w_in
    z_a, z_b, z_c, z_d = jnp.split(z, [COLS_A, COLS_A + COLS_B, COLS_A + COLS_B + COLS_C], axis=-1)
    y_a = spatial_gating(z_a, sgu_norm_g, sgu_w, sgu_b)
    y_b = multiscale_pool(z_b, pool_w, pool_scale)
    y_c = dilated_mixer(z_c, slopes)
    y_d = moba_mixer(z_d, slopes[len(DIL_PAIRS)::ALIBI_SETS])
    branches = jnp.stack([y_a, y_b, y_c, y_d], axis=2)
    gates = jax.nn.sigmoid(jnp.einsum('bsd,dne->bsne', h, merge_w) + merge_b)
    projected = jnp.einsum('bsnw,nwe->bsne', branches, branch_w)
    merged = jnp.sum(gates * projected, axis=2)
    x = x + gate1 * (merged @ out_w)

    h2 = rmsnorm(x, norm2_g) * (1 + scale2) + shift2
    a = lax.conv_general_dilated(h2 @ ffn_wg, conv_w[:, None, :], window_strides=(1,),
                                 padding=[(CONV_WIDTH - 1, 0)],
                                 dimension_numbers=('NWC', 'WIO', 'NWC'),
                                 feature_group_count=D_FF) + conv_b
    f = jax.nn.gelu(a) * (h2 @ ffn_wu)
    return x + gate2 * (f @ ffn_wd)


def setup_inputs(seed: int = 0) -> dict:
    key = jax.random.key(seed)
    ks = jax.random.split(key, 22)
    L, D, W, P = DEPTH, D_MODEL, BRANCH_WIDTH, POOL_GROUP_WIDTH

    def nrm(i, shape, std):
        return jax.random.normal(ks[i], shape, jnp.float32) * std

    return {
        'x': nrm(0, (BATCH, SEQ, D), 1.0),
        'c': nrm(1, (BATCH, D), 1.0),
        'norm1_g': 1.0 + nrm(2, (L, D), 0.05),
        'w_ada': nrm(3, (L, D, N_MOD * D), 0.5 * D ** -0.5),
        'b_ada': nrm(4, (L, N_MOD * D), 0.02),
        'w_in': nrm(5, (L, D, IN_COLS), D ** -0.5),
        'sgu_norm_g': 1.0 + nrm(6, (L, W), 0.05),
        'sgu_w': nrm(7, (L, SGU_GROUPS, SGU_CHUNK, SGU_CHUNK), 0.5 * SGU_CHUNK ** -0.5),
        'sgu_b': 1.0 + nrm(8, (L, SGU_GROUPS, SGU_CHUNK), 0.1),
        'pool_w': nrm(9, (L, len(POOL_WINDOWS), P, P), P ** -0.5),
        'pool_scale': 1.0 + nrm(10, (L, W), 0.1),
        'merge_w': nrm(11, (L, D, N_BRANCH, D), D ** -0.5),
        'merge_b': nrm(12, (L, N_BRANCH, D), 0.02),
        'branch_w': nrm(13, (L, N_BRANCH, W, D), W ** -0.5),
        'out_w': nrm(14, (L, D, D), D ** -0.5),
        'norm2_g': 1.0 + nrm(15, (L, D), 0.05),
        'ffn_wg': nrm(16, (L, D, D_FF), D ** -0.5),
        'ffn_wu': nrm(17, (L, D, D_FF), D ** -0.5),
        'conv_w': nrm(18, (L, CONV_WIDTH, D_FF), CONV_WIDTH ** -0.5),
        'conv_b': nrm(19, (L, D_FF), 0.02),
        'ffn_wd': nrm(20, (L, D_FF, D), D_FF ** -0.5),
        'final_g': 1.0 + nrm(21, (D,), 0.05),
    }


def reference(x, c, norm1_g, w_ada, b_ada, w_in, sgu_norm_g, sgu_w, sgu_b, pool_w, pool_scale,
              merge_w, merge_b, branch_w, out_w, norm2_g, ffn_wg, ffn_wu, conv_w, conv_b, ffn_wd,
              final_g):
    slopes = alibi_slopes()
    for i in range(DEPTH):
        x = hybrid_layer(x, c, norm1_g[i], w_ada[i], b_ada[i], w_in[i], sgu_norm_g[i], sgu_w[i],
                         sgu_b[i], pool_w[i], pool_scale[i], merge_w[i], merge_b[i], branch_w[i],
                         out_w[i], norm2_g[i], ffn_wg[i], ffn_wu[i], conv_w[i], conv_b[i], ffn_wd[i],
                         slopes)
    return rmsnorm(x, final_g)
```

```python
import numpy as np
import ml_dtypes
import concourse.bass as bass
import concourse.mybir as mybir
from concourse.bass_utils import run_bass_kernel_spmd

F32 = mybir.dt.float32
BF16 = mybir.dt.bfloat16
ALU = mybir.AluOpType
AF = mybir.ActivationFunctionType
AX = mybir.AxisListType

D = 4096
S = 2048
S_LEN = 2048
NB = 4
TPC = 1024
DFF = 11008
NFC = DFF // 128
EPS = 1e-6
IN_COLS = 15360
BIG = 30000.0


class Sched:
    def __init__(self, nc, n_dma_sems=20, strict=True):
        self.nc = nc
        self.eng = {"pe": nc.tensor, "act": nc.scalar, "dve": nc.vector,
                    "pool": nc.gpsimd, "sp": nc.sync}
        self.csem = {e: nc.alloc_semaphore(name=f"c_{e}") for e in ("pe", "act", "dve", "pool")}
        self.ccnt = {e: 0 for e in self.csem}
        self.dsem = {}
        self.dcnt = {}
        self.dnext = {}
        for q in ("sp", "pool", "act"):
            self.dsem[q] = [nc.alloc_semaphore(name=f"d_{q}{i}") for i in range(n_dma_sems)]
            self.dcnt[q] = [0] * n_dma_sems
            self.dnext[q] = 0
        self.seen = {e: {} for e in self.eng}
        self.lastw = {}
        self.reads = {}
        self.strict = strict
        self._ps = None
        self._psn = 0

    def _sem(self, k):
        if k[0] == "c":
            return self.csem[k[1]]
        return self.dsem[k[1]][k[2]]

    def _wait(self, e, k, v):
        if self.seen[e].get(k, 0) >= v:
            return
        self.eng[e].wait_ge(self._sem(k), v)
        self.seen[e][k] = v

    def _deps(self, e, reads, writes):
        deps = {}

        def add(t):
            if t is None:
                return
            k, v = t
            if deps.get(k, 0) < v:
                deps[k] = v
        for k in reads:
            add(self.lastw.get(k))
        for k in writes:
            add(self.lastw.get(k))
            for kk, vv in self.reads.get(k, {}).items():
                add((kk, vv))
        for k, v in deps.items():
            if k[0] == "c" and k[1] == e and (e == "pe" or not self.strict):
                continue
            self._wait(e, k, v)

    def _commit(self, ticket, reads, writes):
        for k in writes:
            self.lastw[k] = ticket
            self.reads[k] = {}
        for k in reads:
            r = self.reads.setdefault(k, {})
            if r.get(ticket[0], 0) < ticket[1]:
                r[ticket[0]] = ticket[1]

    def op(self, e, reads, writes, fn):
        self._deps(e, reads, writes)
        ins = fn(self.eng[e])
        self.ccnt[e] += 1
        ins.then_inc(self.csem[e], 1)
        t = (("c", e), self.ccnt[e])
        self._commit(t, reads, writes)
        return t

    def dma(self, q, reads, writes, fn):
        i = self.dnext[q]
        self.dnext[q] = (i + 1) % len(self.dsem[q])
        k = ("d", q, i)
        if self.dcnt[q][i] > 0:
            self._wait(q, k, self.dcnt[q][i])
        self._deps(q, reads, writes)
        insl = fn(self.eng[q])
        if not isinstance(insl, (list, tuple)):
            insl = [insl]
        for ins in insl:
            ins.then_inc(self.dsem[q][i], 16)
            self.dcnt[q][i] += 16
        t = (k, self.dcnt[q][i])
        self._commit(t, reads, writes)
        return t

    def finish(self, e="sp"):
        for q in self.dsem:
            for i, c in enumerate(self.dcnt[q]):
                if c > 0:
                    self._wait(e, ("d", q, i), c)

    def psum_init(self):
        self._ps = [self.nc.alloc_psum_tensor(f"psb{i}", [128, 512], F32) for i in range(8)]

    def psum(self):
        i = self._psn % 8
        self._psn += 1
        return self._ps[i], ("ps", i)


def _mm_group(S, ps_ap, pskey, pairs, reads, extra_writes=()):
    n = len(pairs)

    def fn(pe):
        ins = None
        for i, (l, r) in enumerate(pairs):
            ins = pe.matmul(ps_ap, l, r, start=(i == 0), stop=(i == n - 1))
        return ins
    return S.op("pe", reads, [pskey] + list(extra_writes), fn)


def _rstd(S, ss_ap, rstd_ap, inv_n, sskey, rkey):
    S.op("dve", [sskey], [rkey],
         lambda v: v.tensor_scalar(out=rstd_ap, in0=ss_ap, scalar1=inv_n, scalar2=EPS,
                                   op0=ALU.mult, op1=ALU.add))
    S.op("act", [rkey], [rkey], lambda a: a.activation(out=rstd_ap, in_=rstd_ap, func=AF.Sqrt))
    S.op("dve", [rkey], [rkey], lambda v: v.reciprocal(out=rstd_ap, in_=rstd_ap))


def _norm_to_hT(S, nc, x_ap, n_tok, xbufs, junk, ss, rstd, ident, A, B, hT, hkey, col0):
    ntile = (n_tok + 127) // 128
    for tt in range(ntile):
        p = min(128, n_tok - tt * 128)
        xt = xbufs[tt % 2]
        xk = ("xbuf", id(xt))
        S.dma("sp", [], [xk], lambda q: q.dma_start(out=xt[:p, :], in_=x_ap[tt * 128: tt * 128 + p, :]))
        S.op("act", [xk], [("junk",), ("ss", tt)],
             lambda a: a.activation(out=junk[:p, :], in_=xt[:p, :], func=AF.Square,
                                    accum_out=ss[:p, tt:tt + 1]))
        _rstd(S, ss[:p, tt:tt + 1], rstd[:p, tt:tt + 1], 1.0 / D, ("ss", tt), ("rstd", tt))
        S.op("dve", [("rstd", tt), xk], [xk],
             lambda v: v.tensor_scalar(out=xt[:p, :], in0=xt[:p, :], scalar1=rstd[:p, tt:tt + 1],
                                       scalar2=None, op0=ALU.mult))
        for g in range(8):
            ps, pk = S.psum()

            def tr(pe, g=g, ps=ps):
                ins = None
                for j in range(4):
                    kc = g * 4 + j
                    ins = pe.transpose(out=ps[:, j * 128: j * 128 + p],
                                       in_=xt[:p, kc * 128:(kc + 1) * 128], identity=ident[:p, :p])
                return ins
            S.op("pe", [xk, ("ident",)], [pk], tr)
            for j in range(4):
                kc = g * 4 + j
                S.op("act", [pk, ("AB",)], [hkey],
                     lambda a, kc=kc, j=j, ps=ps: a.activation(
                         out=hT[:, kc, col0 + tt * 128: col0 + tt * 128 + p],
                         in_=ps[:, j * 128: j * 128 + p], func=AF.Identity,
                         bias=B[:, kc:kc + 1], scale=A[:, kc:kc + 1]))


def _load_AB(S, nc, modT_d, gT_d, i_shift, i_scale, A, B, gt, modt):
    S.dma("sp", [], [("modt",)], lambda q: q.dma_start(out=modt[:], in_=modT_d))
    S.dma("sp", [], [("gt",)], lambda q: q.dma_start(out=gt[:], in_=gT_d))
    S.op("dve", [("modt",), ("gt",)], [("AB",)],
         lambda v: v.scalar_tensor_tensor(out=A[:], in0=modt[:, i_scale, :], scalar=1.0, in1=gt[:],
                                          op0=ALU.add, op1=ALU.mult))
    S.op("dve", [("modt",)], [("AB",)],
         lambda v: v.tensor_copy(out=B[:], in_=modt[:, i_shift, :]))


def _gelu(S, out_ap, in_ap, tmp_ap, reads, writes, tmpkey):
    S.op("dve", reads, [tmpkey],
         lambda v: v.tensor_tensor(out=tmp_ap, in0=in_ap, in1=in_ap, op=ALU.mult))
    S.op("dve", [tmpkey], [tmpkey],
         lambda v: v.tensor_scalar(out=tmp_ap, in0=tmp_ap, scalar1=0.044715, scalar2=1.0,
                                   op0=ALU.mult, op1=ALU.add))
    S.op("dve", [tmpkey] + list(reads), [tmpkey],
         lambda v: v.tensor_tensor(out=tmp_ap, in0=tmp_ap, in1=in_ap, op=ALU.mult))
    S.op("act", [tmpkey], [tmpkey],
         lambda a: a.activation(out=tmp_ap, in_=tmp_ap, func=AF.Sigmoid, scale=1.5957691216057308))
    S.op("dve", [tmpkey] + list(reads), writes,
         lambda v: v.tensor_tensor(out=out_ap, in0=tmp_ap, in1=in_ap, op=ALU.mult))


def _sched_ext(S, n_coll_sems=8):
    S.csem_coll = [S.nc.alloc_semaphore(name=f"cc{i}") for i in range(n_coll_sems)]
    S.ccnt_coll = [0] * n_coll_sems
    S.cnext = 0
    S.dsem["coll"] = S.csem_coll
    S.dcnt["coll"] = S.ccnt_coll


def s_coll(S, reads, writes, fn):
    i = S.cnext
    S.cnext = (i + 1) % len(S.csem_coll)
    k = ("d", "coll", i)
    if S.ccnt_coll[i] > 0:
        S._wait("pool", k, S.ccnt_coll[i])
    S._deps("pool", reads, writes)
    ins = fn(S.eng["pool"])
    ins.then_inc(S.csem_coll[i], 1)
    S.ccnt_coll[i] += 1
    t = (k, S.ccnt_coll[i])
    S._commit(t, reads, writes)
    return t


def s_barrier(S):
    for e in ("pe", "act", "dve", "pool", "sp"):
        for o in ("pe", "act", "dve", "pool"):
            if o != e and S.ccnt[o] > 0:
                S._wait(e, ("c", o), S.ccnt[o])
        for q in S.dsem:
            for i, c in enumerate(S.dcnt[q]):
                if c > 0:
                    S._wait(e, ("d", q, i), c)


PAIRS = [[0, 1], [2, 3], [4, 5], [6, 7]]
ALL8 = [list(range(8))]
SCALE = 128.0 ** -0.5
BIGP = 1.0e6
SLOPES = 2.0 ** (-8.0 * np.arange(1, 33) / 32.0)
DILS = (1, 4, 16)
POOL_W = (2, 4, 8, 16)
NOFF = 20


def _ag(S, in_ap, out_ap, groups, reads, writes):
    return s_coll(S, reads, writes, lambda g: g.collective_compute(
        "AllGather", ALU.bypass, replica_groups=groups, ins=[in_ap], outs=[out_ap]))


def _const_tables(core):
    hh = core % 2
    t = {}
    t["ident"] = np.eye(128, dtype=np.float32)
    k = np.arange(128)[:, None]
    q = np.arange(256)[None, :]
    d0 = np.empty((128, 256), np.float32)
    cur = (q[:, :128] - k).astype(np.float32)
    d0[:, :128] = np.where(cur >= 0, cur, BIGP)
    prv = (q[:, :128] - k + 128).astype(np.float32)
    d0[:, 128:] = np.where(prv <= 128, prv, BIGP)
    t["d0dil"] = d0
    t["d0dil4"] = np.ascontiguousarray(np.tile(d0[:, :128], (1, 4)))
    q5 = np.arange(512)[None, :]
    dm = np.empty((5, 128, 512), np.float32)
    dm[0] = q5 - k
    for j in range(4):
        v = (q5 - k).astype(np.float32)
        dm[1 + j] = np.where(v >= 128 * j, v, BIGP)
    t["d0moba"] = dm
    st = np.zeros((128, 16), np.float32)
    for gi in range(3):
        for h4 in range(4):
            st[:, gi * 4 + h4] = -SLOPES[gi + 4 * (4 * hh + h4)] * DILS[gi] / SCALE
    mb = np.zeros((128, 4 * NOFF), np.float32)
    for h4 in range(4):
        sl = SLOPES[3 + 4 * (4 * hh + h4)]
        st[:, 12 + h4] = -sl / SCALE
        for oi in range(NOFF):
            mb[:, h4 * NOFF + oi] = -sl * 128.0 * (oi - 3)
    t["slopetab"] = st
    t["mobabias"] = mb
    vb = np.zeros((128, 16, 8), np.float32)
    v01 = np.zeros((128, 16, 8), np.float32)
    for tt in range(16):
        for n in range(8):
            ok = n < tt // 2
            vb[:, tt, n] = 0.0 if ok else -1.0e30
            v01[:, tt, n] = 1.0 if ok else 0.0
    t["validbias"] = vb.reshape(128, 128)
    t["valid01"] = v01.reshape(128, 128)
    selr = np.zeros((8, 8, 128), np.float32)
    for n in range(8):
        selr[n, n, :] = 1.0
    t["selrows"] = np.ascontiguousarray(selr.transpose(1, 0, 2)).astype(ml_dtypes.bfloat16)
    pm = np.zeros((2, 3, 128, 128), np.float32)
    s_ = np.arange(128)[:, None]
    t_ = np.arange(128)[None, :]
    for pg2 in range(2):
        w = POOL_W[2 * hh + pg2]
        band = ((s_ <= t_) & (s_ > t_ - w)).astype(np.float32)
        pm[pg2, 0] = band / w - (s_ == t_)
        cnt = np.minimum(t_ + 1, w).astype(np.float32)
        pm[pg2, 1] = band / cnt - (s_ == t_)
        pm[pg2, 2] = ((s_ - 128 > t_ - w)).astype(np.float32) / w
    t["poolP"] = np.ascontiguousarray(pm.transpose(2, 0, 1, 3)).astype(ml_dtypes.bfloat16)
    t["tri"] = (s_ <= t_).astype(np.float32)
    t["halo_on"] = np.full((128, 1), float(hh), np.float32)
    t["ones_bf"] = np.ones((128, 128), ml_dtypes.bfloat16)
    return t


HH_TABS = ("slopetab", "mobabias", "poolP")


def _const_tables_all():
    t0 = _const_tables(0)
    t1 = _const_tables(1)
    out = {}
    for k in t0:
        if k in HH_TABS:
            out[k] = np.ascontiguousarray(np.stack([t0[k], t1[k]]))
        elif k != "halo_on":
            out[k] = t0[k]
    return out


W_PIECES = {
    "w_in": (512, 15360, 3072),
    "merge_w": (512, 16384, 4096),
    "branch_w": (512, 4096, 4096),
    "out_w": (512, 4096, 4096),
    "ffn_wg": (512, DFF, 5504),
    "ffn_wu": (512, DFF, 5504),
    "ffn_wd": (1376, 4096, 2048),
}


class Ctx:
    pass


def _declare_io(nc, fake=False, NL=2):
    io = Ctx()
    BIGW = ("w_ada",) + tuple(W_PIECES)
    def inp(name, shape, dt=F32):
        if fake and name in BIGW:
            return nc.dram_tensor(name, list(shape), dt).ap()
        return nc.dram_tensor(name, list(shape), dt, kind="ExternalInput").ap()
    io.x = inp("x", [S_LEN, D])
    io.cT = inp("cT", [128, 32])
    io.w_ada = inp("w_ada", [NL, D, 6 * D])
    io.b_adaT = inp("b_adaT", [NL, 128, 192])
    io.g1T = inp("g1T", [2, 128, 32])
    io.g2T = inp("g2T", [2, 128, 32])
    io.gfin = inp("gfin", [128, D])
    io.gsgu = inp("gsgu", [2, 128, 1024])
    io.sguwT = inp("sguwT", [2, 2, 128, 4, 128])
    io.sgub = inp("sgub", [2, 2, 128, 4, 512])
    io.poolw = inp("poolw", [2, 2, 128, 2, 2, 256])
    io.poolsc = inp("poolsc", [2, 2, 128, 4])
    io.mbT = inp("mbT", [2, 128, 4, 32])
    io.convw = inp("convw", [2, 128, 3, NFC])
    io.convb = inp("convb", [2, 128, NFC])
    for n, (r, c, pw) in W_PIECES.items():
        setattr(io, n, inp(n, [NL, 8 * r, c]))
    tabs = _const_tables_all()
    io.tabs = {}
    for k, v in tabs.items():
        io.tabs[k] = inp("t_" + k, v.shape, BF16 if v.dtype == ml_dtypes.bfloat16 else F32)
    io.out = nc.dram_tensor("out", [S_LEN, D], F32, kind="ExternalOutput").ap()
    return io


def _weights_phase(S, nc, io, l, W):
    for n, (r, c, pw) in W_PIECES.items():
        W[(n, l)] = [(getattr(io, n)[l], ("wext",), c)]


def _wslice(W, n, l, c0, ncols):
    pw = W[(n, l)][0][2]
    j = c0 // pw
    assert (c0 + ncols - 1) // pw == j
    ga, k, _ = W[(n, l)][j]
    return ga[:, c0 - j * pw: c0 - j * pw + ncols], k


def _mod_phase(S, nc, io, C):
    from contextlib import ExitStack
    es = ExitStack()
    cT = es.enter_context(nc.sbuf_tensor("cT_sb", [128, 32], F32))
    wa = [es.enter_context(nc.sbuf_tensor(f"wa{i}", [128, 8, 512], F32)) for i in range(3)]
    bcol = es.enter_context(nc.sbuf_tensor("bada_col", [128, 192], F32))
    msb = es.enter_context(nc.sbuf_tensor("mod_sb", [1, 2, 512], F32))
    one = es.enter_context(nc.sbuf_tensor("one_sb", [1, 1], F32))
    S.dma("sp", [], [("cT",)], lambda q: q.dma_start(out=cT[:], in_=io.cT))
    S.op("dve", [], [("one",)], lambda v: v.memset(one[:], 1.0))
    wv = io.w_ada.rearrange("l (kc p) n -> l p kc n", p=128)
    di = 0
    pcol, pck = S._ps[7], ("ps", 7)
    rot = 0
    for l in range(C.n_layers):
        S.dma("sp", [], [("bcol",)], lambda q: q.dma_start(out=bcol[:], in_=io.b_adaT[l]))
        for cb in range(48):
            ps, pk = S._ps[rot % 6], ("ps", rot % 6)
            rot += 1
            for kg in range(4):
                t = wa[di % 3]
                wk = ("wa", di % 3)
                di += 1
                S.dma("sp", [], [wk], lambda q: q.dma_start(
                    out=t[:], in_=wv[l, :, kg * 8:(kg + 1) * 8, cb * 512:(cb + 1) * 512]))

                def mm(pe, t=t, kg=kg, ps=ps):
                    ins = None
                    for k8 in range(8):
                        kc = kg * 8 + k8
                        ins = pe.matmul(ps[:1, :], cT[:, kc:kc + 1], t[:, k8, :],
                                        start=(kc == 0), stop=(kc == 31))
                    return ins
                S.op("pe", [wk, ("cT",)], [pk], mm)
            mk = ("msb", cb % 2)
            S.op("act", [pk], [mk], lambda a: a.copy(out=msb[:, cb % 2, :], in_=ps[:1, :]))

            def tr(pe, cb=cb):
                ins = None
                for j4 in range(4):
                    gc = cb * 4 + j4
                    ins = pe.matmul(pcol[:, gc:gc + 1], msb[0:1, cb % 2, j4 * 128:(j4 + 1) * 128], one[0:1, 0:1],
                                    start=True, stop=True)
                return ins
            S.op("pe", [mk, ("one",)], [pck], tr)
        S.op("dve", [pck, ("bcol",)], [("modcols",)], lambda v: v.tensor_tensor(
            out=C.modcols[:, l, :], in0=pcol[:, 0:192], in1=bcol[:], op=ALU.add))
    s_barrier(S)
    es.close()


def _mod_ap(C, l, m, kc):
    return C.modcols[:, l, m * 32 + kc: m * 32 + kc + 1]


def _make_AB(S, C, l, m_shift, m_scale, gT_d, A, B, gt):
    S.dma("sp", [("AB",)], [("gt",)], lambda q: q.dma_start(out=gt[:], in_=gT_d))
    S.op("dve", [("modcols",), ("gt",)], [("AB",)], lambda v: v.scalar_tensor_tensor(
        out=A[:], in0=C.modcols[:, l, m_scale * 32:(m_scale + 1) * 32], scalar=1.0, in1=gt[:],
        op0=ALU.add, op1=ALU.mult))
    S.op("dve", [("modcols",)], [("AB",)], lambda v: v.tensor_copy(
        out=B[:], in_=C.modcols[:, l, m_shift * 32:(m_shift + 1) * 32]))


def _make_rep(S, C, l, m, Grep, diag, key):
    for g in range(8):
        ps, pk = S.psum()
        for j in range(4):
            kc = g * 4 + j
            dg = diag[(kc) % 2]
            dk = ("diag", kc % 2)
            S.op("dve", [("modcols",), ("ident",)], [dk], lambda v, dg=dg, kc=kc: v.tensor_scalar(
                out=dg[:], in0=C.ident[:], scalar1=_mod_ap(C, l, m, kc), scalar2=None, op0=ALU.mult))
            S.op("pe", [dk, ("onesf",)], [pk], lambda pe, dg=dg, j=j, ps=ps: pe.matmul(
                ps[:, j * 128:(j + 1) * 128], C.onesf[:], dg[:], start=True, stop=True))
        S.op("act", [pk], [key], lambda a, g=g, ps=ps: a.copy(out=Grep[:, g * 512:(g + 1) * 512], in_=ps[:, :]))


P1_FB = [(0, 0, 0, True), (512, 1, 0, True), (2048, 0, 512, False), (2560, 1, 512, False)]
for _gi in range(3):
    for _hh in range(2):
        P1_FB.append((3072 + _gi * 1024 + _hh * 512, _hh, 1024 + _gi * 512, False))
        P1_FB.append((6144 + _gi * 1024 + _hh * 512, _hh, 2560 + _gi * 512, False))
for _hh in range(2):
    P1_FB.append((12288 + _hh * 512, _hh, 4096, False))
    P1_FB.append((13312 + _hh * 512, _hh, 4608, False))
P1_TB = [(1024, 0, 0, True), (1536, 1, 0, True)]
for _gi in range(3):
    for _hh in range(2):
        P1_TB.append((9216 + _gi * 1024 + _hh * 512, _hh, 512 + _gi * 512, False))
for _hh in range(2):
    P1_TB.append((14336 + _hh * 512, _hh, 2048, False))


def _phase1(S, nc, io, C, W, l, x_src, th):
    hT_d = C.hT_d[th]
    L = f"{l}_{th}"
    from contextlib import ExitStack
    es_ = ExitStack()
    hT = es_.enter_context(nc.sbuf_tensor(f"hT_{L}", [128, 32, TPC], BF16))
    wb0 = es_.enter_context(nc.sbuf_tensor(f"wb0_{L}", [128, 32, 512], BF16))
    wb1 = es_.enter_context(nc.sbuf_tensor(f"wb1_{L}", [128, 32, 512], BF16))
    big = es_.enter_context(nc.sbuf_tensor(f"big_{L}", [128, 8192], F32))
    junk = es_.enter_context(nc.sbuf_tensor(f"junk_{L}", [128, 4096], BF16))
    gsgu = es_.enter_context(nc.sbuf_tensor(f"gsgu_{L}", [128, 1024], F32))
    stF_ = es_.enter_context(nc.sbuf_tensor(f"stF_{L}", [128, 4, 512], BF16))
    gin_ = es_.enter_context(nc.sbuf_tensor(f"gin_{L}", [128, 2, 512], F32))
    gtmp_ = es_.enter_context(nc.sbuf_tensor(f"gtmp_{L}", [128, 2, 512], F32))
    with es_:
        wb = [wb0, wb1]
        xbufs = [big[:, 0:4096], big[:, 4096:8192]]
        S.dma("sp", [], [("gsgu",)], lambda q: q.dma_start(out=gsgu[:], in_=io.gsgu[l]))
        S.op("dve", [], [("ss", i) for i in range(16)], lambda v: v.memset(C.ss[:], 0.0))
        _make_AB(S, C, l, 0, 1, io.g1T[l], C.A, C.B, C.gt)
        blocks = [("F",) + b for b in P1_FB] + [("T",) + b for b in P1_TB]

        def load_w(bi):
            c0 = blocks[bi][1]
            t = wb[bi % 2]
            ap, k = _wslice(W, "w_in", l, c0, 512)
            S.dma("pool", [k], [("wb", bi % 2)], lambda q: q.dma_start(
                out=t[:], in_=ap.rearrange("(kc p) n -> p kc n", p=128)))
        load_w(0)
        load_w(1)
        _norm_to_hT(S, nc, x_src, TPC, xbufs, junk, C.ss, C.rstd, C.ident, C.A, C.B, hT, ("hT",), 0)
        hTdv = hT_d.rearrange("(kc p) t -> p kc t", p=128)
        for k4 in range(8):
            S.dma("sp", [("hT",)], [("hT_d",)], lambda q: q.dma_start(
                out=hTdv[:, k4 * 4:(k4 + 1) * 4, :], in_=hT[:, k4 * 4:(k4 + 1) * 4, :]))
        vg = big
        evi = 0
        for bi, (kind, c0, hh, o0, is_gelu) in enumerate(blocks):
            t = wb[bi % 2]
            wk = ("wb", bi % 2)
            if kind == "F":
                for j in range(4):
                    for t5 in range(2):
                        ps, pk = S.psum()
                        _mm_group(S, ps[:, :], pk,
                                  [(t[:, kc, j * 128:(j + 1) * 128], hT[:, kc, t5 * 512:(t5 + 1) * 512])
                                   for kc in range(32)], [wk, ("hT",)])
                        st = stF_[:, evi % 4, :]
                        sk = ("stF", evi % 4)
                        if is_gelu:
                            S.op("act", [pk], [sk], lambda a: a.activation(out=st, in_=ps[:, :], func=AF.Gelu_apprx_tanh))
                        elif evi % 2 == 0:
                            S.op("act", [pk], [sk], lambda a: a.copy(out=st, in_=ps[:, :]))
                        else:
                            S.op("dve", [pk], [sk], lambda v: v.tensor_copy(out=st, in_=ps[:, :]))
                        r0 = o0 + j * 128
                        S.dma("sp", [sk], [("zF_d",)], lambda q: q.dma_start(
                            out=C.zF_d[hh, th, r0:r0 + 128, t5 * 512:(t5 + 1) * 512], in_=st))
                        evi += 1
            else:
                for tt in range(8):
                    ps, pk = S.psum()
                    _mm_group(S, ps[:, :], pk,
                              [(hT[:, kc, tt * 128:(tt + 1) * 128], t[:, kc, :]) for kc in range(32)],
                              [wk, ("hT",)])
                    if is_gelu:
                        half = hh
                        vdst = vg[:, tt * 1024 + half * 512: tt * 1024 + half * 512 + 512]
                        S.op("act", [pk], [("vg", tt)], lambda a: a.activation(out=vdst, in_=ps[:, :], func=AF.Gelu_apprx_tanh))
                        if half == 1:
                            vt = vg[:, tt * 1024:(tt + 1) * 1024]
                            S.op("act", [("vg", tt)], [("junk",), ("ss", 8 + tt)],
                                 lambda a: a.activation(out=junk[:, 0:1024], in_=vt, func=AF.Square,
                                                        accum_out=C.ss[:, 8 + tt:9 + tt]))
                            _rstd(S, C.ss[:, 8 + tt:9 + tt], C.rstd[:, 8 + tt:9 + tt], 1.0 / 1024,
                                  ("ss", 8 + tt), ("rstd", 8 + tt))
                            for h2 in range(2):
                                st = stF_[:, evi % 4, :]
                                sk = ("stF", evi % 4)
                                S.op("dve", [("vg", tt), ("rstd", 8 + tt), ("gsgu",)], [sk],
                                     lambda v: v.scalar_tensor_tensor(
                                         out=st, in0=vt[:, h2 * 512:(h2 + 1) * 512],
                                         scalar=C.rstd[:, 8 + tt:9 + tt], in1=gsgu[:, h2 * 512:(h2 + 1) * 512],
                                         op0=ALU.mult, op1=ALU.mult))
                                S.dma("sp", [sk], [("zT_d",)], lambda q: q.dma_start(
                                    out=C.zT_d[h2, th, tt * 128:(tt + 1) * 128, 0:512], in_=st))
                                evi += 1
                    else:
                        st = stF_[:, evi % 4, :]
                        sk = ("stF", evi % 4)
                        if evi % 2 == 0:
                            S.op("act", [pk], [sk], lambda a: a.copy(out=st, in_=ps[:, :]))
                        else:
                            S.op("dve", [pk], [sk], lambda v: v.tensor_copy(out=st, in_=ps[:, :]))
                        S.dma("sp", [sk], [("zT_d",)], lambda q: q.dma_start(
                            out=C.zT_d[hh, th, tt * 128:(tt + 1) * 128, o0:o0 + 512], in_=st))
                        evi += 1
            if bi + 2 < len(blocks):
                load_w(bi + 2)
        s_barrier(S)


def _phase2(S, nc, io, C, l, hh):
    yT_d = C.y_d[hh]
    L = f"{l}_{hh}"
    zF_my = C.zF_d[hh]
    zT_my = C.zT_d[hh].rearrange("hf t c -> (hf t) c")
    zT5 = zT_my.rearrange("(hf n p) c -> hf p n c", hf=2, n=8, p=128)
    zT4 = zT_my.rearrange("(hf n2 j r) c -> hf r j n2 c", hf=2, n2=2, j=128, r=4)
    zT16 = zT_my.rearrange("(hf j2 r) c -> hf j2 r c", hf=2, j2=64, r=16)
    T = io.tabs
    srot = [0]

    def s_tile():
        i = srot[0] % 4
        srot[0] += 1
        return S._ps[i], ("ps", i)

    def ldF(dst, r0, key):
        for hf in range(2):
            S.dma("sp", [("zFg",)], [key], lambda q, hf=hf: q.dma_start(
                out=dst[:, hf * 1024:(hf + 1) * 1024], in_=zF_my[hf, r0:r0 + 128, :]))

    def ldV1(dst, c0, key):
        for hf in range(2):
            S.dma("sp", [("zTg",)], [key], lambda q, hf=hf: q.dma_start(
                out=dst[:, hf * 8:(hf + 1) * 8, :], in_=zT5[hf, :, :, c0:c0 + 128]))

    from contextlib import ExitStack
    es_ = ExitStack()
    fa = es_.enter_context(nc.sbuf_tensor(f"p2a_{L}", [128, 4, 2048], BF16))
    fb = es_.enter_context(nc.sbuf_tensor(f"p2b_{L}", [128, 4, 2048], BF16))
    vn = es_.enter_context(nc.sbuf_tensor(f"p2v_{L}", [128, 16, 512], BF16))
    wtmp = es_.enter_context(nc.sbuf_tensor(f"p2w_{L}", [128, 4, 128], F32))
    WcT = es_.enter_context(nc.sbuf_tensor(f"p2wc_{L}", [128, 4, 128], BF16))
    bsrep = es_.enter_context(nc.sbuf_tensor(f"p2bs_{L}", [128, 4, 512], F32))
    tmpf = es_.enter_context(nc.sbuf_tensor(f"p2t_{L}", [128, 2, 512], F32))
    Pbf = es_.enter_context(nc.sbuf_tensor(f"p2p_{L}", [128, 2, 512], BF16))
    wp = es_.enter_context(nc.sbuf_tensor(f"p2wp_{L}", [128, 2, 2, 256], BF16))
    poolP = es_.enter_context(nc.sbuf_tensor(f"p2pp_{L}", [128, 2, 3, 128], BF16))
    zW = es_.enter_context(nc.sbuf_tensor(f"p2zw_{L}", [128, 17, 2, 256], BF16))
    poolsc = es_.enter_context(nc.sbuf_tensor(f"p2ps_{L}", [128, 4], F32))
    acc = es_.enter_context(nc.sbuf_tensor(f"p2acc_{L}", [128, 2, 2048], F32))
    Vt = es_.enter_context(nc.sbuf_tensor(f"p2V_{L}", [128, 3, 16, 128], BF16))
    d0dil = es_.enter_context(nc.sbuf_tensor(f"p2d0_{L}", [128, 256], F32))
    d0dil4 = es_.enter_context(nc.sbuf_tensor(f"p2d4_{L}", [128, 512], F32))
    d0moba = es_.enter_context(nc.sbuf_tensor(f"p2dm_{L}", [128, 5, 512], F32))
    slopetab = es_.enter_context(nc.sbuf_tensor(f"p2st_{L}", [128, 16], F32))
    mobabias = es_.enter_context(nc.sbuf_tensor(f"p2mb_{L}", [128, 4 * NOFF], F32))
    validbias = es_.enter_context(nc.sbuf_tensor(f"p2vb_{L}", [128, 128], F32))
    valid01 = es_.enter_context(nc.sbuf_tensor(f"p2v1_{L}", [128, 128], F32))
    selrows = es_.enter_context(nc.sbuf_tensor(f"p2sr_{L}", [8, 8, 128], BF16))
    selT = es_.enter_context(nc.sbuf_tensor(f"p2sT_{L}", [8, 2048], BF16))
    gsb = es_.enter_context(nc.sbuf_tensor(f"p2g_{L}", [128, 4, 128], F32))
    kmf = es_.enter_context(nc.sbuf_tensor(f"p2km_{L}", [128, 4, 8], F32))
    kmhl = es_.enter_context(nc.sbuf_tensor(f"p2kh_{L}", [128, 2, 8], BF16))
    tri = es_.enter_context(nc.sbuf_tensor(f"p2tri_{L}", [128, 128], F32))
    with es_:
        for dst, nm in ((d0dil, "d0dil"), (d0dil4, "d0dil4"), (slopetab, "slopetab"), (mobabias, "mobabias"),
                        (validbias, "validbias"), (valid01, "valid01"), (selrows, "selrows"),
                        (poolP, "poolP"), (tri, "tri")):
            S.dma("sp", [], [("c", nm)], lambda q, dst=dst, nm=nm: q.dma_start(out=dst[:], in_=(T[nm][hh] if nm in HH_TABS else T[nm])))
        S.dma("sp", [], [("c", "d0moba")], lambda q: q.dma_start(
            out=d0moba[:], in_=T["d0moba"].rearrange("v p q -> p v q")))
        S.dma("sp", [], [("wtmp",)], lambda q: q.dma_start(out=wtmp[:], in_=io.sguwT[l, hh]))
        S.dma("sp", [], [("bsrep",)], lambda q: q.dma_start(out=bsrep[:], in_=io.sgub[l, hh]))
        S.dma("pool", [], [("wp",)], lambda q: q.dma_start(out=wp[:], in_=io.poolw[l, hh]))
        S.dma("sp", [], [("poolsc",)], lambda q: q.dma_start(out=poolsc[:], in_=io.poolsc[l, hh]))
        for g4 in range(4):
            S.op("dve", [("wtmp",), ("c", "tri")], [("WcT",)], lambda v, g4=g4: v.tensor_tensor(
                out=WcT[:, g4, :], in0=wtmp[:, g4, :], in1=tri[:], op=ALU.mult))

        for g4 in range(4):
            ldF(fa[:, g4, :], g4 * 128, ("fa", g4))
        for hf in range(2):
            S.dma("sp", [("zTg",)], [("vn",)], lambda q, hf=hf: q.dma_start(
                out=vn[:, hf * 8:(hf + 1) * 8, :], in_=zT5[hf, :, :, 0:512]))
        for g4 in range(4):
            for tg in range(4):
                ps, pk = S.psum()

                def mm(pe, g4=g4, tg=tg, ps=ps):
                    ins = None
                    for i in range(4):
                        ins = pe.matmul(ps[:, i * 128:(i + 1) * 128], vn[:, tg * 4 + i, g4 * 128:(g4 + 1) * 128],
                                        WcT[:, g4, :], start=True, stop=True)
                    return ins
                S.op("pe", [("vn",), ("WcT",)], [pk], mm)
                tk = ("tmpf", tg % 2)
                S.op("dve", [pk, ("bsrep",)], [tk], lambda v, g4=g4, tg=tg, ps=ps: v.tensor_tensor(
                    out=tmpf[:, tg % 2, :], in0=ps[:, :], in1=bsrep[:, g4, :], op=ALU.add))
                S.op("dve", [tk, ("fa", g4)], [("fb", g4)], lambda v, g4=g4, tg=tg: v.tensor_tensor(
                    out=fb[:, g4, tg * 512:(tg + 1) * 512], in0=tmpf[:, tg % 2, :],
                    in1=fa[:, g4, tg * 512:(tg + 1) * 512], op=ALU.mult))
            S.dma("sp", [("fb", g4)], [("yT_d",)], lambda q, g4=g4: q.dma_start(
                out=yT_d[g4 * 128:(g4 + 1) * 128, :], in_=fb[:, g4, :]))

        for i in range(4):
            ldF(fa[:, i, :], 512 + i * 128, ("fa", i))
        S.op("dve", [], [("zW", 0)], lambda v: v.memset(zW[:, 0, :, :], 0.0))
        for pg2 in range(2):
            for t2 in range(8):
                ps, pk = S.psum()

                def mm(pe, pg2=pg2, t2=t2, ps=ps):
                    ins = None
                    for i in range(2):
                        tt = t2 * 2 + i
                        for cc in range(2):
                            ins = pe.matmul(ps[:, i * 256:(i + 1) * 256],
                                            fa[:, pg2 * 2 + cc, tt * 128:(tt + 1) * 128], wp[:, pg2, cc, :],
                                            start=(cc == 0), stop=(cc == 1))
                    return ins
                S.op("pe", [("fa", pg2 * 2), ("fa", pg2 * 2 + 1), ("wp",)], [pk], mm)
                S.op("act", [pk], [("zW", 1)], lambda a, pg2=pg2, t2=t2, ps=ps: a.copy(
                    out=zW[:, 1 + t2 * 2: 3 + t2 * 2, pg2, :], in_=ps[:, :].rearrange("p (i e) -> p i e", i=2)))
        for pg2 in range(2):
            for ec in range(2):
                oc = pg2 * 2 + ec
                for tg in range(4):
                    ps, pk = S.psum()

                    def mm(pe, pg2=pg2, ec=ec, tg=tg, ps=ps):
                        ins = None
                        for i in range(4):
                            tt = tg * 4 + i
                            pe.matmul(ps[:, i * 128:(i + 1) * 128], zW[:, tt + 1, pg2, ec * 128:(ec + 1) * 128],
                                      poolP[:, pg2, (1 if tt == 0 else 0), :], start=True, stop=False)
                            ins = pe.matmul(ps[:, i * 128:(i + 1) * 128], zW[:, tt, pg2, ec * 128:(ec + 1) * 128],
                                            poolP[:, pg2, 2, :], start=False, stop=True)
                        return ins
                    S.op("pe", [("zW", 0), ("zW", 1), ("c", "poolP")], [pk], mm)
                    S.op("dve", [pk, ("poolsc",)], [("fb", oc)], lambda v, oc=oc, tg=tg, ps=ps: v.tensor_scalar(
                        out=fb[:, oc, tg * 512:(tg + 1) * 512], in0=ps[:, :], scalar1=poolsc[:, oc:oc + 1],
                        scalar2=None, op0=ALU.mult))
                S.dma("sp", [("fb", oc)], [("yT_d",)], lambda q, oc=oc: q.dma_start(
                    out=yT_d[512 + oc * 128: 512 + (oc + 1) * 128, :], in_=fb[:, oc, :]))

        accO = acc[:, 0, :]
        accD = acc[:, 1, :]
        segn = [0]

        def od_tiles():
            i = 4 + 2 * (segn[0] % 2)
            segn[0] += 1
            return (S._ps[i], ("ps", i)), (S._ps[i + 1], ("ps", i + 1))

        def softmax_pv(ps_s, sk, lo, hi, dtab, ccol, bias_ap, pv_list, extra_reads):
            flush_pv()
            pi = segn[1] % 2
            segn[1] += 1
            xk = ("tmpf", pi)
            S.op("dve", [sk, ("c", "d0dil"), ("c", "d0dil4"), ("c", "d0moba"), ("c", "slopetab")], [xk], lambda v: v.scalar_tensor_tensor(
                out=tmpf[:, pi, lo:hi], in0=dtab, scalar=slopetab[:, ccol:ccol + 1], in1=ps_s[:, lo:hi],
                op0=ALU.mult, op1=ALU.add))
            pk_ = ("Pbf", pi)
            if bias_ap is None:
                S.op("act", [xk], [pk_], lambda a: a.activation(out=Pbf[:, pi, lo:hi], in_=tmpf[:, pi, lo:hi],
                                                                func=AF.Exp, scale=SCALE))
            else:
                S.op("act", [xk, ("c", "mobabias")], [pk_], lambda a: a.activation(
                    out=Pbf[:, pi, lo:hi], in_=tmpf[:, pi, lo:hi], func=AF.Exp, scale=SCALE, bias=bias_ap))
            def emit_pv():
                for (otile, okey, ocol, ncol, pcol, lhsT, start, stop) in pv_list:
                    S.op("pe", [pk_] + extra_reads, [okey], lambda pe: pe.matmul(
                        otile[:, ocol:ocol + ncol], lhsT, Pbf[:, pi, pcol:pcol + ncol], start=start, stop=stop))
            pend.append(emit_pv)
        segn.append(0)
        pend = []

        def flush_pv():
            for f in pend:
                f()
            pend.clear()

        for h4 in range(4):
            for gi in range(3):
                ldF(fa[:, 0, :], 1024 + gi * 512 + h4 * 128, ("fa", 0))
                ldF(fa[:, 1, :], 2560 + gi * 512 + h4 * 128, ("fa", 1))
                qT = fa[:, 0, :]
                kT = fa[:, 1, :]
                c0 = 512 + gi * 512 + h4 * 128
                V = Vt[:, gi, :, :]
                vk = ("Vt", gi)
                if gi == 0:
                    ldV1(V, c0, vk)
                elif gi == 1:
                    for hf in range(2):
                        for r in range(4):
                            S.dma("sp", [("zTg",)], [vk], lambda q, hf=hf, r=r: q.dma_start(
                                out=V[:, r * 4 + hf * 2: r * 4 + hf * 2 + 2, :],
                                in_=zT4[hf, r, :, :, c0:c0 + 128]))
                else:
                    for hf in range(2):
                        S.dma("sp", [("zTg",)], [vk], lambda q, hf=hf: q.dma_start(
                            out=Vt[hf * 64:(hf + 1) * 64, gi, :, :], in_=zT16[hf, :, :, c0:c0 + 128]))
                ccol = gi * 4 + h4
                rd = [("fa", 0), ("fa", 1)]
                for seg in range(4):
                    (psO, ok), (psD, dk) = od_tiles()
                    if gi < 2:
                        d = DILS[gi]
                        a = seg if gi == 0 else 0
                        base = 0 if gi == 0 else seg
                        nlo = 4 * a - 1 if (gi == 0 and a > 0) else 4 * a

                        def tok(n, cnt):
                            if gi == 0:
                                return slice(n * 128, n * 128 + cnt)
                            return slice(512 * n + base, 512 * n + base + 4 * (cnt - 1) + 1, 4)

                        def vidx(n):
                            return n if gi == 0 else base * 4 + n
                        for n in range(nlo, 4 * a + 4):
                            cur_ok = n >= 4 * a
                            nxt_ok = n + 1 <= 4 * a + 3
                            ps_s, sk = s_tile()
                            if cur_ok and nxt_ok:
                                lo, hi = 0, 256
                                S.op("pe", rd, [sk], lambda pe: pe.matmul(
                                    ps_s[:, 0:256], kT[:, tok(n, 128)], qT[:, tok(n, 256)], start=True, stop=True))
                            elif nxt_ok:
                                lo, hi = 128, 256
                                S.op("pe", rd, [sk], lambda pe: pe.matmul(
                                    ps_s[:, 128:256], kT[:, tok(n, 128)], qT[:, tok(n + 1, 128)], start=True, stop=True))
                            else:
                                lo, hi = 0, 128
                                S.op("pe", rd, [sk], lambda pe: pe.matmul(
                                    ps_s[:, 0:128], kT[:, tok(n, 128)], qT[:, tok(n, 128)], start=True, stop=True))
                            pv = []
                            if cur_ok:
                                oc = (n - 4 * a) * 128
                                st = (n == 0)
                                pv.append((psO, ok, oc, 128, 0, V[:, vidx(n), :], st, True))
                                pv.append((psD, dk, oc, 128, 0, C.ones_bf[:], st, True))
                            if nxt_ok:
                                oc = (n + 1 - 4 * a) * 128
                                pv.append((psO, ok, oc, 128, 128, V[:, vidx(n), :], True, False))
                                pv.append((psD, dk, oc, 128, 128, C.ones_bf[:], True, False))
                            softmax_pv(ps_s, sk, lo, hi, d0dil[:, lo:hi], ccol, None, pv, [vk, ("ones_bf",)])
                    else:
                        ps_s, sk = s_tile()

                        def mm(pe, seg=seg, ps_s=ps_s):
                            ins = None
                            for i in range(4):
                                r = seg * 4 + i
                                sl = slice(r, r + 16 * 127 + 1, 16)
                                ins = pe.matmul(ps_s[:, i * 128:(i + 1) * 128], kT[:, sl], qT[:, sl],
                                                start=True, stop=True)
                            return ins
                        S.op("pe", rd, [sk], mm)
                        pv = []
                        for i in range(4):
                            r = seg * 4 + i
                            pv.append((psO, ok, i * 128, 128, i * 128, V[:, r, :], True, True))
                            pv.append((psD, dk, i * 128, 128, i * 128, C.ones_bf[:], True, True))
                        softmax_pv(ps_s, sk, 0, 512, d0dil4[:, :], ccol, None, pv, [vk, ("ones_bf",)])
                    flush_pv()
                    if gi == 0:
                        S.op("act", [ok], [("accO",)], lambda a_: a_.copy(out=accO[:, seg * 512:(seg + 1) * 512], in_=psO[:, :]))
                        S.op("dve", [dk], [("accD",)], lambda v: v.tensor_copy(out=accD[:, seg * 512:(seg + 1) * 512], in_=psD[:, :]))
                    elif gi == 1:
                        sl = slice(seg, 2048, 4)
                        S.op("dve", [ok], [("accO",)], lambda v: v.tensor_tensor(out=accO[:, sl], in0=accO[:, sl], in1=psO[:, :], op=ALU.add))
                        S.op("dve", [dk], [("accD",)], lambda v: v.tensor_tensor(out=accD[:, sl], in0=accD[:, sl], in1=psD[:, :], op=ALU.add))
                    else:
                        for (at, ak, pt, pk2) in ((accO, ("accO",), psO, ok), (accD, ("accD",), psD, dk)):
                            av = at.rearrange("p (j r) -> p r j", r=16)[:, seg * 4:(seg + 1) * 4, :]
                            pv_ = pt[:, :].rearrange("p (i j) -> p i j", i=4)
                            S.op("dve", [pk2], [ak], lambda v, av=av, pv_=pv_: v.tensor_tensor(out=av, in0=av, in1=pv_, op=ALU.add))
            S.op("dve", [("accD",)], [("accD",)], lambda v: v.reciprocal(out=accD, in_=accD))
            S.op("dve", [("accD",), ("accO",)], [("fb", h4)], lambda v: v.tensor_tensor(
                out=fb[:, h4, :], in0=accO, in1=accD, op=ALU.mult))
            S.dma("sp", [("fb", h4)], [("yT_d",)], lambda q: q.dma_start(
                out=yT_d[1024 + h4 * 128: 1024 + (h4 + 1) * 128, :], in_=fb[:, h4, :]))

        gm = gsb[:, 0, :]
        top8 = gsb[:, 1, :]
        sel = gsb[:, 2, :]
        for h4 in range(4):
            ldF(fa[:, 2, :], 4096 + h4 * 128, ("fa", 2))
            ldF(fa[:, 3, :], 4608 + h4 * 128, ("fa", 3))
            qT = fa[:, 2, :]
            kT = fa[:, 3, :]
            V = Vt[:, 0, :, :]
            vk = ("Vt", 0)
            ldV1(V, 2048 + h4 * 128, vk)
            rd = [("fa", 2), ("fa", 3)]
            S.op("dve", [("fa", 3)], [("kmf",)], lambda v: v.reduce_sum(
                out=kmf[:, 0, :], in_=kT.rearrange("p (n t) -> p n t", n=8), axis=AX.X))
            S.op("dve", [("kmf",)], [("kmf",)], lambda v: v.tensor_scalar(
                out=kmf[:, 0, :], in0=kmf[:, 0, :], scalar1=1.0 / 256, scalar2=None, op0=ALU.mult))
            S.op("dve", [("kmf",)], [("kmhl",)], lambda v: v.tensor_copy(out=kmhl[:, 0, :], in_=kmf[:, 0, :]))
            S.op("dve", [("kmhl",)], [("kmf",)], lambda v: v.tensor_copy(out=kmf[:, 1, :], in_=kmhl[:, 0, :]))
            S.op("dve", [("kmf",)], [("kmf",)], lambda v: v.tensor_tensor(
                out=kmf[:, 2, :], in0=kmf[:, 0, :], in1=kmf[:, 1, :], op=ALU.subtract))
            S.op("dve", [("kmf",)], [("kmhl",)], lambda v: v.tensor_copy(out=kmhl[:, 1, :], in_=kmf[:, 2, :]))
            psg, gk = s_tile()

            def gate(pe, psg=psg):
                ins = None
                for tt in range(16):
                    pe.matmul(psg[:, tt * 8:(tt + 1) * 8], qT[:, tt * 128:(tt + 1) * 128], kmhl[:, 0, :],
                              start=True, stop=False)
                    ins = pe.matmul(psg[:, tt * 8:(tt + 1) * 8], qT[:, tt * 128:(tt + 1) * 128], kmhl[:, 1, :],
                                    start=False, stop=True)
                return ins
            S.op("pe", rd + [("kmhl",)], [gk], gate)
            S.op("dve", [gk, ("c", "validbias")], [("gm",)], lambda v: v.tensor_tensor(
                out=gm, in0=psg[:, 0:128], in1=validbias[:], op=ALU.add))
            for tt in range(16):
                S.op("dve", [("gm",)], [("top8",)], lambda v, tt=tt: v.max(
                    out=top8[:, tt * 8:(tt + 1) * 8], in_=gm[:, tt * 8:(tt + 1) * 8]))
            for tt in range(16):
                S.op("dve", [("gm",), ("top8",)], [("sel",)], lambda v, tt=tt: v.tensor_scalar(
                    out=sel[:, tt * 8:(tt + 1) * 8], in0=gm[:, tt * 8:(tt + 1) * 8],
                    scalar1=top8[:, tt * 8 + 2: tt * 8 + 3], scalar2=None, op0=ALU.is_ge))
            S.op("dve", [("sel",)], [("sel",)], lambda v: v.tensor_scalar(
                out=sel, in0=sel, scalar1=BIG, scalar2=-BIG, op0=ALU.mult, op1=ALU.add))
            S.op("dve", [("sel",), ("c", "valid01")], [("sel",)], lambda v: v.tensor_tensor(
                out=sel, in0=sel, in1=valid01[:], op=ALU.mult))
            for g in range(4):
                pst, tk = s_tile()

                def trs(pe, g=g, pst=pst):
                    ins = None
                    for i in range(4):
                        tt = g * 4 + i
                        ins = pe.transpose(out=pst[:8, i * 128:(i + 1) * 128], in_=sel[:, tt * 8:(tt + 1) * 8],
                                           identity=C.ident[:, :])
                    return ins
                S.op("pe", [("sel",), ("ident",)], [tk], trs)
                S.op("act", [tk], [("selT",)], lambda a, g=g, pst=pst: a.copy(
                    out=selT[:8, g * 512:(g + 1) * 512], in_=pst[:8, :]))
            for qr in range(4):
                (psO, ok), (psD, dk) = od_tiles()
                nks = 4 * qr + 4
                for ks in range(nks):
                    ps_s, sk = s_tile()

                    def mm(pe, ks=ks, qr=qr, ps_s=ps_s):
                        pe.matmul(ps_s[:, :], kT[:, ks * 128:(ks + 1) * 128], qT[:, qr * 512:(qr + 1) * 512],
                                  start=True, stop=False)
                        return pe.matmul(ps_s[:, :], selrows[:8, ks // 2, :], selT[:8, qr * 512:(qr + 1) * 512],
                                         start=False, stop=True)
                    S.op("pe", rd + [("selT",), ("c", "selrows")], [sk], mm)
                    j = ks - 4 * qr
                    dv = 0 if j < 0 else 1 + j
                    oi = (4 * qr - ks) + 3
                    bias_ap = mobabias[:, h4 * NOFF + oi: h4 * NOFF + oi + 1]
                    pv = [(psO, ok, 0, 512, 0, V[:, ks, :], ks == 0, ks == nks - 1),
                          (psD, dk, 0, 512, 0, C.ones_bf[:], ks == 0, ks == nks - 1)]
                    softmax_pv(ps_s, sk, 0, 512, d0moba[:, dv, :], 12 + h4, bias_ap, pv, [vk, ("ones_bf",)])
                flush_pv()
                rk = ("tmpf", qr % 2)
                S.op("dve", [dk], [rk], lambda v: v.reciprocal(out=tmpf[:, qr % 2, :], in_=psD[:, :]))
                S.op("dve", [rk, ok], [("fb", h4)], lambda v: v.tensor_tensor(
                    out=fb[:, h4, qr * 512:(qr + 1) * 512], in0=psO[:, :], in1=tmpf[:, qr % 2, :], op=ALU.mult))
            S.dma("sp", [("fb", h4)], [("yT_d",)], lambda q: q.dma_start(
                out=yT_d[1536 + h4 * 128: 1536 + (h4 + 1) * 128, :], in_=fb[:, h4, :]))
        s_barrier(S)


def _rep_piece(S, C, l, m, eb, dst, dkey):
    ps, pk = S.psum()
    for j in range(4):
        kc = eb * 4 + j
        dg = C.diag[kc % 2]
        dk = ("diag", kc % 2)
        S.op("dve", [("modcols",), ("ident",)], [dk], lambda v: v.tensor_scalar(
            out=dg[:], in0=C.ident[:], scalar1=_mod_ap(C, l, m, kc), scalar2=None, op0=ALU.mult))
        S.op("pe", [dk, ("onesf",)], [pk], lambda pe: pe.matmul(
            ps[:, j * 128:(j + 1) * 128], C.onesf[:], dg[:], start=True, stop=True))
    S.op("act", [pk], [dkey], lambda a: a.copy(out=dst, in_=ps[:, :]))


def _phase3a(S, nc, io, C, W, l, th):
    mT_d = C.mT_d[th]
    L = f"{l}_{th}"
    from contextlib import ExitStack
    es_ = ExitStack()
    hT = es_.enter_context(nc.sbuf_tensor(f"p3h_{L}", [128, 32, TPC], BF16))
    yT = es_.enter_context(nc.sbuf_tensor(f"p3y_{L}", [128, 32, TPC], BF16))
    mwb = es_.enter_context(nc.sbuf_tensor(f"p3mw_{L}", [128, 2, 32, 256], BF16))
    bwb = es_.enter_context(nc.sbuf_tensor(f"p3bw_{L}", [128, 2, 8, 256], BF16))
    macc = es_.enter_context(nc.sbuf_tensor(f"p3ma_{L}", [128, 4, 512], F32))
    gsig = es_.enter_context(nc.sbuf_tensor(f"p3gs_{L}", [128, 2, 512], F32))
    tmp = es_.enter_context(nc.sbuf_tensor(f"p3tm_{L}", [128, 512], F32))
    stg = es_.enter_context(nc.sbuf_tensor(f"p3st_{L}", [128, 2, 512], BF16))
    mb = es_.enter_context(nc.sbuf_tensor(f"p3mb_{L}", [128, 4, 32], F32))
    with es_:
        S.dma("sp", [], [("mb",)], lambda q: q.dma_start(out=mb[:], in_=io.mbT[l]))
        hv = C.hT_d[th].rearrange("(kc p) t -> p kc t", p=128)
        for k4 in range(8):
            S.dma("sp", [("hT_d",)], [("hT",)], lambda q: q.dma_start(
                out=hT[:, k4 * 4:(k4 + 1) * 4, :], in_=hv[:, k4 * 4:(k4 + 1) * 4, :]))
        for br in range(4):
            for hs in range(2):
                S.dma("sp", [("yT_d",)], [("yT",)], lambda q: q.dma_start(
                    out=yT[:, br * 8 + hs * 4: br * 8 + hs * 4 + 4, :],
                    in_=C.y_d[hs, br * 512:(br + 1) * 512, th * TPC:(th + 1) * TPC].rearrange("(k p) t -> p k t", p=128)))
        wi = 0
        gi_ = 0
        for eg in range(16):
            for br in range(4):
                mt = mwb[:, wi % 2, :, :]
                bt = bwb[:, wi % 2, :, :]
                mk = ("mwb", wi % 2)
                bk = ("bwb", wi % 2)
                wi += 1
                ap, k = _wslice(W, "merge_w", l, br * 4096 + eg * 256, 256)
                S.dma("pool", [k], [mk], lambda q: q.dma_start(out=mt, in_=ap.rearrange("(kc p) n -> p kc n", p=128)))
                ap2, k2 = _wslice(W, "branch_w", l, eg * 256, 256)
                S.dma("pool", [k2], [bk], lambda q: q.dma_start(
                    out=bt, in_=ap2[br * 1024:(br + 1) * 1024, :].rearrange("(kc p) n -> p kc n", p=128)))
                for e2 in range(2):
                    ec = eg * 2 + e2
                    for t5 in range(2):
                        ai = e2 * 2 + t5
                        psg, gk = S.psum()
                        _mm_group(S, psg[:, :], gk, [(mt[:, kc, e2 * 128:(e2 + 1) * 128], hT[:, kc, t5 * 512:(t5 + 1) * 512])
                                                     for kc in range(32)], [mk, ("hT",)])
                        gs = gsig[:, gi_ % 2, :]
                        gsk = ("gsig", gi_ % 2)
                        gi_ += 1
                        S.op("act", [gk, ("mb",)], [gsk], lambda a: a.activation(
                            out=gs, in_=psg[:, :], func=AF.Sigmoid, bias=mb[:, br, ec:ec + 1]))
                        psp, ppk = S.psum()
                        _mm_group(S, psp[:, :], ppk, [(bt[:, k8, e2 * 128:(e2 + 1) * 128], yT[:, br * 8 + k8, t5 * 512:(t5 + 1) * 512])
                                                      for k8 in range(8)], [bk, ("yT",)])
                        if br == 0:
                            S.op("dve", [gsk, ppk], [("macc", ai)], lambda v: v.tensor_tensor(
                                out=macc[:, ai, :], in0=gs, in1=psp[:, :], op=ALU.mult))
                        else:
                            S.op("dve", [gsk, ppk], [("tmp3",)], lambda v: v.tensor_tensor(
                                out=tmp[:], in0=gs, in1=psp[:, :], op=ALU.mult))
                            S.op("dve", [("tmp3",), ("macc", ai)], [("macc", ai)], lambda v: v.tensor_tensor(
                                out=macc[:, ai, :], in0=macc[:, ai, :], in1=tmp[:], op=ALU.add))
                        if br == 3:
                            st = stg[:, ai % 2, :]
                            sk = ("stg", ai % 2)
                            S.op("act", [("macc", ai)], [sk], lambda a: a.copy(out=st, in_=macc[:, ai, :]))
                            S.dma("sp", [sk], [("mT_d",)], lambda q: q.dma_start(
                                out=mT_d[ec * 128:(ec + 1) * 128, t5 * 512:(t5 + 1) * 512], in_=st))
        s_barrier(S)


def _epilogue_tile(S, C, ps, pk, grep, gkey, x_ap, out_ap, okey, xt, ot, ei):
    xk = ("xt", ei % 2)
    ok_ = ("ot", ei % 2)
    x_ = xt[:, ei % 2, :]
    o_ = ot[:, ei % 2, :]
    S.dma("sp", [("xsrc",)], [xk], lambda q: q.dma_start(out=x_, in_=x_ap))
    S.op("dve", [pk, gkey], [ok_], lambda v: v.tensor_tensor(out=o_, in0=ps[:, :], in1=grep, op=ALU.mult))
    S.op("dve", [ok_, xk], [ok_], lambda v: v.tensor_tensor(out=o_, in0=o_, in1=x_, op=ALU.add))
    S.dma("sp", [ok_], [okey], lambda q: q.dma_start(out=out_ap, in_=o_))


def _phase3b(S, nc, io, C, W, l, x_src, x_dst, th):
    xmid_d = C.xmid_d[th * TPC:(th + 1) * TPC, :]
    L = f"{l}_{th}"
    h2_d = nc.dram_tensor(f"h2T{L}", [D, TPC + 2], BF16).ap()
    mT_d = C.mT_d[th]
    with (nc.sbuf_tensor(f"p4m_{L}", [128, 32, TPC], BF16) as mT,
          nc.sbuf_tensor(f"p4w_{L}", [128, 2, 32, 512], BF16) as owb,
          nc.sbuf_tensor(f"p4g_{L}", [128, 2, 512], F32) as grep,
          nc.sbuf_tensor(f"p4x_{L}", [128, 2, 512], F32) as xt,
          nc.sbuf_tensor(f"p4o_{L}", [128, 2, 512], F32) as ot):
        mv = mT_d.rearrange("(kc p) t -> p kc t", p=128)
        for k4 in range(8):
            S.dma("sp", [("mT_d",)], [("mT",)], lambda q: q.dma_start(
                out=mT[:, k4 * 4:(k4 + 1) * 4, :], in_=mv[:, k4 * 4:(k4 + 1) * 4, :]))
        ei = 0
        for eb in range(8):
            wt = owb[:, eb % 2, :, :]
            wk = ("owb", eb % 2)
            ap, k = _wslice(W, "out_w", l, eb * 512, 512)
            S.dma("pool", [k], [wk], lambda q: q.dma_start(out=wt, in_=ap.rearrange("(kc p) n -> p kc n", p=128)))
            gr = grep[:, eb % 2, :]
            gk = ("grep", eb % 2)
            _rep_piece(S, C, l, 2, eb, gr, gk)
            for tt in range(8):
                ps, pk = S.psum()
                _mm_group(S, ps[:, :], pk, [(mT[:, kc, tt * 128:(tt + 1) * 128], wt[:, kc, :]) for kc in range(32)],
                          [wk, ("mT",)])
                _epilogue_tile(S, C, ps, pk, gr, gk, x_src[tt * 128:(tt + 1) * 128, eb * 512:(eb + 1) * 512],
                               xmid_d[tt * 128:(tt + 1) * 128, eb * 512:(eb + 1) * 512], ("xmid_d",), xt, ot, ei)
                ei += 1
        s_barrier(S)
    with (nc.sbuf_tensor(f"p5h_{L}", [128, 32, TPC + 2], BF16) as h2T,
          nc.sbuf_tensor(f"p5x_{L}", [128, 8192], F32) as big,
          nc.sbuf_tensor(f"p5j_{L}", [128, 4096], BF16) as junk):
        xbufs = [big[:, 0:4096], big[:, 4096:8192]]
        _make_AB(S, C, l, 3, 4, io.g2T[l], C.A, C.B, C.gt)
        S.op("dve", [], [("ss", i) for i in range(16)], lambda v: v.memset(C.ss[:], 0.0))
        _norm_to_hT(S, nc, xmid_d, TPC, xbufs, junk, C.ss, C.rstd, C.ident, C.A, C.B, h2T, ("h2T",), 2)
        S.op("dve", [("h2T",)], [("h2T",)], lambda v: v.memset(h2T[:, :, 0:2], 0.0))
        hv = h2_d.rearrange("(kc p) t -> p kc t", p=128)
        for k4 in range(8):
            S.dma("sp", [("h2T",)], [("h2_d",)], lambda q: q.dma_start(
                out=hv[:, k4 * 4:(k4 + 1) * 4, :], in_=h2T[:, k4 * 4:(k4 + 1) * 4, :]))
        s_barrier(S)
    from contextlib import ExitStack
    es_ = ExitStack()
    fT = es_.enter_context(nc.sbuf_tensor(f"p6f_{L}", [128, NFC, 512], BF16))
    h2h = es_.enter_context(nc.sbuf_tensor(f"p6h_{L}", [128, 32, 514], BF16))
    wgb = es_.enter_context(nc.sbuf_tensor(f"p6wg_{L}", [128, 2, 32, 128], BF16))
    wub = es_.enter_context(nc.sbuf_tensor(f"p6wu_{L}", [128, 2, 32, 128], BF16))
    wdb = es_.enter_context(nc.sbuf_tensor(f"p6wd_{L}", [128, 2, 4, 512], BF16))
    gsb = es_.enter_context(nc.sbuf_tensor(f"p6gs_{L}", [128, 2, 514], F32))
    ab = es_.enter_context(nc.sbuf_tensor(f"p6a_{L}", [128, 2, 512], F32))
    cw = es_.enter_context(nc.sbuf_tensor(f"p6cw_{L}", [128, 3, NFC], F32))
    cb = es_.enter_context(nc.sbuf_tensor(f"p6cb_{L}", [128, NFC], F32))
    halo_on = es_.enter_context(nc.sbuf_tensor(f"p6ho_{L}", [128, 1], F32))
    grep = es_.enter_context(nc.sbuf_tensor(f"p6g_{L}", [128, 2, 512], F32))
    xt = es_.enter_context(nc.sbuf_tensor(f"p6x_{L}", [128, 2, 512], F32))
    ot = es_.enter_context(nc.sbuf_tensor(f"p6o_{L}", [128, 2, 512], F32))
    with es_:
        S.dma("sp", [], [("cw",)], lambda q: q.dma_start(out=cw[:], in_=io.convw[l]))
        S.dma("sp", [], [("cb",)], lambda q: q.dma_start(out=cb[:], in_=io.convb[l]))
        hv = h2_d.rearrange("(kc p) t -> p kc t", p=128)
        ei = 0
        for t2 in range(2):
            for k4 in range(8):
                S.dma("sp", [("h2_d",)], [("h2h",)], lambda q: q.dma_start(
                    out=h2h[:, k4 * 4:(k4 + 1) * 4, :], in_=hv[:, k4 * 4:(k4 + 1) * 4, t2 * 512: t2 * 512 + 514]))
            for fc in range(NFC):
                wg = wgb[:, fc % 2, :, :]
                wu = wub[:, fc % 2, :, :]
                gk_, uk_ = ("wgb", fc % 2), ("wub", fc % 2)
                ap, k = _wslice(W, "ffn_wg", l, fc * 128, 128)
                S.dma("pool", [k], [gk_], lambda q: q.dma_start(out=wg, in_=ap.rearrange("(kc p) n -> p kc n", p=128)))
                ap, k = _wslice(W, "ffn_wu", l, fc * 128, 128)
                S.dma("pool", [k], [uk_], lambda q: q.dma_start(out=wu, in_=ap.rearrange("(kc p) n -> p kc n", p=128)))
                psg, pgk = S.psum()
                _mm_group(S, psg[:, :], pgk, [(wg[:, kc, :], h2h[:, kc, 2:514]) for kc in range(32)], [gk_, ("h2h",)])
                psu, puk = S.psum()
                _mm_group(S, psu[:, :], puk, [(wu[:, kc, :], h2h[:, kc, 2:514]) for kc in range(32)], [uk_, ("h2h",)])
                gs = gsb[:, fc % 2, :]
                gsk = ("gsb", fc % 2)
                a_ = ab[:, fc % 2, :]
                ak = ("ab", fc % 2)
                S.op("act", [pgk], [gsk], lambda a: a.copy(out=gs[:, 2:514], in_=psg[:, :]))
                S.op("dve", [("glast",)], [gsk], lambda v: v.tensor_copy(out=gs[:, 0:2], in_=C.glast[:, fc, :]))
                S.op("dve", [gsk], [("glast",)], lambda v: v.tensor_copy(out=C.glast[:, fc, :], in_=gs[:, 512:514]))
                S.op("dve", [gsk, ("cw",), ("cb",)], [ak], lambda v: v.tensor_scalar(
                    out=a_, in0=gs[:, 2:514], scalar1=cw[:, 2, fc:fc + 1], scalar2=cb[:, fc:fc + 1],
                    op0=ALU.mult, op1=ALU.add))
                S.op("dve", [gsk, ak], [ak], lambda v: v.scalar_tensor_tensor(
                    out=a_, in0=gs[:, 1:513], scalar=cw[:, 1, fc:fc + 1], in1=a_, op0=ALU.mult, op1=ALU.add))
                S.op("dve", [gsk, ak], [ak], lambda v: v.scalar_tensor_tensor(
                    out=a_, in0=gs[:, 0:512], scalar=cw[:, 0, fc:fc + 1], in1=a_, op0=ALU.mult, op1=ALU.add))
                S.op("act", [ak], [ak], lambda a: a.activation(out=a_, in_=a_, func=AF.Gelu_apprx_tanh))
                S.op("dve", [ak, puk], [("fT",)], lambda v: v.tensor_tensor(
                    out=fT[:, fc, :], in0=a_, in1=psu[:, :], op=ALU.mult))
            wdi = 0
            for eb in range(8):
                banks = [(S._ps[(eb % 2) * 4 + i], ("ps", (eb % 2) * 4 + i)) for i in range(4)]
                gr = grep[:, eb % 2, :]
                gk = ("grep", eb % 2)
                _rep_piece_fixed(S, C, l, 5, eb, gr, gk, banks)
                for fg in range(22):
                    nf = 4 if fg < 21 else 2
                    wd = wdb[:, wdi % 2, :, :]
                    wk = ("wdb", wdi % 2)
                    wdi += 1
                    ap, k = _wslice(W, "ffn_wd", l, eb * 512, 512)
                    S.dma("pool", [k], [wk], lambda q: q.dma_start(
                        out=wd[:, 0:nf, :], in_=ap[fg * 512: fg * 512 + nf * 128, :].rearrange("(k p) n -> p k n", p=128)))

                    def mm(pe, fg=fg, nf=nf, wd=wd, banks=banks):
                        ins = None
                        for k_ in range(nf):
                            fc = fg * 4 + k_
                            for t4 in range(4):
                                ins = pe.matmul(banks[t4][0][:, :], fT[:, fc, t4 * 128:(t4 + 1) * 128], wd[:, k_, :],
                                                start=(fc == 0), stop=(fc == NFC - 1))
                        return ins
                    S.op("pe", [wk, ("fT",)], [b[1] for b in banks], mm)
                for t4 in range(4):
                    r0 = t2 * 512 + t4 * 128
                    _epilogue_tile(S, C, banks[t4][0], banks[t4][1], gr, gk,
                                   xmid_d[r0:r0 + 128, eb * 512:(eb + 1) * 512],
                                   x_dst[r0:r0 + 128, eb * 512:(eb + 1) * 512], ("x_dst",), xt, ot, ei)
                    ei += 1
        s_barrier(S)


def _rep_piece_fixed(S, C, l, m, eb, dst, dkey, banks):
    ps, pk = banks[0]
    for j in range(4):
        kc = eb * 4 + j
        dg = C.diag[kc % 2]
        dk = ("diag", kc % 2)
        S.op("dve", [("modcols",), ("ident",)], [dk], lambda v: v.tensor_scalar(
            out=dg[:], in0=C.ident[:], scalar1=_mod_ap(C, l, m, kc), scalar2=None, op0=ALU.mult))
        S.op("pe", [dk, ("onesf",)], [pk], lambda pe: pe.matmul(
            ps[:, j * 128:(j + 1) * 128], C.onesf[:], dg[:], start=True, stop=True))
    S.op("act", [pk], [dkey], lambda a: a.copy(out=dst, in_=ps[:, :]))


def _final_norm(S, nc, io, C, x_src):
    with (nc.sbuf_tensor("pfx", [128, 2, 4096], F32) as xb,
          nc.sbuf_tensor("pfj", [128, 4096], BF16) as junk,
          nc.sbuf_tensor("pfg", [128, 4096], F32) as gf):
        S.dma("sp", [], [("gf",)], lambda q: q.dma_start(out=gf[:], in_=io.gfin))
        S.op("dve", [], [("ss", i) for i in range(16)], lambda v: v.memset(C.ss[:], 0.0))
        for tt in range(16):
            xt = xb[:, tt % 2, :]
            xk = ("xb", tt % 2)
            S.dma("sp", [("x_dst",)], [xk], lambda q: q.dma_start(out=xt, in_=x_src[tt * 128:(tt + 1) * 128, :]))
            S.op("act", [xk], [("junk",), ("ss", tt)], lambda a: a.activation(
                out=junk[:], in_=xt, func=AF.Square, accum_out=C.ss[:, tt:tt + 1]))
            _rstd(S, C.ss[:, tt:tt + 1], C.rstd[:, tt:tt + 1], 1.0 / D, ("ss", tt), ("rstd", tt))
            S.op("dve", [xk, ("rstd", tt), ("gf",)], [xk], lambda v: v.scalar_tensor_tensor(
                out=xt, in0=xt, scalar=C.rstd[:, tt:tt + 1], in1=gf[:], op0=ALU.mult, op1=ALU.mult))
            S.dma("sp", [xk], [("out",)], lambda q: q.dma_start(out=io.out[tt * 128:(tt + 1) * 128, :], in_=xt))


NCORES = 4


def build_fused(debug=False, n_layers=2, fake=False, stop_after=None):
    nc = bass.Bass("TRN2", target_bir_lowering=False)
    io = _declare_io(nc, fake, n_layers)
    dbg = {}
    if debug:
        for nm, shp, dt in (("mod", [128, 384], F32), ("p1", [4 * 5120, TPC], BF16), ("p1t", [4 * TPC, 2560], BF16),
                            ("p2", [2 * 2048, S_LEN], BF16),
                            ("p3a", [2 * D, TPC], BF16), ("p3b", [S_LEN, D], F32)):
            dbg[nm] = nc.dram_tensor("dbg_" + nm, shp, dt, kind="ExternalOutput").ap()
    S = Sched(nc)
    _sched_ext(S)
    S.psum_init()
    C = Ctx()
    C.n_layers = n_layers
    C.ident = nc.alloc_sbuf_tensor("ident_sb", [128, 128], F32)
    C.onesf = nc.alloc_sbuf_tensor("onesf", [128, 128], F32)
    C.ones_bf = nc.alloc_sbuf_tensor("ones_bf", [128, 128], BF16)
    C.modcols = nc.alloc_sbuf_tensor("modcols", [128, 2, 192], F32)
    C.A = nc.alloc_sbuf_tensor("A", [128, 32], F32)
    C.B = nc.alloc_sbuf_tensor("B", [128, 32], F32)
    C.gt = nc.alloc_sbuf_tensor("gt", [128, 32], F32)
    C.ss = nc.alloc_sbuf_tensor("ss", [128, 16], F32)
    C.rstd = nc.alloc_sbuf_tensor("rstd", [128, 16], F32)
    C.diag = [nc.alloc_sbuf_tensor(f"diag{i}", [128, 128], F32) for i in range(2)]
    C.glast = nc.alloc_sbuf_tensor("glast", [128, NFC, 2], F32)
    S.dma("sp", [], [("ident",)], lambda q: q.dma_start(out=C.ident[:], in_=io.tabs["ident"]))
    S.dma("sp", [], [("ones_bf",)], lambda q: q.dma_start(out=C.ones_bf[:], in_=io.tabs["ones_bf"]))
    S.op("dve", [], [("onesf",)], lambda v: v.memset(C.onesf[:], 1.0))
    W = {}
    x1_d = nc.dram_tensor("x1_d", [S_LEN, D], F32).ap()
    x2_d = nc.dram_tensor("x2_d", [S_LEN, D], F32).ap()
    C.xmid_d = nc.dram_tensor("xmid_d", [S_LEN, D], F32).ap()
    C.hT_d = nc.dram_tensor("hT_d", [2, D, TPC], BF16).ap()
    C.mT_d = nc.dram_tensor("mT_d", [2, D, TPC], BF16).ap()
    C.zF_d = nc.dram_tensor("zF_d", [2, 2, 5120, TPC], BF16).ap()
    C.zT_d = nc.dram_tensor("zT_d", [2, 2, TPC, 2560], BF16).ap()
    C.y_d = nc.dram_tensor("y_d", [2, 2048, S_LEN], BF16).ap()

    def dump(nm, src):
        if debug:
            S.dma("sp", [], [("dbg", nm)], lambda q: q.dma_start(out=dbg[nm], in_=src))

    def stop():
        S.dma("sp", [], [("out",)], lambda q: q.dma_start(out=io.out[0:128, 0:128], in_=io.x[0:128, 0:128]))
        S.finish("sp")
        return nc
    _mod_phase(S, nc, io, C)
    if stop_after == "mod":
        return stop()
    if debug:
        S.dma("sp", [("modcols",)], [("dbg", "mod")], lambda q: q.dma_start(
            out=dbg["mod"], in_=C.modcols[:].rearrange("p a b -> p (a b)")))
    x_src = io.x
    for l in range(n_layers):
        _weights_phase(S, nc, io, l, W)
        S.op("dve", [("glast",)], [("glast",)], lambda v: v.memset(C.glast[:], 0.0))
        x_dst = x1_d if l == 0 else x2_d
        for th in range(2):
            _phase1(S, nc, io, C, W, l, x_src[th * TPC:(th + 1) * TPC, :], th)
        if stop_after == "p1":
            return stop()
        dump("p1", C.zF_d.rearrange("a b r t -> (a b r) t"))
        dump("p1t", C.zT_d.rearrange("a b t c -> (a b t) c"))
        for hh in range(2):
            _phase2(S, nc, io, C, l, hh)
        if stop_after == "p2":
            return stop()
        dump("p2", C.y_d.rearrange("a r t -> (a r) t"))
        for th in range(2):
            _phase3a(S, nc, io, C, W, l, th)
        if stop_after == "p3a":
            return stop()
        dump("p3a", C.mT_d.rearrange("a r t -> (a r) t"))
        for th in range(2):
            _phase3b(S, nc, io, C, W, l, x_src[th * TPC:(th + 1) * TPC, :], x_dst[th * TPC:(th + 1) * TPC, :], th)
        if stop_after == "p3b":
            return stop()
        dump("p3b", x_dst)
        x_src = x_dst
    _final_norm(S, nc, io, C, x_src)
    S.finish("sp")
    print("instr counts", S.ccnt, flush=True)
    return nc


def _prep_inputs(inp, NL=2):
    ca = np.ascontiguousarray
    x = inp["x"]
    g1T = ca(inp["norm1_g"].reshape(2, 32, 128).transpose(0, 2, 1))
    g2T = ca(inp["norm2_g"].reshape(2, 32, 128).transpose(0, 2, 1))
    gfin = ca(np.broadcast_to(inp["final_g"][None, :], (128, D)))
    gsgu = ca(np.broadcast_to(inp["sgu_norm_g"][:, None, :], (2, 128, 1024)))
    mbT = ca(inp["merge_b"].reshape(2, 4, 32, 128).transpose(0, 3, 1, 2))
    convw = ca(inp["conv_w"].reshape(2, 3, NFC, 128).transpose(0, 3, 1, 2))
    convb = ca(inp["conv_b"].reshape(2, NFC, 128).transpose(0, 2, 1))
    sguwT = ca(np.stack([inp["sgu_w"][:, 4 * hh:4 * hh + 4].transpose(0, 3, 1, 2) for hh in range(2)], axis=1))
    sgub = ca(np.stack([np.broadcast_to(np.tile(inp["sgu_b"][:, 4 * hh:4 * hh + 4, :], (1, 1, 4))[:, None, :, :],
                                        (2, 128, 4, 512)) for hh in range(2)], axis=1))
    poolw = ca(np.stack([inp["pool_w"][:, 2 * hh:2 * hh + 2].reshape(2, 2, 2, 128, 256).transpose(0, 3, 1, 2, 4)
                         for hh in range(2)], axis=1))
    poolsc = ca(np.stack([inp["pool_scale"][:, hh * 512:(hh + 1) * 512].reshape(2, 4, 128).transpose(0, 2, 1)
                          for hh in range(2)], axis=1))
    shared = {"w_ada": ca(inp["w_ada"][:NL]), "b_adaT": ca(inp["b_ada"][:NL].reshape(NL, 192, 128).transpose(0, 2, 1)),
              "g1T": g1T, "g2T": g2T, "gfin": gfin, "gsgu": gsgu, "mbT": mbT, "convw": convw, "convb": convb,
              "sguwT": sguwT, "sgub": sgub, "poolw": poolw, "poolsc": poolsc,
              "w_in": ca(inp["w_in"][:NL]), "merge_w": ca(inp["merge_w"].reshape(2, D, 4 * D)[:NL]),
              "branch_w": ca(inp["branch_w"].reshape(2, D, D)[:NL]), "out_w": ca(inp["out_w"][:NL]),
              "ffn_wg": ca(inp["ffn_wg"][:NL]), "ffn_wu": ca(inp["ffn_wu"][:NL]), "ffn_wd": ca(inp["ffn_wd"][:NL])}
    for k, v in _const_tables_all().items():
        shared["t_" + k] = v
    maps = []
    for b in range(NCORES):
        m = dict(shared)
        m["x"] = ca(x[b])
        m["cT"] = ca(inp["c"][b].reshape(32, 128).T)
        maps.append(m)
    return maps


_NC_CACHE = {}


def kernel(**inputs):
    inp = {k: np.asarray(v) for k, v in inputs.items()}
    if "nc" not in _NC_CACHE:
        _NC_CACHE["nc"] = build_fused()
    maps = _prep_inputs(inp)
    res = run_bass_kernel_spmd(_NC_CACHE["nc"], maps, core_ids=list(range(NCORES)))
    out = np.empty((NB, S_LEN, D), np.float32)
    for b in range(NCORES):
        out[b] = np.asarray(res.results[b]["out"], dtype=np.float32)
    return out
```

```python
import numpy as np
import ml_dtypes
import concourse.bass as bass
import concourse.mybir as mybir
from concourse.bass_utils import run_bass_kernel_spmd

F32 = mybir.dt.float32
BF16 = mybir.dt.bfloat16
ALU = mybir.AluOpType
AF = mybir.ActivationFunctionType
AX = mybir.AxisListType

D = 4096
S = 2048
S_LEN = 2048
NB = 4
TPC = 1024
DFF = 11008
NFC = DFF // 128
EPS = 1e-6
IN_COLS = 15360
BIG = 30000.0


class Sched:
    def __init__(self, nc, n_dma_sems=20, strict=True):
        self.nc = nc
        self.eng = {"pe": nc.tensor, "act": nc.scalar, "dve": nc.vector,
                    "pool": nc.gpsimd, "sp": nc.sync}
        self.csem = {e: nc.alloc_semaphore(name=f"c_{e}") for e in ("pe", "act", "dve", "pool")}
        self.ccnt = {e: 0 for e in self.csem}
        self.dsem = {}
        self.dcnt = {}
        self.dnext = {}
        for q in ("sp", "pool", "act"):
            self.dsem[q] = [nc.alloc_semaphore(name=f"d_{q}{i}") for i in range(n_dma_sems)]
            self.dcnt[q] = [0] * n_dma_sems
            self.dnext[q] = 0
        self.seen = {e: {} for e in self.eng}
        self.lastw = {}
        self.reads = {}
        self.strict = strict
        self._ps = None
        self._psn = 0

    def _sem(self, k):
        if k[0] == "c":
            return self.csem[k[1]]
        return self.dsem[k[1]][k[2]]

    def _wait(self, e, k, v):
        if self.seen[e].get(k, 0) >= v:
            return
        self.eng[e].wait_ge(self._sem(k), v)
        self.seen[e][k] = v

    def _deps(self, e, reads, writes):
        deps = {}

        def add(t):
            if t is None:
                return
            k, v = t
            if deps.get(k, 0) < v:
                deps[k] = v
        for k in reads:
            add(self.lastw.get(k))
        for k in writes:
            add(self.lastw.get(k))
            for kk, vv in self.reads.get(k, {}).items():
                add((kk, vv))
        for k, v in deps.items():
            if k[0] == "c" and k[1] == e and (e == "pe" or not self.strict):
                continue
            self._wait(e, k, v)

    def _commit(self, ticket, reads, writes):
        for k in writes:
            self.lastw[k] = ticket
            self.reads[k] = {}
        for k in reads:
            r = self.reads.setdefault(k, {})
            if r.get(ticket[0], 0) < ticket[1]:
                r[ticket[0]] = ticket[1]

    def op(self, e, reads, writes, fn):
        self._deps(e, reads, writes)
        ins = fn(self.eng[e])
        self.ccnt[e] += 1
        ins.then_inc(self.csem[e], 1)
        t = (("c", e), self.ccnt[e])
        self._commit(t, reads, writes)
        return t

    def dma(self, q, reads, writes, fn):
        i = self.dnext[q]
        self.dnext[q] = (i + 1) % len(self.dsem[q])
        k = ("d", q, i)
        if self.dcnt[q][i] > 0:
            self._wait(q, k, self.dcnt[q][i])
        self._deps(q, reads, writes)
        insl = fn(self.eng[q])
        if not isinstance(insl, (list, tuple)):
            insl = [insl]
        for ins in insl:
            ins.then_inc(self.dsem[q][i], 16)
            self.dcnt[q][i] += 16
        t = (k, self.dcnt[q][i])
        self._commit(t, reads, writes)
        return t

    def finish(self, e="sp"):
        for q in self.dsem:
            for i, c in enumerate(self.dcnt[q]):
                if c > 0:
                    self._wait(e, ("d", q, i), c)

    def psum_init(self):
        self._ps = [self.nc.alloc_psum_tensor(f"psb{i}", [128, 512], F32) for i in range(8)]

    def psum(self):
        i = self._psn % 8
        self._psn += 1
        return self._ps[i], ("ps", i)


def _mm_group(S, ps_ap, pskey, pairs, reads, extra_writes=()):
    n = len(pairs)

    def fn(pe):
        ins = None
        for i, (l, r) in enumerate(pairs):
            ins = pe.matmul(ps_ap, l, r, start=(i == 0), stop=(i == n - 1))
        return ins
    return S.op("pe", reads, [pskey] + list(extra_writes), fn)


def _rstd(S, ss_ap, rstd_ap, inv_n, sskey, rkey):
    S.op("dve", [sskey], [rkey],
         lambda v: v.tensor_scalar(out=rstd_ap, in0=ss_ap, scalar1=inv_n, scalar2=EPS,
                                   op0=ALU.mult, op1=ALU.add))
    S.op("act", [rkey], [rkey], lambda a: a.activation(out=rstd_ap, in_=rstd_ap, func=AF.Sqrt))
    S.op("dve", [rkey], [rkey], lambda v: v.reciprocal(out=rstd_ap, in_=rstd_ap))


def _norm_to_hT(S, nc, x_ap, n_tok, xbufs, junk, ss, rstd, ident, A, B, hT, hkey, col0):
    ntile = (n_tok + 127) // 128
    for tt in range(ntile):
        p = min(128, n_tok - tt * 128)
        xt = xbufs[tt % 2]
        xk = ("xbuf", id(xt))
        S.dma("sp", [], [xk], lambda q: q.dma_start(out=xt[:p, :], in_=x_ap[tt * 128: tt * 128 + p, :]))
        S.op("act", [xk], [("junk",), ("ss", tt)],
             lambda a: a.activation(out=junk[:p, :], in_=xt[:p, :], func=AF.Square,
                                    accum_out=ss[:p, tt:tt + 1]))
        _rstd(S, ss[:p, tt:tt + 1], rstd[:p, tt:tt + 1], 1.0 / D, ("ss", tt), ("rstd", tt))
        S.op("dve", [("rstd", tt), xk], [xk],
             lambda v: v.tensor_scalar(out=xt[:p, :], in0=xt[:p, :], scalar1=rstd[:p, tt:tt + 1],
                                       scalar2=None, op0=ALU.mult))
        for g in range(8):
            ps, pk = S.psum()

            def tr(pe, g=g, ps=ps):
                ins = None
                for j in range(4):
                    kc = g * 4 + j
                    ins = pe.transpose(out=ps[:, j * 128: j * 128 + p],
                                       in_=xt[:p, kc * 128:(kc + 1) * 128], identity=ident[:p, :p])
                return ins
            S.op("pe", [xk, ("ident",)], [pk], tr)
            for j in range(4):
                kc = g * 4 + j
                S.op("act", [pk, ("AB",)], [hkey],
                     lambda a, kc=kc, j=j, ps=ps: a.activation(
                         out=hT[:, kc, col0 + tt * 128: col0 + tt * 128 + p],
                         in_=ps[:, j * 128: j * 128 + p], func=AF.Identity,
                         bias=B[:, kc:kc + 1], scale=A[:, kc:kc + 1]))


def _load_AB(S, nc, modT_d, gT_d, i_shift, i_scale, A, B, gt, modt):
    S.dma("sp", [], [("modt",)], lambda q: q.dma_start(out=modt[:], in_=modT_d))
    S.dma("sp", [], [("gt",)], lambda q: q.dma_start(out=gt[:], in_=gT_d))
    S.op("dve", [("modt",), ("gt",)], [("AB",)],
         lambda v: v.scalar_tensor_tensor(out=A[:], in0=modt[:, i_scale, :], scalar=1.0, in1=gt[:],
                                          op0=ALU.add, op1=ALU.mult))
    S.op("dve", [("modt",)], [("AB",)],
         lambda v: v.tensor_copy(out=B[:], in_=modt[:, i_shift, :]))


def _gelu(S, out_ap, in_ap, tmp_ap, reads, writes, tmpkey):
    S.op("dve", reads, [tmpkey],
         lambda v: v.tensor_tensor(out=tmp_ap, in0=in_ap, in1=in_ap, op=ALU.mult))
    S.op("dve", [tmpkey], [tmpkey],
         lambda v: v.tensor_scalar(out=tmp_ap, in0=tmp_ap, scalar1=0.044715, scalar2=1.0,
                                   op0=ALU.mult, op1=ALU.add))
    S.op("dve", [tmpkey] + list(reads), [tmpkey],
         lambda v: v.tensor_tensor(out=tmp_ap, in0=tmp_ap, in1=in_ap, op=ALU.mult))
    S.op("act", [tmpkey], [tmpkey],
         lambda a: a.activation(out=tmp_ap, in_=tmp_ap, func=AF.Sigmoid, scale=1.5957691216057308))
    S.op("dve", [tmpkey] + list(reads), writes,
         lambda v: v.tensor_tensor(out=out_ap, in0=tmp_ap, in1=in_ap, op=ALU.mult))


def _sched_ext(S, n_coll_sems=8):
    S.csem_coll = [S.nc.alloc_semaphore(name=f"cc{i}") for i in range(n_coll_sems)]
    S.ccnt_coll = [0] * n_coll_sems
    S.cnext = 0
    S.dsem["coll"] = S.csem_coll
    S.dcnt["coll"] = S.ccnt_coll


def s_coll(S, reads, writes, fn):
    i = S.cnext
    S.cnext = (i + 1) % len(S.csem_coll)
    k = ("d", "coll", i)
    if S.ccnt_coll[i] > 0:
        S._wait("pool", k, S.ccnt_coll[i])
    S._deps("pool", reads, writes)
    ins = fn(S.eng["pool"])
    ins.then_inc(S.csem_coll[i], 1)
    S.ccnt_coll[i] += 1
    t = (k, S.ccnt_coll[i])
    S._commit(t, reads, writes)
    return t


def s_barrier(S):
    for e in ("pe", "act", "dve", "pool", "sp"):
        for o in ("pe", "act", "dve", "pool"):
            if o != e and S.ccnt[o] > 0:
                S._wait(e, ("c", o), S.ccnt[o])
        for q in S.dsem:
            for i, c in enumerate(S.dcnt[q]):
                if c > 0:
                    S._wait(e, ("d", q, i), c)


PAIRS = [[0, 1], [2, 3], [4, 5], [6, 7]]
ALL8 = [list(range(8))]
SCALE = 128.0 ** -0.5
BIGP = 1.0e6
SLOPES = 2.0 ** (-8.0 * np.arange(1, 33) / 32.0)
DILS = (1, 4, 16)
POOL_W = (2, 4, 8, 16)
NOFF = 20


def _ag(S, in_ap, out_ap, groups, reads, writes):
    return s_coll(S, reads, writes, lambda g: g.collective_compute(
        "AllGather", ALU.bypass, replica_groups=groups, ins=[in_ap], outs=[out_ap]))


def _const_tables(core):
    hh = core % 2
    t = {}
    t["ident"] = np.eye(128, dtype=np.float32)
    k = np.arange(128)[:, None]
    q = np.arange(256)[None, :]
    d0 = np.empty((128, 256), np.float32)
    cur = (q[:, :128] - k).astype(np.float32)
    d0[:, :128] = np.where(cur >= 0, cur, BIGP)
    prv = (q[:, :128] - k + 128).astype(np.float32)
    d0[:, 128:] = np.where(prv <= 128, prv, BIGP)
    t["d0dil"] = d0
    t["d0dil4"] = np.ascontiguousarray(np.tile(d0[:, :128], (1, 4)))
    q5 = np.arange(512)[None, :]
    dm = np.empty((5, 128, 512), np.float32)
    dm[0] = q5 - k
    for j in range(4):
        v = (q5 - k).astype(np.float32)
        dm[1 + j] = np.where(v >= 128 * j, v, BIGP)
    t["d0moba"] = dm
    st = np.zeros((128, 16), np.float32)
    for gi in range(3):
        for h4 in range(4):
            st[:, gi * 4 + h4] = -SLOPES[gi + 4 * (4 * hh + h4)] * DILS[gi] / SCALE
    mb = np.zeros((128, 4 * NOFF), np.float32)
    for h4 in range(4):
        sl = SLOPES[3 + 4 * (4 * hh + h4)]
        st[:, 12 + h4] = -sl / SCALE
        for oi in range(NOFF):
            mb[:, h4 * NOFF + oi] = -sl * 128.0 * (oi - 3)
    t["slopetab"] = st
    t["mobabias"] = mb
    vb = np.zeros((128, 16, 8), np.float32)
    v01 = np.zeros((128, 16, 8), np.float32)
    for tt in range(16):
        for n in range(8):
            ok = n < tt // 2
            vb[:, tt, n] = 0.0 if ok else -1.0e30
            v01[:, tt, n] = 1.0 if ok else 0.0
    t["validbias"] = vb.reshape(128, 128)
    t["valid01"] = v01.reshape(128, 128)
    selr = np.zeros((8, 8, 128), np.float32)
    for n in range(8):
        selr[n, n, :] = 1.0
    t["selrows"] = np.ascontiguousarray(selr.transpose(1, 0, 2)).astype(ml_dtypes.bfloat16)
    pm = np.zeros((2, 3, 128, 128), np.float32)
    s_ = np.arange(128)[:, None]
    t_ = np.arange(128)[None, :]
    for pg2 in range(2):
        w = POOL_W[2 * hh + pg2]
        band = ((s_ <= t_) & (s_ > t_ - w)).astype(np.float32)
        pm[pg2, 0] = band / w - (s_ == t_)
        cnt = np.minimum(t_ + 1, w).astype(np.float32)
        pm[pg2, 1] = band / cnt - (s_ == t_)
        pm[pg2, 2] = ((s_ - 128 > t_ - w)).astype(np.float32) / w
    t["poolP"] = np.ascontiguousarray(pm.transpose(2, 0, 1, 3)).astype(ml_dtypes.bfloat16)
    t["tri"] = (s_ <= t_).astype(np.float32)
    t["halo_on"] = np.full((128, 1), float(hh), np.float32)
    t["ones_bf"] = np.ones((128, 128), ml_dtypes.bfloat16)
    return t


HH_TABS = ("slopetab", "mobabias", "poolP")


def _const_tables_all():
    t0 = _const_tables(0)
    t1 = _const_tables(1)
    out = {}
    for k in t0:
        if k in HH_TABS:
            out[k] = np.ascontiguousarray(np.stack([t0[k], t1[k]]))
        elif k != "halo_on":
            out[k] = t0[k]
    return out


W_PIECES = {
    "w_in": (512, 15360, 3072),
    "merge_w": (512, 16384, 4096),
    "branch_w": (512, 4096, 4096),
    "out_w": (512, 4096, 4096),
    "ffn_wg": (512, DFF, 5504),
    "ffn_wu": (512, DFF, 5504),
    "ffn_wd": (1376, 4096, 2048),
}


class Ctx:
    pass


def _declare_io(nc, fake=False, NL=2):
    io = Ctx()
    BIGW = ("w_ada",) + tuple(W_PIECES)
    def inp(name, shape, dt=F32):
        if fake and name in BIGW:
            return nc.dram_tensor(name, list(shape), dt).ap()
        return nc.dram_tensor(name, list(shape), dt, kind="ExternalInput").ap()
    io.x = inp("x", [S_LEN, D])
    io.cT = inp("cT", [128, 32])
    io.w_ada = inp("w_ada", [NL, D, 6 * D])
    io.b_adaT = inp("b_adaT", [NL, 128, 192])
    io.g1T = inp("g1T", [2, 128, 32])
    io.g2T = inp("g2T", [2, 128, 32])
    io.gfin = inp("gfin", [128, D])
    io.gsgu = inp("gsgu", [2, 128, 1024])
    io.sguwT = inp("sguwT", [2, 2, 128, 4, 128])
    io.sgub = inp("sgub", [2, 2, 128, 4, 512])
    io.poolw = inp("poolw", [2, 2, 128, 2, 2, 256])
    io.poolsc = inp("poolsc", [2, 2, 128, 4])
    io.mbT = inp("mbT", [2, 128, 4, 32])
    io.convw = inp("convw", [2, 128, 3, NFC])
    io.convb = inp("convb", [2, 128, NFC])
    for n, (r, c, pw) in W_PIECES.items():
        setattr(io, n, inp(n, [NL, 8 * r, c]))
    tabs = _const_tables_all()
    io.tabs = {}
    for k, v in tabs.items():
        io.tabs[k] = inp("t_" + k, v.shape, BF16 if v.dtype == ml_dtypes.bfloat16 else F32)
    io.out = nc.dram_tensor("out", [S_LEN, D], F32, kind="ExternalOutput").ap()
    return io


def _weights_phase(S, nc, io, l, W):
    for n, (r, c, pw) in W_PIECES.items():
        W[(n, l)] = [(getattr(io, n)[l], ("wext",), c)]


def _wslice(W, n, l, c0, ncols):
    pw = W[(n, l)][0][2]
    j = c0 // pw
    assert (c0 + ncols - 1) // pw == j
    ga, k, _ = W[(n, l)][j]
    return ga[:, c0 - j * pw: c0 - j * pw + ncols], k


def _mod_phase(S, nc, io, C):
    from contextlib import ExitStack
    es = ExitStack()
    cT = es.enter_context(nc.sbuf_tensor("cT_sb", [128, 32], F32))
    wa = [es.enter_context(nc.sbuf_tensor(f"wa{i}", [128, 8, 512], F32)) for i in range(3)]
    bcol = es.enter_context(nc.sbuf_tensor("bada_col", [128, 192], F32))
    msb = es.enter_context(nc.sbuf_tensor("mod_sb", [1, 2, 512], F32))
    one = es.enter_context(nc.sbuf_tensor("one_sb", [1, 1], F32))
    S.dma("sp", [], [("cT",)], lambda q: q.dma_start(out=cT[:], in_=io.cT))
    S.op("dve", [], [("one",)], lambda v: v.memset(one[:], 1.0))
    wv = io.w_ada.rearrange("l (kc p) n -> l p kc n", p=128)
    di = 0
    pcol, pck = S._ps[7], ("ps", 7)
    rot = 0
    for l in range(C.n_layers):
        S.dma("sp", [], [("bcol",)], lambda q: q.dma_start(out=bcol[:], in_=io.b_adaT[l]))
        for cb in range(48):
            ps, pk = S._ps[rot % 6], ("ps", rot % 6)
            rot += 1
            for kg in range(4):
                t = wa[di % 3]
                wk = ("wa", di % 3)
                di += 1
                S.dma("sp", [], [wk], lambda q: q.dma_start(
                    out=t[:], in_=wv[l, :, kg * 8:(kg + 1) * 8, cb * 512:(cb + 1) * 512]))

                def mm(pe, t=t, kg=kg, ps=ps):
                    ins = None
                    for k8 in range(8):
                        kc = kg * 8 + k8
                        ins = pe.matmul(ps[:1, :], cT[:, kc:kc + 1], t[:, k8, :],
                                        start=(kc == 0), stop=(kc == 31))
                    return ins
                S.op("pe", [wk, ("cT",)], [pk], mm)
            mk = ("msb", cb % 2)
            S.op("act", [pk], [mk], lambda a: a.copy(out=msb[:, cb % 2, :], in_=ps[:1, :]))

            def tr(pe, cb=cb):
                ins = None
                for j4 in range(4):
                    gc = cb * 4 + j4
                    ins = pe.matmul(pcol[:, gc:gc + 1], msb[0:1, cb % 2, j4 * 128:(j4 + 1) * 128], one[0:1, 0:1],
                                    start=True, stop=True)
                return ins
            S.op("pe", [mk, ("one",)], [pck], tr)
        S.op("dve", [pck, ("bcol",)], [("modcols",)], lambda v: v.tensor_tensor(
            out=C.modcols[:, l, :], in0=pcol[:, 0:192], in1=bcol[:], op=ALU.add))
    s_barrier(S)
    es.close()


def _mod_ap(C, l, m, kc):
    return C.modcols[:, l, m * 32 + kc: m * 32 + kc + 1]


def _make_AB(S, C, l, m_shift, m_scale, gT_d, A, B, gt):
    S.dma("sp", [("AB",)], [("gt",)], lambda q: q.dma_start(out=gt[:], in_=gT_d))
    S.op("dve", [("modcols",), ("gt",)], [("AB",)], lambda v: v.scalar_tensor_tensor(
        out=A[:], in0=C.modcols[:, l, m_scale * 32:(m_scale + 1) * 32], scalar=1.0, in1=gt[:],
        op0=ALU.add, op1=ALU.mult))
    S.op("dve", [("modcols",)], [("AB",)], lambda v: v.tensor_copy(
        out=B[:], in_=C.modcols[:, l, m_shift * 32:(m_shift + 1) * 32]))


def _make_rep(S, C, l, m, Grep, diag, key):
    for g in range(8):
        ps, pk = S.psum()
        for j in range(4):
            kc = g * 4 + j
            dg = diag[(kc) % 2]
            dk = ("diag", kc % 2)
            S.op("dve", [("modcols",), ("ident",)], [dk], lambda v, dg=dg, kc=kc: v.tensor_scalar(
                out=dg[:], in0=C.ident[:], scalar1=_mod_ap(C, l, m, kc), scalar2=None, op0=ALU.mult))
            S.op("pe", [dk, ("onesf",)], [pk], lambda pe, dg=dg, j=j, ps=ps: pe.matmul(
                ps[:, j * 128:(j + 1) * 128], C.onesf[:], dg[:], start=True, stop=True))
        S.op("act", [pk], [key], lambda a, g=g, ps=ps: a.copy(out=Grep[:, g * 512:(g + 1) * 512], in_=ps[:, :]))


P1_FB = [(0, 0, 0, True), (512, 1, 0, True), (2048, 0, 512, False), (2560, 1, 512, False)]
for _gi in range(3):
    for _hh in range(2):
        P1_FB.append((3072 + _gi * 1024 + _hh * 512, _hh, 1024 + _gi * 512, False))
        P1_FB.append((6144 + _gi * 1024 + _hh * 512, _hh, 2560 + _gi * 512, False))
for _hh in range(2):
    P1_FB.append((12288 + _hh * 512, _hh, 4096, False))
    P1_FB.append((13312 + _hh * 512, _hh, 4608, False))
P1_TB = [(1024, 0, 0, True), (1536, 1, 0, True)]
for _gi in range(3):
    for _hh in range(2):
        P1_TB.append((9216 + _gi * 1024 + _hh * 512, _hh, 512 + _gi * 512, False))
for _hh in range(2):
    P1_TB.append((14336 + _hh * 512, _hh, 2048, False))


def _phase1(S, nc, io, C, W, l, x_src, th):
    hT_d = C.hT_d[th]
    L = f"{l}_{th}"
    from contextlib import ExitStack
    es_ = ExitStack()
    hT = es_.enter_context(nc.sbuf_tensor(f"hT_{L}", [128, 32, TPC], BF16))
    wb0 = es_.enter_context(nc.sbuf_tensor(f"wb0_{L}", [128, 32, 512], BF16))
    wb1 = es_.enter_context(nc.sbuf_tensor(f"wb1_{L}", [128, 32, 512], BF16))
    big = es_.enter_context(nc.sbuf_tensor(f"big_{L}", [128, 8192], F32))
    junk = es_.enter_context(nc.sbuf_tensor(f"junk_{L}", [128, 4096], BF16))
    gsgu = es_.enter_context(nc.sbuf_tensor(f"gsgu_{L}", [128, 1024], F32))
    stF_ = es_.enter_context(nc.sbuf_tensor(f"stF_{L}", [128, 4, 512], BF16))
    gin_ = es_.enter_context(nc.sbuf_tensor(f"gin_{L}", [128, 2, 512], F32))
    gtmp_ = es_.enter_context(nc.sbuf_tensor(f"gtmp_{L}", [128, 2, 512], F32))
    with es_:
        wb = [wb0, wb1]
        xbufs = [big[:, 0:4096], big[:, 4096:8192]]
        S.dma("sp", [], [("gsgu",)], lambda q: q.dma_start(out=gsgu[:], in_=io.gsgu[l]))
        S.op("dve", [], [("ss", i) for i in range(16)], lambda v: v.memset(C.ss[:], 0.0))
        _make_AB(S, C, l, 0, 1, io.g1T[l], C.A, C.B, C.gt)
        blocks = [("F",) + b for b in P1_FB] + [("T",) + b for b in P1_TB]

        def load_w(bi):
            c0 = blocks[bi][1]
            t = wb[bi % 2]
            ap, k = _wslice(W, "w_in", l, c0, 512)
            S.dma("pool", [k], [("wb", bi % 2)], lambda q: q.dma_start(
                out=t[:], in_=ap.rearrange("(kc p) n -> p kc n", p=128)))
        load_w(0)
        load_w(1)
        _norm_to_hT(S, nc, x_src, TPC, xbufs, junk, C.ss, C.rstd, C.ident, C.A, C.B, hT, ("hT",), 0)
        hTdv = hT_d.rearrange("(kc p) t -> p kc t", p=128)
        for k4 in range(8):
            S.dma("sp", [("hT",)], [("hT_d",)], lambda q: q.dma_start(
                out=hTdv[:, k4 * 4:(k4 + 1) * 4, :], in_=hT[:, k4 * 4:(k4 + 1) * 4, :]))
        vg = big
        evi = 0
        for bi, (kind, c0, hh, o0, is_gelu) in enumerate(blocks):
            t = wb[bi % 2]
            wk = ("wb", bi % 2)
            if kind == "F":
                for j in range(4):
                    for t5 in range(2):
                        ps, pk = S.psum()
                        _mm_group(S, ps[:, :], pk,
                                  [(t[:, kc, j * 128:(j + 1) * 128], hT[:, kc, t5 * 512:(t5 + 1) * 512])
                                   for kc in range(32)], [wk, ("hT",)])
                        st = stF_[:, evi % 4, :]
                        sk = ("stF", evi % 4)
                        if is_gelu:
                            S.op("act", [pk], [sk], lambda a: a.activation(out=st, in_=ps[:, :], func=AF.Gelu_apprx_tanh))
                        elif evi % 2 == 0:
                            S.op("act", [pk], [sk], lambda a: a.copy(out=st, in_=ps[:, :]))
                        else:
                            S.op("dve", [pk], [sk], lambda v: v.tensor_copy(out=st, in_=ps[:, :]))
                        r0 = o0 + j * 128
                        S.dma("sp", [sk], [("zF_d",)], lambda q: q.dma_start(
                            out=C.zF_d[hh, th, r0:r0 + 128, t5 * 512:(t5 + 1) * 512], in_=st))
                        evi += 1
            else:
                for tt in range(8):
                    ps, pk = S.psum()
                    _mm_group(S, ps[:, :], pk,
                              [(hT[:, kc, tt * 128:(tt + 1) * 128], t[:, kc, :]) for kc in range(32)],
                              [wk, ("hT",)])
                    if is_gelu:
                        half = hh
                        vdst = vg[:, tt * 1024 + half * 512: tt * 1024 + half * 512 + 512]
                        S.op("act", [pk], [("vg", tt)], lambda a: a.activation(out=vdst, in_=ps[:, :], func=AF.Gelu_apprx_tanh))
                        if half == 1:
                            vt = vg[:, tt * 1024:(tt + 1) * 1024]
                            S.op("act", [("vg", tt)], [("junk",), ("ss", 8 + tt)],
                                 lambda a: a.activation(out=junk[:, 0:1024], in_=vt, func=AF.Square,
                                                        accum_out=C.ss[:, 8 + tt:9 + tt]))
                            _rstd(S, C.ss[:, 8 + tt:9 + tt], C.rstd[:, 8 + tt:9 + tt], 1.0 / 1024,
                                  ("ss", 8 + tt), ("rstd", 8 + tt))
                            for h2 in range(2):
                                st = stF_[:, evi % 4, :]
                                sk = ("stF", evi % 4)
                                S.op("dve", [("vg", tt), ("rstd", 8 + tt), ("gsgu",)], [sk],
                                     lambda v: v.scalar_tensor_tensor(
                                         out=st, in0=vt[:, h2 * 512:(h2 + 1) * 512],
                                         scalar=C.rstd[:, 8 + tt:9 + tt], in1=gsgu[:, h2 * 512:(h2 + 1) * 512],
                                         op0=ALU.mult, op1=ALU.mult))
                                S.dma("sp", [sk], [("zT_d",)], lambda q: q.dma_start(
                                    out=C.zT_d[h2, th, tt * 128:(tt + 1) * 128, 0:512], in_=st))
                                evi += 1
                    else:
                        st = stF_[:, evi % 4, :]
                        sk = ("stF", evi % 4)
                        if evi % 2 == 0:
                            S.op("act", [pk], [sk], lambda a: a.copy(out=st, in_=ps[:, :]))
                        else:
                            S.op("dve", [pk], [sk], lambda v: v.tensor_copy(out=st, in_=ps[:, :]))
                        S.dma("sp", [sk], [("zT_d",)], lambda q: q.dma_start(
                            out=C.zT_d[hh, th, tt * 128:(tt + 1) * 128, o0:o0 + 512], in_=st))
                        evi += 1
            if bi + 2 < len(blocks):
                load_w(bi + 2)
        s_barrier(S)


def _phase2(S, nc, io, C, l, hh):
    yT_d = C.y_d[hh]
    L = f"{l}_{hh}"
    zF_my = C.zF_d[hh]
    zT_my = C.zT_d[hh].rearrange("hf t c -> (hf t) c")
    zT5 = zT_my.rearrange("(hf n p) c -> hf p n c", hf=2, n=8, p=128)
    zT4 = zT_my.rearrange("(hf n2 j r) c -> hf r j n2 c", hf=2, n2=2, j=128, r=4)
    zT16 = zT_my.rearrange("(hf j2 r) c -> hf j2 r c", hf=2, j2=64, r=16)
    T = io.tabs
    srot = [0]

    def s_tile():
        i = srot[0] % 4
        srot[0] += 1
        return S._ps[i], ("ps", i)

    def ldF(dst, r0, key):
        for hf in range(2):
            S.dma("sp", [("zFg",)], [key], lambda q, hf=hf: q.dma_start(
                out=dst[:, hf * 1024:(hf + 1) * 1024], in_=zF_my[hf, r0:r0 + 128, :]))

    def ldV1(dst, c0, key):
        for hf in range(2):
            S.dma("sp", [("zTg",)], [key], lambda q, hf=hf: q.dma_start(
                out=dst[:, hf * 8:(hf + 1) * 8, :], in_=zT5[hf, :, :, c0:c0 + 128]))

    from contextlib import ExitStack
    es_ = ExitStack()
    fa = es_.enter_context(nc.sbuf_tensor(f"p2a_{L}", [128, 4, 2048], BF16))
    fb = es_.enter_context(nc.sbuf_tensor(f"p2b_{L}", [128, 4, 2048], BF16))
    vn = es_.enter_context(nc.sbuf_tensor(f"p2v_{L}", [128, 16, 512], BF16))
    wtmp = es_.enter_context(nc.sbuf_tensor(f"p2w_{L}", [128, 4, 128], F32))
    WcT = es_.enter_context(nc.sbuf_tensor(f"p2wc_{L}", [128, 4, 128], BF16))
    bsrep = es_.enter_context(nc.sbuf_tensor(f"p2bs_{L}", [128, 4, 512], F32))
    tmpf = es_.enter_context(nc.sbuf_tensor(f"p2t_{L}", [128, 2, 512], F32))
    Pbf = es_.enter_context(nc.sbuf_tensor(f"p2p_{L}", [128, 2, 512], BF16))
    wp = es_.enter_context(nc.sbuf_tensor(f"p2wp_{L}", [128, 2, 2, 256], BF16))
    poolP = es_.enter_context(nc.sbuf_tensor(f"p2pp_{L}", [128, 2, 3, 128], BF16))
    zW = es_.enter_context(nc.sbuf_tensor(f"p2zw_{L}", [128, 17, 2, 256], BF16))
    poolsc = es_.enter_context(nc.sbuf_tensor(f"p2ps_{L}", [128, 4], F32))
    acc = es_.enter_context(nc.sbuf_tensor(f"p2acc_{L}", [128, 2, 2048], F32))
    Vt = es_.enter_context(nc.sbuf_tensor(f"p2V_{L}", [128, 3, 16, 128], BF16))
    d0dil = es_.enter_context(nc.sbuf_tensor(f"p2d0_{L}", [128, 256], F32))
    d0dil4 = es_.enter_context(nc.sbuf_tensor(f"p2d4_{L}", [128, 512], F32))
    d0moba = es_.enter_context(nc.sbuf_tensor(f"p2dm_{L}", [128, 5, 512], F32))
    slopetab = es_.enter_context(nc.sbuf_tensor(f"p2st_{L}", [128, 16], F32))
    mobabias = es_.enter_context(nc.sbuf_tensor(f"p2mb_{L}", [128, 4 * NOFF], F32))
    validbias = es_.enter_context(nc.sbuf_tensor(f"p2vb_{L}", [128, 128], F32))
    valid01 = es_.enter_context(nc.sbuf_tensor(f"p2v1_{L}", [128, 128], F32))
    selrows = es_.enter_context(nc.sbuf_tensor(f"p2sr_{L}", [8, 8, 128], BF16))
    selT = es_.enter_context(nc.sbuf_tensor(f"p2sT_{L}", [8, 2048], BF16))
    gsb = es_.enter_context(nc.sbuf_tensor(f"p2g_{L}", [128, 4, 128], F32))
    kmf = es_.enter_context(nc.sbuf_tensor(f"p2km_{L}", [128, 4, 8], F32))
    kmhl = es_.enter_context(nc.sbuf_tensor(f"p2kh_{L}", [128, 2, 8], BF16))
    tri = es_.enter_context(nc.sbuf_tensor(f"p2tri_{L}", [128, 128], F32))
    with es_:
        for dst, nm in ((d0dil, "d0dil"), (d0dil4, "d0dil4"), (slopetab, "slopetab"), (mobabias, "mobabias"),
                        (validbias, "validbias"), (valid01, "valid01"), (selrows, "selrows"),
                        (poolP, "poolP"), (tri, "tri")):
            S.dma("sp", [], [("c", nm)], lambda q, dst=dst, nm=nm: q.dma_start(out=dst[:], in_=(T[nm][hh] if nm in HH_TABS else T[nm])))
        S.dma("sp", [], [("c", "d0moba")], lambda q: q.dma_start(
            out=d0moba[:], in_=T["d0moba"].rearrange("v p q -> p v q")))
        S.dma("sp", [], [("wtmp",)], lambda q: q.dma_start(out=wtmp[:], in_=io.sguwT[l, hh]))
        S.dma("sp", [], [("bsrep",)], lambda q: q.dma_start(out=bsrep[:], in_=io.sgub[l, hh]))
        S.dma("pool", [], [("wp",)], lambda q: q.dma_start(out=wp[:], in_=io.poolw[l, hh]))
        S.dma("sp", [], [("poolsc",)], lambda q: q.dma_start(out=poolsc[:], in_=io.poolsc[l, hh]))
        for g4 in range(4):
            S.op("dve", [("wtmp",), ("c", "tri")], [("WcT",)], lambda v, g4=g4: v.tensor_tensor(
                out=WcT[:, g4, :], in0=wtmp[:, g4, :], in1=tri[:], op=ALU.mult))

        for g4 in range(4):
            ldF(fa[:, g4, :], g4 * 128, ("fa", g4))
        for hf in range(2):
            S.dma("sp", [("zTg",)], [("vn",)], lambda q, hf=hf: q.dma_start(
                out=vn[:, hf * 8:(hf + 1) * 8, :], in_=zT5[hf, :, :, 0:512]))
        for g4 in range(4):
            for tg in range(4):
                ps, pk = S.psum()

                def mm(pe, g4=g4, tg=tg, ps=ps):
                    ins = None
                    for i in range(4):
                        ins = pe.matmul(ps[:, i * 128:(i + 1) * 128], vn[:, tg * 4 + i, g4 * 128:(g4 + 1) * 128],
                                        WcT[:, g4, :], start=True, stop=True)
                    return ins
                S.op("pe", [("vn",), ("WcT",)], [pk], mm)
                tk = ("tmpf", tg % 2)
                S.op("dve", [pk, ("bsrep",)], [tk], lambda v, g4=g4, tg=tg, ps=ps: v.tensor_tensor(
                    out=tmpf[:, tg % 2, :], in0=ps[:, :], in1=bsrep[:, g4, :], op=ALU.add))
                S.op("dve", [tk, ("fa", g4)], [("fb", g4)], lambda v, g4=g4, tg=tg: v.tensor_tensor(
                    out=fb[:, g4, tg * 512:(tg + 1) * 512], in0=tmpf[:, tg % 2, :],
                    in1=fa[:, g4, tg * 512:(tg + 1) * 512], op=ALU.mult))
            S.dma("sp", [("fb", g4)], [("yT_d",)], lambda q, g4=g4: q.dma_start(
                out=yT_d[g4 * 128:(g4 + 1) * 128, :], in_=fb[:, g4, :]))

        for i in range(4):
            ldF(fa[:, i, :], 512 + i * 128, ("fa", i))
        S.op("dve", [], [("zW", 0)], lambda v: v.memset(zW[:, 0, :, :], 0.0))
        for pg2 in range(2):
            for t2 in range(8):
                ps, pk = S.psum()

                def mm(pe, pg2=pg2, t2=t2, ps=ps):
                    ins = None
                    for i in range(2):
                        tt = t2 * 2 + i
                        for cc in range(2):
                            ins = pe.matmul(ps[:, i * 256:(i + 1) * 256],
                                            fa[:, pg2 * 2 + cc, tt * 128:(tt + 1) * 128], wp[:, pg2, cc, :],
                                            start=(cc == 0), stop=(cc == 1))
                    return ins
                S.op("pe", [("fa", pg2 * 2), ("fa", pg2 * 2 + 1), ("wp",)], [pk], mm)
                S.op("act", [pk], [("zW", 1)], lambda a, pg2=pg2, t2=t2, ps=ps: a.copy(
                    out=zW[:, 1 + t2 * 2: 3 + t2 * 2, pg2, :], in_=ps[:, :].rearrange("p (i e) -> p i e", i=2)))
        for pg2 in range(2):
            for ec in range(2):
                oc = pg2 * 2 + ec
                for tg in range(4):
                    ps, pk = S.psum()

                    def mm(pe, pg2=pg2, ec=ec, tg=tg, ps=ps):
                        ins = None
                        for i in range(4):
                            tt = tg * 4 + i
                            pe.matmul(ps[:, i * 128:(i + 1) * 128], zW[:, tt + 1, pg2, ec * 128:(ec + 1) * 128],
                                      poolP[:, pg2, (1 if tt == 0 else 0), :], start=True, stop=False)
                            ins = pe.matmul(ps[:, i * 128:(i + 1) * 128], zW[:, tt, pg2, ec * 128:(ec + 1) * 128],
                                            poolP[:, pg2, 2, :], start=False, stop=True)
                        return ins
                    S.op("pe", [("zW", 0), ("zW", 1), ("c", "poolP")], [pk], mm)
                    S.op("dve", [pk, ("poolsc",)], [("fb", oc)], lambda v, oc=oc, tg=tg, ps=ps: v.tensor_scalar(
                        out=fb[:, oc, tg * 512:(tg + 1) * 512], in0=ps[:, :], scalar1=poolsc[:, oc:oc + 1],
                        scalar2=None, op0=ALU.mult))
                S.dma("sp", [("fb", oc)], [("yT_d",)], lambda q, oc=oc: q.dma_start(
                    out=yT_d[512 + oc * 128: 512 + (oc + 1) * 128, :], in_=fb[:, oc, :]))

        accO = acc[:, 0, :]
        accD = acc[:, 1, :]
        segn = [0]

        def od_tiles():
            i = 4 + 2 * (segn[0] % 2)
            segn[0] += 1
            return (S._ps[i], ("ps", i)), (S._ps[i + 1], ("ps", i + 1))

        def softmax_pv(ps_s, sk, lo, hi, dtab, ccol, bias_ap, pv_list, extra_reads):
            flush_pv()
            pi = segn[1] % 2
            segn[1] += 1
            xk = ("tmpf", pi)
            S.op("dve", [sk, ("c", "d0dil"), ("c", "d0dil4"), ("c", "d0moba"), ("c", "slopetab")], [xk], lambda v: v.scalar_tensor_tensor(
                out=tmpf[:, pi, lo:hi], in0=dtab, scalar=slopetab[:, ccol:ccol + 1], in1=ps_s[:, lo:hi],
                op0=ALU.mult, op1=ALU.add))
            pk_ = ("Pbf", pi)
            if bias_ap is None:
                S.op("act", [xk], [pk_], lambda a: a.activation(out=Pbf[:, pi, lo:hi], in_=tmpf[:, pi, lo:hi],
                                                                func=AF.Exp, scale=SCALE))
            else:
                S.op("act", [xk, ("c", "mobabias")], [pk_], lambda a: a.activation(
                    out=Pbf[:, pi, lo:hi], in_=tmpf[:, pi, lo:hi], func=AF.Exp, scale=SCALE, bias=bias_ap))
            def emit_pv():
                for (otile, okey, ocol, ncol, pcol, lhsT, start, stop) in pv_list:
                    S.op("pe", [pk_] + extra_reads, [okey], lambda pe: pe.matmul(
                        otile[:, ocol:ocol + ncol], lhsT, Pbf[:, pi, pcol:pcol + ncol], start=start, stop=stop))
            pend.append(emit_pv)
        segn.append(0)
        pend = []

        def flush_pv():
            for f in pend:
                f()
            pend.clear()

        for h4 in range(4):
            for gi in range(3):
                ldF(fa[:, 0, :], 1024 + gi * 512 + h4 * 128, ("fa", 0))
                ldF(fa[:, 1, :], 2560 + gi * 512 + h4 * 128, ("fa", 1))
                qT = fa[:, 0, :]
                kT = fa[:, 1, :]
                c0 = 512 + gi * 512 + h4 * 128
                V = Vt[:, gi, :, :]
                vk = ("Vt", gi)
                if gi == 0:
                    ldV1(V, c0, vk)
                elif gi == 1:
                    for hf in range(2):
                        for r in range(4):
                            S.dma("sp", [("zTg",)], [vk], lambda q, hf=hf, r=r: q.dma_start(
                                out=V[:, r * 4 + hf * 2: r * 4 + hf * 2 + 2, :],
                                in_=zT4[hf, r, :, :, c0:c0 + 128]))
                else:
                    for hf in range(2):
                        S.dma("sp", [("zTg",)], [vk], lambda q, hf=hf: q.dma_start(
                            out=Vt[hf * 64:(hf + 1) * 64, gi, :, :], in_=zT16[hf, :, :, c0:c0 + 128]))
                ccol = gi * 4 + h4
                rd = [("fa", 0), ("fa", 1)]
                for seg in range(4):
                    (psO, ok), (psD, dk) = od_tiles()
                    if gi < 2:
                        d = DILS[gi]
                        a = seg if gi == 0 else 0
                        base = 0 if gi == 0 else seg
                        nlo = 4 * a - 1 if (gi == 0 and a > 0) else 4 * a

                        def tok(n, cnt):
                            if gi == 0:
                                return slice(n * 128, n * 128 + cnt)
                            return slice(512 * n + base, 512 * n + base + 4 * (cnt - 1) + 1, 4)

                        def vidx(n):
                            return n if gi == 0 else base * 4 + n
                        for n in range(nlo, 4 * a + 4):
                            cur_ok = n >= 4 * a
                            nxt_ok = n + 1 <= 4 * a + 3
                            ps_s, sk = s_tile()
                            if cur_ok and nxt_ok:
                                lo, hi = 0, 256
                                S.op("pe", rd, [sk], lambda pe: pe.matmul(
                                    ps_s[:, 0:256], kT[:, tok(n, 128)], qT[:, tok(n, 256)], start=True, stop=True))
                            elif nxt_ok:
                                lo, hi = 128, 256
                                S.op("pe", rd, [sk], lambda pe: pe.matmul(
                                    ps_s[:, 128:256], kT[:, tok(n, 128)], qT[:, tok(n + 1, 128)], start=True, stop=True))
                            else:
                                lo, hi = 0, 128
                                S.op("pe", rd, [sk], lambda pe: pe.matmul(
                                    ps_s[:, 0:128], kT[:, tok(n, 128)], qT[:, tok(n, 128)], start=True, stop=True))
                            pv = []
                            if cur_ok:
                                oc = (n - 4 * a) * 128
                                st = (n == 0)
                                pv.append((psO, ok, oc, 128, 0, V[:, vidx(n), :], st, True))
                                pv.append((psD, dk, oc, 128, 0, C.ones_bf[:], st, True))
                            if nxt_ok:
                                oc = (n + 1 - 4 * a) * 128
                                pv.append((psO, ok, oc, 128, 128, V[:, vidx(n), :], True, False))
                                pv.append((psD, dk, oc, 128, 128, C.ones_bf[:], True, False))
                            softmax_pv(ps_s, sk, lo, hi, d0dil[:, lo:hi], ccol, None, pv, [vk, ("ones_bf",)])
                    else:
                        ps_s, sk = s_tile()

                        def mm(pe, seg=seg, ps_s=ps_s):
                            ins = None
                            for i in range(4):
                                r = seg * 4 + i
                                sl = slice(r, r + 16 * 127 + 1, 16)
                                ins = pe.matmul(ps_s[:, i * 128:(i + 1) * 128], kT[:, sl], qT[:, sl],
                                                start=True, stop=True)
                            return ins
                        S.op("pe", rd, [sk], mm)
                        pv = []
                        for i in range(4):
                            r = seg * 4 + i
                            pv.append((psO, ok, i * 128, 128, i * 128, V[:, r, :], True, True))
                            pv.append((psD, dk, i * 128, 128, i * 128, C.ones_bf[:], True, True))
                        softmax_pv(ps_s, sk, 0, 512, d0dil4[:, :], ccol, None, pv, [vk, ("ones_bf",)])
                    flush_pv()
                    if gi == 0:
                        S.op("act", [ok], [("accO",)], lambda a_: a_.copy(out=accO[:, seg * 512:(seg + 1) * 512], in_=psO[:, :]))
                        S.op("dve", [dk], [("accD",)], lambda v: v.tensor_copy(out=accD[:, seg * 512:(seg + 1) * 512], in_=psD[:, :]))
                    elif gi == 1:
                        sl = slice(seg, 2048, 4)
                        S.op("dve", [ok], [("accO",)], lambda v: v.tensor_tensor(out=accO[:, sl], in0=accO[:, sl], in1=psO[:, :], op=ALU.add))
                        S.op("dve", [dk], [("accD",)], lambda v: v.tensor_tensor(out=accD[:, sl], in0=accD[:, sl], in1=psD[:, :], op=ALU.add))
                    else:
                        for (at, ak, pt, pk2) in ((accO, ("accO",), psO, ok), (accD, ("accD",), psD, dk)):
                            av = at.rearrange("p (j r) -> p r j", r=16)[:, seg * 4:(seg + 1) * 4, :]
                            pv_ = pt[:, :].rearrange("p (i j) -> p i j", i=4)
                            S.op("dve", [pk2], [ak], lambda v, av=av, pv_=pv_: v.tensor_tensor(out=av, in0=av, in1=pv_, op=ALU.add))
            S.op("dve", [("accD",)], [("accD",)], lambda v: v.reciprocal(out=accD, in_=accD))
            S.op("dve", [("accD",), ("accO",)], [("fb", h4)], lambda v: v.tensor_tensor(
                out=fb[:, h4, :], in0=accO, in1=accD, op=ALU.mult))
            S.dma("sp", [("fb", h4)], [("yT_d",)], lambda q: q.dma_start(
                out=yT_d[1024 + h4 * 128: 1024 + (h4 + 1) * 128, :], in_=fb[:, h4, :]))

        gm = gsb[:, 0, :]
        top8 = gsb[:, 1, :]
        sel = gsb[:, 2, :]
        for h4 in range(4):
            ldF(fa[:, 2, :], 4096 + h4 * 128, ("fa", 2))
            ldF(fa[:, 3, :], 4608 + h4 * 128, ("fa", 3))
            qT = fa[:, 2, :]
            kT = fa[:, 3, :]
            V = Vt[:, 0, :, :]
            vk = ("Vt", 0)
            ldV1(V, 2048 + h4 * 128, vk)
            rd = [("fa", 2), ("fa", 3)]
            S.op("dve", [("fa", 3)], [("kmf",)], lambda v: v.reduce_sum(
                out=kmf[:, 0, :], in_=kT.rearrange("p (n t) -> p n t", n=8), axis=AX.X))
            S.op("dve", [("kmf",)], [("kmf",)], lambda v: v.tensor_scalar(
                out=kmf[:, 0, :], in0=kmf[:, 0, :], scalar1=1.0 / 256, scalar2=None, op0=ALU.mult))
            S.op("dve", [("kmf",)], [("kmhl",)], lambda v: v.tensor_copy(out=kmhl[:, 0, :], in_=kmf[:, 0, :]))
            S.op("dve", [("kmhl",)], [("kmf",)], lambda v: v.tensor_copy(out=kmf[:, 1, :], in_=kmhl[:, 0, :]))
            S.op("dve", [("kmf",)], [("kmf",)], lambda v: v.tensor_tensor(
                out=kmf[:, 2, :], in0=kmf[:, 0, :], in1=kmf[:, 1, :], op=ALU.subtract))
            S.op("dve", [("kmf",)], [("kmhl",)], lambda v: v.tensor_copy(out=kmhl[:, 1, :], in_=kmf[:, 2, :]))
            psg, gk = s_tile()

            def gate(pe, psg=psg):
                ins = None
                for tt in range(16):
                    pe.matmul(psg[:, tt * 8:(tt + 1) * 8], qT[:, tt * 128:(tt + 1) * 128], kmhl[:, 0, :],
                              start=True, stop=False)
                    ins = pe.matmul(psg[:, tt * 8:(tt + 1) * 8], qT[:, tt * 128:(tt + 1) * 128], kmhl[:, 1, :],
                                    start=False, stop=True)
                return ins
            S.op("pe", rd + [("kmhl",)], [gk], gate)
            S.op("dve", [gk, ("c", "validbias")], [("gm",)], lambda v: v.tensor_tensor(
                out=gm, in0=psg[:, 0:128], in1=validbias[:], op=ALU.add))
            for tt in range(16):
                S.op("dve", [("gm",)], [("top8",)], lambda v, tt=tt: v.max(
                    out=top8[:, tt * 8:(tt + 1) * 8], in_=gm[:, tt * 8:(tt + 1) * 8]))
            for tt in range(16):
                S.op("dve", [("gm",), ("top8",)], [("sel",)], lambda v, tt=tt: v.tensor_scalar(
                    out=sel[:, tt * 8:(tt + 1) * 8], in0=gm[:, tt * 8:(tt + 1) * 8],
                    scalar1=top8[:, tt * 8 + 2: tt * 8 + 3], scalar2=None, op0=ALU.is_ge))
            S.op("dve", [("sel",)], [("sel",)], lambda v: v.tensor_scalar(
                out=sel, in0=sel, scalar1=BIG, scalar2=-BIG, op0=ALU.mult, op1=ALU.add))
            S.op("dve", [("sel",), ("c", "valid01")], [("sel",)], lambda v: v.tensor_tensor(
                out=sel, in0=sel, in1=valid01[:], op=ALU.mult))
            for g in range(4):
                pst, tk = s_tile()

                def trs(pe, g=g, pst=pst):
                    ins = None
                    for i in range(4):
                        tt = g * 4 + i
                        ins = pe.transpose(out=pst[:8, i * 128:(i + 1) * 128], in_=sel[:, tt * 8:(tt + 1) * 8],
                                           identity=C.ident[:, :])
                    return ins
                S.op("pe", [("sel",), ("ident",)], [tk], trs)
                S.op("act", [tk], [("selT",)], lambda a, g=g, pst=pst: a.copy(
                    out=selT[:8, g * 512:(g + 1) * 512], in_=pst[:8, :]))
            for qr in range(4):
                (psO, ok), (psD, dk) = od_tiles()
                nks = 4 * qr + 4
                for ks in range(nks):
                    ps_s, sk = s_tile()

                    def mm(pe, ks=ks, qr=qr, ps_s=ps_s):
                        pe.matmul(ps_s[:, :], kT[:, ks * 128:(ks + 1) * 128], qT[:, qr * 512:(qr + 1) * 512],
                                  start=True, stop=False)
                        return pe.matmul(ps_s[:, :], selrows[:8, ks // 2, :], selT[:8, qr * 512:(qr + 1) * 512],
                                         start=False, stop=True)
                    S.op("pe", rd + [("selT",), ("c", "selrows")], [sk], mm)
                    j = ks - 4 * qr
                    dv = 0 if j < 0 else 1 + j
                    oi = (4 * qr - ks) + 3
                    bias_ap = mobabias[:, h4 * NOFF + oi: h4 * NOFF + oi + 1]
                    pv = [(psO, ok, 0, 512, 0, V[:, ks, :], ks == 0, ks == nks - 1),
                          (psD, dk, 0, 512, 0, C.ones_bf[:], ks == 0, ks == nks - 1)]
                    softmax_pv(ps_s, sk, 0, 512, d0moba[:, dv, :], 12 + h4, bias_ap, pv, [vk, ("ones_bf",)])
                flush_pv()
                rk = ("tmpf", qr % 2)
                S.op("dve", [dk], [rk], lambda v: v.reciprocal(out=tmpf[:, qr % 2, :], in_=psD[:, :]))
                S.op("dve", [rk, ok], [("fb", h4)], lambda v: v.tensor_tensor(
                    out=fb[:, h4, qr * 512:(qr + 1) * 512], in0=psO[:, :], in1=tmpf[:, qr % 2, :], op=ALU.mult))
            S.dma("sp", [("fb", h4)], [("yT_d",)], lambda q: q.dma_start(
                out=yT_d[1536 + h4 * 128: 1536 + (h4 + 1) * 128, :], in_=fb[:, h4, :]))
        s_barrier(S)


def _rep_piece(S, C, l, m, eb, dst, dkey):
    ps, pk = S.psum()
    for j in range(4):
        kc = eb * 4 + j
        dg = C.diag[kc % 2]
        dk = ("diag", kc % 2)
        S.op("dve", [("modcols",), ("ident",)], [dk], lambda v: v.tensor_scalar(
            out=dg[:], in0=C.ident[:], scalar1=_mod_ap(C, l, m, kc), scalar2=None, op0=ALU.mult))
        S.op("pe", [dk, ("onesf",)], [pk], lambda pe: pe.matmul(
            ps[:, j * 128:(j + 1) * 128], C.onesf[:], dg[:], start=True, stop=True))
    S.op("act", [pk], [dkey], lambda a: a.copy(out=dst, in_=ps[:, :]))


def _phase3a(S, nc, io, C, W, l, th):
    mT_d = C.mT_d[th]
    L = f"{l}_{th}"
    from contextlib import ExitStack
    es_ = ExitStack()
    hT = es_.enter_context(nc.sbuf_tensor(f"p3h_{L}", [128, 32, TPC], BF16))
    yT = es_.enter_context(nc.sbuf_tensor(f"p3y_{L}", [128, 32, TPC], BF16))
    mwb = es_.enter_context(nc.sbuf_tensor(f"p3mw_{L}", [128, 2, 32, 256], BF16))
    bwb = es_.enter_context(nc.sbuf_tensor(f"p3bw_{L}", [128, 2, 8, 256], BF16))
    macc = es_.enter_context(nc.sbuf_tensor(f"p3ma_{L}", [128, 4, 512], F32))
    gsig = es_.enter_context(nc.sbuf_tensor(f"p3gs_{L}", [128, 2, 512], F32))
    tmp = es_.enter_context(nc.sbuf_tensor(f"p3tm_{L}", [128, 512], F32))
    stg = es_.enter_context(nc.sbuf_tensor(f"p3st_{L}", [128, 2, 512], BF16))
    mb = es_.enter_context(nc.sbuf_tensor(f"p3mb_{L}", [128, 4, 32], F32))
    with es_:
        S.dma("sp", [], [("mb",)], lambda q: q.dma_start(out=mb[:], in_=io.mbT[l]))
        hv = C.hT_d[th].rearrange("(kc p) t -> p kc t", p=128)
        for k4 in range(8):
            S.dma("sp", [("hT_d",)], [("hT",)], lambda q: q.dma_start(
                out=hT[:, k4 * 4:(k4 + 1) * 4, :], in_=hv[:, k4 * 4:(k4 + 1) * 4, :]))
        for br in range(4):
            for hs in range(2):
                S.dma("sp", [("yT_d",)], [("yT",)], lambda q: q.dma_start(
                    out=yT[:, br * 8 + hs * 4: br * 8 + hs * 4 + 4, :],
                    in_=C.y_d[hs, br * 512:(br + 1) * 512, th * TPC:(th + 1) * TPC].rearrange("(k p) t -> p k t", p=128)))
        wi = 0
        gi_ = 0
        for eg in range(16):
            for br in range(4):
                mt = mwb[:, wi % 2, :, :]
                bt = bwb[:, wi % 2, :, :]
                mk = ("mwb", wi % 2)
                bk = ("bwb", wi % 2)
                wi += 1
                ap, k = _wslice(W, "merge_w", l, br * 4096 + eg * 256, 256)
                S.dma("pool", [k], [mk], lambda q: q.dma_start(out=mt, in_=ap.rearrange("(kc p) n -> p kc n", p=128)))
                ap2, k2 = _wslice(W, "branch_w", l, eg * 256, 256)
                S.dma("pool", [k2], [bk], lambda q: q.dma_start(
                    out=bt, in_=ap2[br * 1024:(br + 1) * 1024, :].rearrange("(kc p) n -> p kc n", p=128)))
                for e2 in range(2):
                    ec = eg * 2 + e2
                    for t5 in range(2):
                        ai = e2 * 2 + t5
                        psg, gk = S.psum()
                        _mm_group(S, psg[:, :], gk, [(mt[:, kc, e2 * 128:(e2 + 1) * 128], hT[:, kc, t5 * 512:(t5 + 1) * 512])
                                                     for kc in range(32)], [mk, ("hT",)])
                        gs = gsig[:, gi_ % 2, :]
                        gsk = ("gsig", gi_ % 2)
                        gi_ += 1
                        S.op("act", [gk, ("mb",)], [gsk], lambda a: a.activation(
                            out=gs, in_=psg[:, :], func=AF.Sigmoid, bias=mb[:, br, ec:ec + 1]))
                        psp, ppk = S.psum()
                        _mm_group(S, psp[:, :], ppk, [(bt[:, k8, e2 * 128:(e2 + 1) * 128], yT[:, br * 8 + k8, t5 * 512:(t5 + 1) * 512])
                                                      for k8 in range(8)], [bk, ("yT",)])
                        if br == 0:
                            S.op("dve", [gsk, ppk], [("macc", ai)], lambda v: v.tensor_tensor(
                                out=macc[:, ai, :], in0=gs, in1=psp[:, :], op=ALU.mult))
                        else:
                            S.op("dve", [gsk, ppk], [("tmp3",)], lambda v: v.tensor_tensor(
                                out=tmp[:], in0=gs, in1=psp[:, :], op=ALU.mult))
                            S.op("dve", [("tmp3",), ("macc", ai)], [("macc", ai)], lambda v: v.tensor_tensor(
                                out=macc[:, ai, :], in0=macc[:, ai, :], in1=tmp[:], op=ALU.add))
                        if br == 3:
                            st = stg[:, ai % 2, :]
                            sk = ("stg", ai % 2)
                            S.op("act", [("macc", ai)], [sk], lambda a: a.copy(out=st, in_=macc[:, ai, :]))
                            S.dma("sp", [sk], [("mT_d",)], lambda q: q.dma_start(
                                out=mT_d[ec * 128:(ec + 1) * 128, t5 * 512:(t5 + 1) * 512], in_=st))
        s_barrier(S)


def _epilogue_tile(S, C, ps, pk, grep, gkey, x_ap, out_ap, okey, xt, ot, ei):
    xk = ("xt", ei % 2)
    ok_ = ("ot", ei % 2)
    x_ = xt[:, ei % 2, :]
    o_ = ot[:, ei % 2, :]
    S.dma("sp", [("xsrc",)], [xk], lambda q: q.dma_start(out=x_, in_=x_ap))
    S.op("dve", [pk, gkey], [ok_], lambda v: v.tensor_tensor(out=o_, in0=ps[:, :], in1=grep, op=ALU.mult))
    S.op("dve", [ok_, xk], [ok_], lambda v: v.tensor_tensor(out=o_, in0=o_, in1=x_, op=ALU.add))
    S.dma("sp", [ok_], [okey], lambda q: q.dma_start(out=out_ap, in_=o_))


def _phase3b(S, nc, io, C, W, l, x_src, x_dst, th):
    xmid_d = C.xmid_d[th * TPC:(th + 1) * TPC, :]
    L = f"{l}_{th}"
    h2_d = nc.dram_tensor(f"h2T{L}", [D, TPC + 2], BF16).ap()
    mT_d = C.mT_d[th]
    with (nc.sbuf_tensor(f"p4m_{L}", [128, 32, TPC], BF16) as mT,
          nc.sbuf_tensor(f"p4w_{L}", [128, 2, 32, 512], BF16) as owb,
          nc.sbuf_tensor(f"p4g_{L}", [128, 2, 512], F32) as grep,
          nc.sbuf_tensor(f"p4x_{L}", [128, 2, 512], F32) as xt,
          nc.sbuf_tensor(f"p4o_{L}", [128, 2, 512], F32) as ot):
        mv = mT_d.rearrange("(kc p) t -> p kc t", p=128)
        for k4 in range(8):
            S.dma("sp", [("mT_d",)], [("mT",)], lambda q: q.dma_start(
                out=mT[:, k4 * 4:(k4 + 1) * 4, :], in_=mv[:, k4 * 4:(k4 + 1) * 4, :]))
        ei = 0
        for eb in range(8):
            wt = owb[:, eb % 2, :, :]
            wk = ("owb", eb % 2)
            ap, k = _wslice(W, "out_w", l, eb * 512, 512)
            S.dma("pool", [k], [wk], lambda q: q.dma_start(out=wt, in_=ap.rearrange("(kc p) n -> p kc n", p=128)))
            gr = grep[:, eb % 2, :]
            gk = ("grep", eb % 2)
            _rep_piece(S, C, l, 2, eb, gr, gk)
            for tt in range(8):
                ps, pk = S.psum()
                _mm_group(S, ps[:, :], pk, [(mT[:, kc, tt * 128:(tt + 1) * 128], wt[:, kc, :]) for kc in range(32)],
                          [wk, ("mT",)])
                _epilogue_tile(S, C, ps, pk, gr, gk, x_src[tt * 128:(tt + 1) * 128, eb * 512:(eb + 1) * 512],
                               xmid_d[tt * 128:(tt + 1) * 128, eb * 512:(eb + 1) * 512], ("xmid_d",), xt, ot, ei)
                ei += 1
        s_barrier(S)
    with (nc.sbuf_tensor(f"p5h_{L}", [128, 32, TPC + 2], BF16) as h2T,
          nc.sbuf_tensor(f"p5x_{L}", [128, 8192], F32) as big,
          nc.sbuf_tensor(f"p5j_{L}", [128, 4096], BF16) as junk):
        xbufs = [big[:, 0:4096], big[:, 4096:8192]]
        _make_AB(S, C, l, 3, 4, io.g2T[l], C.A, C.B, C.gt)
        S.op("dve", [], [("ss", i) for i in range(16)], lambda v: v.memset(C.ss[:], 0.0))
        _norm_to_hT(S, nc, xmid_d, TPC, xbufs, junk, C.ss, C.rstd, C.ident, C.A, C.B, h2T, ("h2T",), 2)
        S.op("dve", [("h2T",)], [("h2T",)], lambda v: v.memset(h2T[:, :, 0:2], 0.0))
        hv = h2_d.rearrange("(kc p) t -> p kc t", p=128)
        for k4 in range(8):
            S.dma("sp", [("h2T",)], [("h2_d",)], lambda q: q.dma_start(
                out=hv[:, k4 * 4:(k4 + 1) * 4, :], in_=h2T[:, k4 * 4:(k4 + 1) * 4, :]))
        s_barrier(S)
    fT_d = nc.dram_tensor(f"fT{L}", [DFF, TPC], BF16).ap()
    from contextlib import ExitStack
    es_ = ExitStack()
    h2h = es_.enter_context(nc.sbuf_tensor(f"p6h_{L}", [128, 32, TPC + 2], BF16))
    wgb = es_.enter_context(nc.sbuf_tensor(f"p6wg_{L}", [128, 2, 32, 128], BF16))
    wub = es_.enter_context(nc.sbuf_tensor(f"p6wu_{L}", [128, 2, 32, 128], BF16))
    gsb = es_.enter_context(nc.sbuf_tensor(f"p6gs_{L}", [128, 2, 514], F32))
    ab = es_.enter_context(nc.sbuf_tensor(f"p6a_{L}", [128, 2, 512], F32))
    fst = es_.enter_context(nc.sbuf_tensor(f"p6fs_{L}", [128, 4, 512], BF16))
    cw = es_.enter_context(nc.sbuf_tensor(f"p6cw_{L}", [128, 3, NFC], F32))
    cb = es_.enter_context(nc.sbuf_tensor(f"p6cb_{L}", [128, NFC], F32))
    with es_:
        S.dma("sp", [], [("cw",)], lambda q: q.dma_start(out=cw[:], in_=io.convw[l]))
        S.dma("sp", [], [("cb",)], lambda q: q.dma_start(out=cb[:], in_=io.convb[l]))
        hv = h2_d.rearrange("(kc p) t -> p kc t", p=128)
        for k4 in range(8):
            S.dma("sp", [("h2_d",)], [("h2h",)], lambda q: q.dma_start(
                out=h2h[:, k4 * 4:(k4 + 1) * 4, :], in_=hv[:, k4 * 4:(k4 + 1) * 4, :]))
        it = 0
        for fc in range(NFC):
            wg = wgb[:, fc % 2, :, :]
            wu = wub[:, fc % 2, :, :]
            gk_, uk_ = ("wgb", fc % 2), ("wub", fc % 2)
            ap, k = _wslice(W, "ffn_wg", l, fc * 128, 128)
            S.dma("pool", [k], [gk_], lambda q: q.dma_start(out=wg, in_=ap.rearrange("(kc p) n -> p kc n", p=128)))
            ap, k = _wslice(W, "ffn_wu", l, fc * 128, 128)
            S.dma("pool", [k], [uk_], lambda q: q.dma_start(out=wu, in_=ap.rearrange("(kc p) n -> p kc n", p=128)))
            for t2 in range(2):
                c0 = 2 + t2 * 512
                psg, pgk = S.psum()
                _mm_group(S, psg[:, :], pgk, [(wg[:, kc, :], h2h[:, kc, c0:c0 + 512]) for kc in range(32)], [gk_, ("h2h",)])
                psu, puk = S.psum()
                _mm_group(S, psu[:, :], puk, [(wu[:, kc, :], h2h[:, kc, c0:c0 + 512]) for kc in range(32)], [uk_, ("h2h",)])
                gs = gsb[:, it % 2, :]
                gsk = ("gsb", it % 2)
                a_ = ab[:, it % 2, :]
                ak = ("ab", it % 2)
                fs = fst[:, it % 4, :]
                fk = ("fst", it % 4)
                it += 1
                S.op("act", [pgk], [gsk], lambda a: a.copy(out=gs[:, 2:514], in_=psg[:, :]))
                S.op("dve", [("glast",)], [gsk], lambda v: v.tensor_copy(out=gs[:, 0:2], in_=C.glast[:, fc, :]))
                S.op("dve", [gsk], [("glast",)], lambda v: v.tensor_copy(out=C.glast[:, fc, :], in_=gs[:, 512:514]))
                S.op("dve", [gsk, ("cw",), ("cb",)], [ak], lambda v: v.tensor_scalar(
                    out=a_, in0=gs[:, 2:514], scalar1=cw[:, 2, fc:fc + 1], scalar2=cb[:, fc:fc + 1],
                    op0=ALU.mult, op1=ALU.add))
                S.op("dve", [gsk, ak], [ak], lambda v: v.scalar_tensor_tensor(
                    out=a_, in0=gs[:, 1:513], scalar=cw[:, 1, fc:fc + 1], in1=a_, op0=ALU.mult, op1=ALU.add))
                S.op("dve", [gsk, ak], [ak], lambda v: v.scalar_tensor_tensor(
                    out=a_, in0=gs[:, 0:512], scalar=cw[:, 0, fc:fc + 1], in1=a_, op0=ALU.mult, op1=ALU.add))
                S.op("act", [ak], [ak], lambda a: a.activation(out=a_, in_=a_, func=AF.Gelu_apprx_tanh))
                S.op("dve", [ak, puk], [fk], lambda v: v.tensor_tensor(out=fs, in0=a_, in1=psu[:, :], op=ALU.mult))
                S.dma("sp", [fk], [("fT_d",)], lambda q: q.dma_start(
                    out=fT_d[fc * 128:(fc + 1) * 128, t2 * 512:(t2 + 1) * 512], in_=fs))
        s_barrier(S)
    es_ = ExitStack()
    fT = es_.enter_context(nc.sbuf_tensor(f"p7f_{L}", [128, NFC, 512], BF16))
    wdb = es_.enter_context(nc.sbuf_tensor(f"p7wd_{L}", [128, 2, 4, 512], BF16))
    grep = es_.enter_context(nc.sbuf_tensor(f"p7g_{L}", [128, 2, 512], F32))
    xt = es_.enter_context(nc.sbuf_tensor(f"p7x_{L}", [128, 2, 512], F32))
    ot = es_.enter_context(nc.sbuf_tensor(f"p7o_{L}", [128, 2, 512], F32))
    with es_:
        ei = 0
        wdi = 0
        fv = fT_d.rearrange("(fc p) t -> p fc t", p=128)
        for t2 in range(2):
            for f4 in range(0, NFC, 8):
                n8 = min(8, NFC - f4)
                S.dma("sp", [("fT_d",)], [("fT",)], lambda q: q.dma_start(
                    out=fT[:, f4:f4 + n8, :], in_=fv[:, f4:f4 + n8, t2 * 512:(t2 + 1) * 512]))
            for eb in range(8):
                banks = [(S._ps[(eb % 2) * 4 + i], ("ps", (eb % 2) * 4 + i)) for i in range(4)]
                gr = grep[:, eb % 2, :]
                gk = ("grep", eb % 2)
                _rep_piece_fixed(S, C, l, 5, eb, gr, gk, banks)
                for fg in range(22):
                    nf = 4 if fg < 21 else 2
                    wd = wdb[:, wdi % 2, :, :]
                    wk = ("wdb", wdi % 2)
                    wdi += 1
                    ap, k = _wslice(W, "ffn_wd", l, eb * 512, 512)
                    S.dma("pool", [k], [wk], lambda q: q.dma_start(
                        out=wd[:, 0:nf, :], in_=ap[fg * 512: fg * 512 + nf * 128, :].rearrange("(k p) n -> p k n", p=128)))

                    def mm(pe, fg=fg, nf=nf, wd=wd, banks=banks):
                        ins = None
                        for k_ in range(nf):
                            fc = fg * 4 + k_
                            for t4 in range(4):
                                ins = pe.matmul(banks[t4][0][:, :], fT[:, fc, t4 * 128:(t4 + 1) * 128], wd[:, k_, :],
                                                start=(fc == 0), stop=(fc == NFC - 1))
                        return ins
                    S.op("pe", [wk, ("fT",)], [b[1] for b in banks], mm)
                for t4 in range(4):
                    r0 = t2 * 512 + t4 * 128
                    _epilogue_tile(S, C, banks[t4][0], banks[t4][1], gr, gk,
                                   xmid_d[r0:r0 + 128, eb * 512:(eb + 1) * 512],
                                   x_dst[r0:r0 + 128, eb * 512:(eb + 1) * 512], ("x_dst",), xt, ot, ei)
                    ei += 1
        s_barrier(S)


def _rep_piece_fixed(S, C, l, m, eb, dst, dkey, banks):
    ps, pk = banks[0]
    for j in range(4):
        kc = eb * 4 + j
        dg = C.diag[kc % 2]
        dk = ("diag", kc % 2)
        S.op("dve", [("modcols",), ("ident",)], [dk], lambda v: v.tensor_scalar(
            out=dg[:], in0=C.ident[:], scalar1=_mod_ap(C, l, m, kc), scalar2=None, op0=ALU.mult))
        S.op("pe", [dk, ("onesf",)], [pk], lambda pe: pe.matmul(
            ps[:, j * 128:(j + 1) * 128], C.onesf[:], dg[:], start=True, stop=True))
    S.op("act", [pk], [dkey], lambda a: a.copy(out=dst, in_=ps[:, :]))


def _final_norm(S, nc, io, C, x_src):
    with (nc.sbuf_tensor("pfx", [128, 2, 4096], F32) as xb,
          nc.sbuf_tensor("pfj", [128, 4096], BF16) as junk,
          nc.sbuf_tensor("pfg", [128, 4096], F32) as gf):
        S.dma("sp", [], [("gf",)], lambda q: q.dma_start(out=gf[:], in_=io.gfin))
        S.op("dve", [], [("ss", i) for i in range(16)], lambda v: v.memset(C.ss[:], 0.0))
        for tt in range(16):
            xt = xb[:, tt % 2, :]
            xk = ("xb", tt % 2)
            S.dma("sp", [("x_dst",)], [xk], lambda q: q.dma_start(out=xt, in_=x_src[tt * 128:(tt + 1) * 128, :]))
            S.op("act", [xk], [("junk",), ("ss", tt)], lambda a: a.activation(
                out=junk[:], in_=xt, func=AF.Square, accum_out=C.ss[:, tt:tt + 1]))
            _rstd(S, C.ss[:, tt:tt + 1], C.rstd[:, tt:tt + 1], 1.0 / D, ("ss", tt), ("rstd", tt))
            S.op("dve", [xk, ("rstd", tt), ("gf",)], [xk], lambda v: v.scalar_tensor_tensor(
                out=xt, in0=xt, scalar=C.rstd[:, tt:tt + 1], in1=gf[:], op0=ALU.mult, op1=ALU.mult))
            S.dma("sp", [xk], [("out",)], lambda q: q.dma_start(out=io.out[tt * 128:(tt + 1) * 128, :], in_=xt))


NCORES = 4


def build_fused(debug=False, n_layers=2, fake=False, stop_after=None):
    nc = bass.Bass("TRN2", target_bir_lowering=False)
    io = _declare_io(nc, fake, n_layers)
    dbg = {}
    if debug:
        for nm, shp, dt in (("mod", [128, 384], F32), ("p1", [4 * 5120, TPC], BF16), ("p1t", [4 * TPC, 2560], BF16),
                            ("p2", [2 * 2048, S_LEN], BF16),
                            ("p3a", [2 * D, TPC], BF16), ("p3b", [S_LEN, D], F32)):
            dbg[nm] = nc.dram_tensor("dbg_" + nm, shp, dt, kind="ExternalOutput").ap()
    S = Sched(nc)
    _sched_ext(S)
    S.psum_init()
    C = Ctx()
    C.n_layers = n_layers
    C.ident = nc.alloc_sbuf_tensor("ident_sb", [128, 128], F32)
    C.onesf = nc.alloc_sbuf_tensor("onesf", [128, 128], F32)
    C.ones_bf = nc.alloc_sbuf_tensor("ones_bf", [128, 128], BF16)
    C.modcols = nc.alloc_sbuf_tensor("modcols", [128, 2, 192], F32)
    C.A = nc.alloc_sbuf_tensor("A", [128, 32], F32)
    C.B = nc.alloc_sbuf_tensor("B", [128, 32], F32)
    C.gt = nc.alloc_sbuf_tensor("gt", [128, 32], F32)
    C.ss = nc.alloc_sbuf_tensor("ss", [128, 16], F32)
    C.rstd = nc.alloc_sbuf_tensor("rstd", [128, 16], F32)
    C.diag = [nc.alloc_sbuf_tensor(f"diag{i}", [128, 128], F32) for i in range(2)]
    C.glast = nc.alloc_sbuf_tensor("glast", [128, NFC, 2], F32)
    S.dma("sp", [], [("ident",)], lambda q: q.dma_start(out=C.ident[:], in_=io.tabs["ident"]))
    S.dma("sp", [], [("ones_bf",)], lambda q: q.dma_start(out=C.ones_bf[:], in_=io.tabs["ones_bf"]))
    S.op("dve", [], [("onesf",)], lambda v: v.memset(C.onesf[:], 1.0))
    W = {}
    x1_d = nc.dram_tensor("x1_d", [S_LEN, D], F32).ap()
    x2_d = nc.dram_tensor("x2_d", [S_LEN, D], F32).ap()
    C.xmid_d = nc.dram_tensor("xmid_d", [S_LEN, D], F32).ap()
    C.hT_d = nc.dram_tensor("hT_d", [2, D, TPC], BF16).ap()
    C.mT_d = nc.dram_tensor("mT_d", [2, D, TPC], BF16).ap()
    C.zF_d = nc.dram_tensor("zF_d", [2, 2, 5120, TPC], BF16).ap()
    C.zT_d = nc.dram_tensor("zT_d", [2, 2, TPC, 2560], BF16).ap()
    C.y_d = nc.dram_tensor("y_d", [2, 2048, S_LEN], BF16).ap()

    def dump(nm, src):
        if debug:
            S.dma("sp", [], [("dbg", nm)], lambda q: q.dma_start(out=dbg[nm], in_=src))

    def stop():
        S.dma("sp", [], [("out",)], lambda q: q.dma_start(out=io.out[0:128, 0:128], in_=io.x[0:128, 0:128]))
        S.finish("sp")
        return nc
    _mod_phase(S, nc, io, C)
    if stop_after == "mod":
        return stop()
    if debug:
        S.dma("sp", [("modcols",)], [("dbg", "mod")], lambda q: q.dma_start(
            out=dbg["mod"], in_=C.modcols[:].rearrange("p a b -> p (a b)")))
    x_src = io.x
    for l in range(n_layers):
        _weights_phase(S, nc, io, l, W)
        S.op("dve", [("glast",)], [("glast",)], lambda v: v.memset(C.glast[:], 0.0))
        x_dst = x1_d if l == 0 else x2_d
        for th in range(2):
            _phase1(S, nc, io, C, W, l, x_src[th * TPC:(th + 1) * TPC, :], th)
        if stop_after == "p1":
            return stop()
        dump("p1", C.zF_d.rearrange("a b r t -> (a b r) t"))
        dump("p1t", C.zT_d.rearrange("a b t c -> (a b t) c"))
        for hh in range(2):
            _phase2(S, nc, io, C, l, hh)
        if stop_after == "p2":
            return stop()
        dump("p2", C.y_d.rearrange("a r t -> (a r) t"))
        for th in range(2):
            _phase3a(S, nc, io, C, W, l, th)
        if stop_after == "p3a":
            return stop()
        dump("p3a", C.mT_d.rearrange("a r t -> (a r) t"))
        for th in range(2):
            _phase3b(S, nc, io, C, W, l, x_src[th * TPC:(th + 1) * TPC, :], x_dst[th * TPC:(th + 1) * TPC, :], th)
        if stop_after == "p3b":
            return stop()
        dump("p3b", x_dst)
        x_src = x_dst
    _final_norm(S, nc, io, C, x_src)
    S.finish("sp")
    print("instr counts", S.ccnt, flush=True)
    return nc


def _prep_inputs(inp, NL=2):
    ca = np.ascontiguousarray
    x = inp["x"]
    g1T = ca(inp["norm1_g"].reshape(2, 32, 128).transpose(0, 2, 1))
    g2T = ca(inp["norm2_g"].reshape(2, 32, 128).transpose(0, 2, 1))
    gfin = ca(np.broadcast_to(inp["final_g"][None, :], (128, D)))
    gsgu = ca(np.broadcast_to(inp["sgu_norm_g"][:, None, :], (2, 128, 1024)))
    mbT = ca(inp["merge_b"].reshape(2, 4, 32, 128).transpose(0, 3, 1, 2))
    convw = ca(inp["conv_w"].reshape(2, 3, NFC, 128).transpose(0, 3, 1, 2))
    convb = ca(inp["conv_b"].reshape(2, NFC, 128).transpose(0, 2, 1))
    sguwT = ca(np.stack([inp["sgu_w"][:, 4 * hh:4 * hh + 4].transpose(0, 3, 1, 2) for hh in range(2)], axis=1))
    sgub = ca(np.stack([np.broadcast_to(np.tile(inp["sgu_b"][:, 4 * hh:4 * hh + 4, :], (1, 1, 4))[:, None, :, :],
                                        (2, 128, 4, 512)) for hh in range(2)], axis=1))
    poolw = ca(np.stack([inp["pool_w"][:, 2 * hh:2 * hh + 2].reshape(2, 2, 2, 128, 256).transpose(0, 3, 1, 2, 4)
                         for hh in range(2)], axis=1))
    poolsc = ca(np.stack([inp["pool_scale"][:, hh * 512:(hh + 1) * 512].reshape(2, 4, 128).transpose(0, 2, 1)
                          for hh in range(2)], axis=1))
    shared = {"w_ada": ca(inp["w_ada"][:NL]), "b_adaT": ca(inp["b_ada"][:NL].reshape(NL, 192, 128).transpose(0, 2, 1)),
              "g1T": g1T, "g2T": g2T, "gfin": gfin, "gsgu": gsgu, "mbT": mbT, "convw": convw, "convb": convb,
              "sguwT": sguwT, "sgub": sgub, "poolw": poolw, "poolsc": poolsc,
              "w_in": ca(inp["w_in"][:NL]), "merge_w": ca(inp["merge_w"].reshape(2, D, 4 * D)[:NL]),
              "branch_w": ca(inp["branch_w"].reshape(2, D, D)[:NL]), "out_w": ca(inp["out_w"][:NL]),
              "ffn_wg": ca(inp["ffn_wg"][:NL]), "ffn_wu": ca(inp["ffn_wu"][:NL]), "ffn_wd": ca(inp["ffn_wd"][:NL])}
    for k, v in _const_tables_all().items():
        shared["t_" + k] = v
    maps = []
    for b in range(NCORES):
        m = dict(shared)
        m["x"] = ca(x[b])
        m["cT"] = ca(inp["c"][b].reshape(32, 128).T)
        maps.append(m)
    return maps


_NC_CACHE = {}


def kernel(**inputs):
    inp = {k: np.asarray(v) for k, v in inputs.items()}
    if "nc" not in _NC_CACHE:
        _NC_CACHE["nc"] = build_fused()
    maps = _prep_inputs(inp)
    res = run_bass_kernel_spmd(_NC_CACHE["nc"], maps, core_ids=list(range(NCORES)))
    out = np.empty((NB, S_LEN, D), np.float32)
    for b in range(NCORES):
        out[b] = np.asarray(res.results[b]["out"], dtype=np.float32)
    return out
```

```python
import numpy as np
import ml_dtypes
import concourse.bass as bass
import concourse.mybir as mybir
from concourse.bass_utils import run_bass_kernel_spmd

F32 = mybir.dt.float32
BF16 = mybir.dt.bfloat16
ALU = mybir.AluOpType
AF = mybir.ActivationFunctionType
AX = mybir.AxisListType

D = 4096
S = 2048
S_LEN = 2048
NB = 4
TPC = 1024
DFF = 11008
NFC = DFF // 128
EPS = 1e-6
IN_COLS = 15360
BIG = 30000.0


class Sched:
    def __init__(self, nc, n_dma_sems=20, strict=True):
        self.nc = nc
        self.eng = {"pe": nc.tensor, "act": nc.scalar, "dve": nc.vector,
                    "pool": nc.gpsimd, "sp": nc.sync}
        self.csem = {e: nc.alloc_semaphore(name=f"c_{e}") for e in ("pe", "act", "dve", "pool")}
        self.ccnt = {e: 0 for e in self.csem}
        self.dsem = {}
        self.dcnt = {}
        self.dnext = {}
        for q in ("sp", "pool", "act"):
            self.dsem[q] = [nc.alloc_semaphore(name=f"d_{q}{i}") for i in range(n_dma_sems)]
            self.dcnt[q] = [0] * n_dma_sems
            self.dnext[q] = 0
        self.seen = {e: {} for e in self.eng}
        self.lastw = {}
        self.reads = {}
        self.strict = strict
        self._ps = None
        self._psn = 0

    def _sem(self, k):
        if k[0] == "c":
            return self.csem[k[1]]
        return self.dsem[k[1]][k[2]]

    def _wait(self, e, k, v):
        if self.seen[e].get(k, 0) >= v:
            return
        self.eng[e].wait_ge(self._sem(k), v)
        self.seen[e][k] = v

    def _deps(self, e, reads, writes):
        deps = {}

        def add(t):
            if t is None:
                return
            k, v = t
            if deps.get(k, 0) < v:
                deps[k] = v
        for k in reads:
            add(self.lastw.get(k))
        for k in writes:
            add(self.lastw.get(k))
            for kk, vv in self.reads.get(k, {}).items():
                add((kk, vv))
        for k, v in deps.items():
            if k[0] == "c" and k[1] == e and (e == "pe" or not self.strict):
                continue
            self._wait(e, k, v)

    def _commit(self, ticket, reads, writes):
        for k in writes:
            self.lastw[k] = ticket
            self.reads[k] = {}
        for k in reads:
            r = self.reads.setdefault(k, {})
            if r.get(ticket[0], 0) < ticket[1]:
                r[ticket[0]] = ticket[1]

    def op(self, e, reads, writes, fn):
        self._deps(e, reads, writes)
        ins = fn(self.eng[e])
        self.ccnt[e] += 1
        ins.then_inc(self.csem[e], 1)
        t = (("c", e), self.ccnt[e])
        self._commit(t, reads, writes)
        return t

    def dma(self, q, reads, writes, fn):
        i = self.dnext[q]
        self.dnext[q] = (i + 1) % len(self.dsem[q])
        k = ("d", q, i)
        if self.dcnt[q][i] > 0:
            self._wait(q, k, self.dcnt[q][i])
        self._deps(q, reads, writes)
        insl = fn(self.eng[q])
        if not isinstance(insl, (list, tuple)):
            insl = [insl]
        for ins in insl:
            ins.then_inc(self.dsem[q][i], 16)
            self.dcnt[q][i] += 16
        t = (k, self.dcnt[q][i])
        self._commit(t, reads, writes)
        return t

    def finish(self, e="sp"):
        for q in self.dsem:
            for i, c in enumerate(self.dcnt[q]):
                if c > 0:
                    self._wait(e, ("d", q, i), c)

    def psum_init(self):
        self._ps = [self.nc.alloc_psum_tensor(f"psb{i}", [128, 512], F32) for i in range(8)]

    def psum(self):
        i = self._psn % 8
        self._psn += 1
        return self._ps[i], ("ps", i)


def _mm_group(S, ps_ap, pskey, pairs, reads, extra_writes=()):
    n = len(pairs)

    def fn(pe):
        ins = None
        for i, (l, r) in enumerate(pairs):
            ins = pe.matmul(ps_ap, l, r, start=(i == 0), stop=(i == n - 1))
        return ins
    return S.op("pe", reads, [pskey] + list(extra_writes), fn)


def _rstd(S, ss_ap, rstd_ap, inv_n, sskey, rkey):
    S.op("dve", [sskey], [rkey],
         lambda v: v.tensor_scalar(out=rstd_ap, in0=ss_ap, scalar1=inv_n, scalar2=EPS,
                                   op0=ALU.mult, op1=ALU.add))
    S.op("act", [rkey], [rkey], lambda a: a.activation(out=rstd_ap, in_=rstd_ap, func=AF.Sqrt))
    S.op("dve", [rkey], [rkey], lambda v: v.reciprocal(out=rstd_ap, in_=rstd_ap))


def _norm_to_hT(S, nc, x_ap, n_tok, xbufs, junk, ss, rstd, ident, A, B, hT, hkey, col0):
    ntile = (n_tok + 127) // 128
    for tt in range(ntile):
        p = min(128, n_tok - tt * 128)
        xt = xbufs[tt % 2]
        xk = ("xbuf", id(xt))
        S.dma("sp", [], [xk], lambda q: q.dma_start(out=xt[:p, :], in_=x_ap[tt * 128: tt * 128 + p, :]))
        S.op("act", [xk], [("junk",), ("ss", tt)],
             lambda a: a.activation(out=junk[:p, :], in_=xt[:p, :], func=AF.Square,
                                    accum_out=ss[:p, tt:tt + 1]))
        _rstd(S, ss[:p, tt:tt + 1], rstd[:p, tt:tt + 1], 1.0 / D, ("ss", tt), ("rstd", tt))
        S.op("dve", [("rstd", tt), xk], [xk],
             lambda v: v.tensor_scalar(out=xt[:p, :], in0=xt[:p, :], scalar1=rstd[:p, tt:tt + 1],
                                       scalar2=None, op0=ALU.mult))
        for g in range(8):
            ps, pk = S.psum()

            def tr(pe, g=g, ps=ps):
                ins = None
                for j in range(4):
                    kc = g * 4 + j
                    ins = pe.transpose(out=ps[:, j * 128: j * 128 + p],
                                       in_=xt[:p, kc * 128:(kc + 1) * 128], identity=ident[:p, :p])
                return ins
            S.op("pe", [xk, ("ident",)], [pk], tr)
            for j in range(4):
                kc = g * 4 + j
                S.op("act", [pk, ("AB",)], [hkey],
                     lambda a, kc=kc, j=j, ps=ps: a.activation(
                         out=hT[:, kc, col0 + tt * 128: col0 + tt * 128 + p],
                         in_=ps[:, j * 128: j * 128 + p], func=AF.Identity,
                         bias=B[:, kc:kc + 1], scale=A[:, kc:kc + 1]))


def _load_AB(S, nc, modT_d, gT_d, i_shift, i_scale, A, B, gt, modt):
    S.dma("sp", [], [("modt",)], lambda q: q.dma_start(out=modt[:], in_=modT_d))
    S.dma("sp", [], [("gt",)], lambda q: q.dma_start(out=gt[:], in_=gT_d))
    S.op("dve", [("modt",), ("gt",)], [("AB",)],
         lambda v: v.scalar_tensor_tensor(out=A[:], in0=modt[:, i_scale, :], scalar=1.0, in1=gt[:],
                                          op0=ALU.add, op1=ALU.mult))
    S.op("dve", [("modt",)], [("AB",)],
         lambda v: v.tensor_copy(out=B[:], in_=modt[:, i_shift, :]))


def _gelu(S, out_ap, in_ap, tmp_ap, reads, writes, tmpkey):
    S.op("dve", reads, [tmpkey],
         lambda v: v.tensor_tensor(out=tmp_ap, in0=in_ap, in1=in_ap, op=ALU.mult))
    S.op("dve", [tmpkey], [tmpkey],
         lambda v: v.tensor_scalar(out=tmp_ap, in0=tmp_ap, scalar1=0.044715, scalar2=1.0,
                                   op0=ALU.mult, op1=ALU.add))
    S.op("dve", [tmpkey] + list(reads), [tmpkey],
         lambda v: v.tensor_tensor(out=tmp_ap, in0=tmp_ap, in1=in_ap, op=ALU.mult))
    S.op("act", [tmpkey], [tmpkey],
         lambda a: a.activation(out=tmp_ap, in_=tmp_ap, func=AF.Sigmoid, scale=1.5957691216057308))
    S.op("dve", [tmpkey] + list(reads), writes,
         lambda v: v.tensor_tensor(out=out_ap, in0=tmp_ap, in1=in_ap, op=ALU.mult))


def _sched_ext(S, n_coll_sems=8):
    S.csem_coll = [S.nc.alloc_semaphore(name=f"cc{i}") for i in range(n_coll_sems)]
    S.ccnt_coll = [0] * n_coll_sems
    S.cnext = 0
    S.dsem["coll"] = S.csem_coll
    S.dcnt["coll"] = S.ccnt_coll


def s_coll(S, reads, writes, fn):
    i = S.cnext
    S.cnext = (i + 1) % len(S.csem_coll)
    k = ("d", "coll", i)
    if S.ccnt_coll[i] > 0:
        S._wait("pool", k, S.ccnt_coll[i])
    S._deps("pool", reads, writes)
    ins = fn(S.eng["pool"])
    ins.then_inc(S.csem_coll[i], 1)
    S.ccnt_coll[i] += 1
    t = (k, S.ccnt_coll[i])
    S._commit(t, reads, writes)
    return t


def s_barrier(S):
    for e in ("pe", "act", "dve", "pool", "sp"):
        for o in ("pe", "act", "dve", "pool"):
            if o != e and S.ccnt[o] > 0:
                S._wait(e, ("c", o), S.ccnt[o])
        for q in S.dsem:
            for i, c in enumerate(S.dcnt[q]):
                if c > 0:
                    S._wait(e, ("d", q, i), c)


PAIRS = [[0, 1], [2, 3], [4, 5], [6, 7]]
ALL8 = [list(range(8))]
SCALE = 128.0 ** -0.5
BIGP = 1.0e6
SLOPES = 2.0 ** (-8.0 * np.arange(1, 33) / 32.0)
DILS = (1, 4, 16)
POOL_W = (2, 4, 8, 16)
NOFF = 20


def _ag(S, in_ap, out_ap, groups, reads, writes):
    return s_coll(S, reads, writes, lambda g: g.collective_compute(
        "AllGather", ALU.bypass, replica_groups=groups, ins=[in_ap], outs=[out_ap]))


def _const_tables(core):
    hh = core % 2
    t = {}
    t["ident"] = np.eye(128, dtype=np.float32)
    k = np.arange(128)[:, None]
    q = np.arange(256)[None, :]
    d0 = np.empty((128, 256), np.float32)
    cur = (q[:, :128] - k).astype(np.float32)
    d0[:, :128] = np.where(cur >= 0, cur, BIGP)
    prv = (q[:, :128] - k + 128).astype(np.float32)
    d0[:, 128:] = np.where(prv <= 128, prv, BIGP)
    t["d0dil"] = d0
    t["d0dil4"] = np.ascontiguousarray(np.tile(d0[:, :128], (1, 4)))
    q5 = np.arange(512)[None, :]
    dm = np.empty((5, 128, 512), np.float32)
    dm[0] = q5 - k
    for j in range(4):
        v = (q5 - k).astype(np.float32)
        dm[1 + j] = np.where(v >= 128 * j, v, BIGP)
    t["d0moba"] = dm
    st = np.zeros((128, 16), np.float32)
    for gi in range(3):
        for h4 in range(4):
            st[:, gi * 4 + h4] = -SLOPES[gi + 4 * (4 * hh + h4)] * DILS[gi] / SCALE
    mb = np.zeros((128, 4 * NOFF), np.float32)
    for h4 in range(4):
        sl = SLOPES[3 + 4 * (4 * hh + h4)]
        st[:, 12 + h4] = -sl / SCALE
        for oi in range(NOFF):
            mb[:, h4 * NOFF + oi] = -sl * 128.0 * (oi - 3)
    t["slopetab"] = st
    t["mobabias"] = mb
    vb = np.zeros((128, 16, 8), np.float32)
    v01 = np.zeros((128, 16, 8), np.float32)
    for tt in range(16):
        for n in range(8):
            ok = n < tt // 2
            vb[:, tt, n] = 0.0 if ok else -1.0e30
            v01[:, tt, n] = 1.0 if ok else 0.0
    t["validbias"] = vb.reshape(128, 128)
    t["valid01"] = v01.reshape(128, 128)
    selr = np.zeros((8, 8, 128), np.float32)
    for n in range(8):
        selr[n, n, :] = 1.0
    t["selrows"] = np.ascontiguousarray(selr.transpose(1, 0, 2)).astype(ml_dtypes.bfloat16)
    pm = np.zeros((2, 3, 128, 128), np.float32)
    s_ = np.arange(128)[:, None]
    t_ = np.arange(128)[None, :]
    for pg2 in range(2):
        w = POOL_W[2 * hh + pg2]
        band = ((s_ <= t_) & (s_ > t_ - w)).astype(np.float32)
        pm[pg2, 0] = band / w - (s_ == t_)
        cnt = np.minimum(t_ + 1, w).astype(np.float32)
        pm[pg2, 1] = band / cnt - (s_ == t_)
        pm[pg2, 2] = ((s_ - 128 > t_ - w)).astype(np.float32) / w
    t["poolP"] = np.ascontiguousarray(pm.transpose(2, 0, 1, 3)).astype(ml_dtypes.bfloat16)
    t["tri"] = (s_ <= t_).astype(np.float32)
    t["halo_on"] = np.full((128, 1), float(hh), np.float32)
    t["ones_bf"] = np.ones((128, 128), ml_dtypes.bfloat16)
    return t


HH_TABS = ("slopetab", "mobabias", "poolP")


def _const_tables_all():
    t0 = _const_tables(0)
    t1 = _const_tables(1)
    out = {}
    for k in t0:
        if k in HH_TABS:
            out[k] = np.ascontiguousarray(np.stack([t0[k], t1[k]]))
        elif k != "halo_on":
            out[k] = t0[k]
    return out


W_PIECES = {
    "w_in": (512, 15360, 3072),
    "merge_w": (512, 16384, 4096),
    "branch_w": (512, 4096, 4096),
    "out_w": (512, 4096, 4096),
    "ffn_wg": (512, DFF, 5504),
    "ffn_wu": (512, DFF, 5504),
    "ffn_wd": (1376, 4096, 2048),
}


class Ctx:
    pass


def _declare_io(nc, fake=False, NL=2):
    io = Ctx()
    BIGW = ("w_ada",) + tuple(W_PIECES)
    def inp(name, shape, dt=F32):
        if fake and name in BIGW:
            return nc.dram_tensor(name, list(shape), dt).ap()
        return nc.dram_tensor(name, list(shape), dt, kind="ExternalInput").ap()
    io.x = inp("x", [S_LEN, D])
    io.cT = inp("cT", [128, 32])
    io.w_ada = inp("w_ada", [NL, D, 6 * D])
    io.b_adaT = inp("b_adaT", [NL, 128, 192])
    io.g1T = inp("g1T", [2, 128, 32])
    io.g2T = inp("g2T", [2, 128, 32])
    io.gfin = inp("gfin", [128, D])
    io.gsgu = inp("gsgu", [2, 128, 1024])
    io.sguwT = inp("sguwT", [2, 2, 128, 4, 128])
    io.sgub = inp("sgub", [2, 2, 128, 4, 512])
    io.poolw = inp("poolw", [2, 2, 128, 2, 2, 256])
    io.poolsc = inp("poolsc", [2, 2, 128, 4])
    io.mbT = inp("mbT", [2, 128, 4, 32])
    io.convw = inp("convw", [2, 128, 3, NFC])
    io.convb = inp("convb", [2, 128, NFC])
    for n, (r, c, pw) in W_PIECES.items():
        setattr(io, n, inp(n, [NL, 8 * r, c]))
    tabs = _const_tables_all()
    io.tabs = {}
    for k, v in tabs.items():
        io.tabs[k] = inp("t_" + k, v.shape, BF16 if v.dtype == ml_dtypes.bfloat16 else F32)
    io.out = nc.dram_tensor("out", [S_LEN, D], F32, kind="ExternalOutput").ap()
    return io


def _weights_phase(S, nc, io, l, W):
    for n, (r, c, pw) in W_PIECES.items():
        W[(n, l)] = [(getattr(io, n)[l], ("wext",), c)]


def _wslice(W, n, l, c0, ncols):
    pw = W[(n, l)][0][2]
    j = c0 // pw
    assert (c0 + ncols - 1) // pw == j
    ga, k, _ = W[(n, l)][j]
    return ga[:, c0 - j * pw: c0 - j * pw + ncols], k


def _mod_phase(S, nc, io, C):
    from contextlib import ExitStack
    es = ExitStack()
    cT = es.enter_context(nc.sbuf_tensor("cT_sb", [128, 32], F32))
    wa = [es.enter_context(nc.sbuf_tensor(f"wa{i}", [128, 8, 512], F32)) for i in range(3)]
    bcol = es.enter_context(nc.sbuf_tensor("bada_col", [128, 192], F32))
    msb = es.enter_context(nc.sbuf_tensor("mod_sb", [1, 2, 512], F32))
    one = es.enter_context(nc.sbuf_tensor("one_sb", [1, 1], F32))
    S.dma("sp", [], [("cT",)], lambda q: q.dma_start(out=cT[:], in_=io.cT))
    S.op("dve", [], [("one",)], lambda v: v.memset(one[:], 1.0))
    wv = io.w_ada.rearrange("l (kc p) n -> l p kc n", p=128)
    di = 0
    pcol, pck = S._ps[7], ("ps", 7)
    rot = 0
    for l in range(C.n_layers):
        S.dma("sp", [], [("bcol",)], lambda q: q.dma_start(out=bcol[:], in_=io.b_adaT[l]))
        for cb in range(48):
            ps, pk = S._ps[rot % 6], ("ps", rot % 6)
            rot += 1
            for kg in range(4):
                t = wa[di % 3]
                wk = ("wa", di % 3)
                di += 1
                S.dma("sp", [], [wk], lambda q: q.dma_start(
                    out=t[:], in_=wv[l, :, kg * 8:(kg + 1) * 8, cb * 512:(cb + 1) * 512]))

                def mm(pe, t=t, kg=kg, ps=ps):
                    ins = None
                    for k8 in range(8):
                        kc = kg * 8 + k8
                        ins = pe.matmul(ps[:1, :], cT[:, kc:kc + 1], t[:, k8, :],
                                        start=(kc == 0), stop=(kc == 31))
                    return ins
                S.op("pe", [wk, ("cT",)], [pk], mm)
            mk = ("msb", cb % 2)
            S.op("act", [pk], [mk], lambda a: a.copy(out=msb[:, cb % 2, :], in_=ps[:1, :]))

            def tr(pe, cb=cb):
                ins = None
                for j4 in range(4):
                    gc = cb * 4 + j4
                    ins = pe.matmul(pcol[:, gc:gc + 1], msb[0:1, cb % 2, j4 * 128:(j4 + 1) * 128], one[0:1, 0:1],
                                    start=True, stop=True)
                return ins
            S.op("pe", [mk, ("one",)], [pck], tr)
        S.op("dve", [pck, ("bcol",)], [("modcols",)], lambda v: v.tensor_tensor(
            out=C.modcols[:, l, :], in0=pcol[:, 0:192], in1=bcol[:], op=ALU.add))
    s_barrier(S)
    es.close()


def _mod_ap(C, l, m, kc):
    return C.modcols[:, l, m * 32 + kc: m * 32 + kc + 1]


def _make_AB(S, C, l, m_shift, m_scale, gT_d, A, B, gt):
    S.dma("sp", [("AB",)], [("gt",)], lambda q: q.dma_start(out=gt[:], in_=gT_d))
    S.op("dve", [("modcols",), ("gt",)], [("AB",)], lambda v: v.scalar_tensor_tensor(
        out=A[:], in0=C.modcols[:, l, m_scale * 32:(m_scale + 1) * 32], scalar=1.0, in1=gt[:],
        op0=ALU.add, op1=ALU.mult))
    S.op("dve", [("modcols",)], [("AB",)], lambda v: v.tensor_copy(
        out=B[:], in_=C.modcols[:, l, m_shift * 32:(m_shift + 1) * 32]))


def _make_rep(S, C, l, m, Grep, diag, key):
    for g in range(8):
        ps, pk = S.psum()
        for j in range(4):
            kc = g * 4 + j
            dg = diag[(kc) % 2]
            dk = ("diag", kc % 2)
            S.op("dve", [("modcols",), ("ident",)], [dk], lambda v, dg=dg, kc=kc: v.tensor_scalar(
                out=dg[:], in0=C.ident[:], scalar1=_mod_ap(C, l, m, kc), scalar2=None, op0=ALU.mult))
            S.op("pe", [dk, ("onesf",)], [pk], lambda pe, dg=dg, j=j, ps=ps: pe.matmul(
                ps[:, j * 128:(j + 1) * 128], C.onesf[:], dg[:], start=True, stop=True))
        S.op("act", [pk], [key], lambda a, g=g, ps=ps: a.copy(out=Grep[:, g * 512:(g + 1) * 512], in_=ps[:, :]))


P1_FB = [(0, 0, 0, True), (512, 1, 0, True), (2048, 0, 512, False), (2560, 1, 512, False)]
for _gi in range(3):
    for _hh in range(2):
        P1_FB.append((3072 + _gi * 1024 + _hh * 512, _hh, 1024 + _gi * 512, False))
        P1_FB.append((6144 + _gi * 1024 + _hh * 512, _hh, 2560 + _gi * 512, False))
for _hh in range(2):
    P1_FB.append((12288 + _hh * 512, _hh, 4096, False))
    P1_FB.append((13312 + _hh * 512, _hh, 4608, False))
P1_TB = [(1024, 0, 0, True), (1536, 1, 0, True)]
for _gi in range(3):
    for _hh in range(2):
        P1_TB.append((9216 + _gi * 1024 + _hh * 512, _hh, 512 + _gi * 512, False))
for _hh in range(2):
    P1_TB.append((14336 + _hh * 512, _hh, 2048, False))


def _phase1(S, nc, io, C, W, l, x_src, th):
    hT_d = C.hT_d[th]
    L = f"{l}_{th}"
    from contextlib import ExitStack
    es_ = ExitStack()
    hT = es_.enter_context(nc.sbuf_tensor(f"hT_{L}", [128, 32, TPC], BF16))
    wb0 = es_.enter_context(nc.sbuf_tensor(f"wb0_{L}", [128, 32, 512], BF16))
    wb1 = es_.enter_context(nc.sbuf_tensor(f"wb1_{L}", [128, 32, 512], BF16))
    big = es_.enter_context(nc.sbuf_tensor(f"big_{L}", [128, 8192], F32))
    junk = es_.enter_context(nc.sbuf_tensor(f"junk_{L}", [128, 4096], BF16))
    gsgu = es_.enter_context(nc.sbuf_tensor(f"gsgu_{L}", [128, 1024], F32))
    stF_ = es_.enter_context(nc.sbuf_tensor(f"stF_{L}", [128, 4, 512], BF16))
    gin_ = es_.enter_context(nc.sbuf_tensor(f"gin_{L}", [128, 2, 512], F32))
    gtmp_ = es_.enter_context(nc.sbuf_tensor(f"gtmp_{L}", [128, 2, 512], F32))
    with es_:
        wb = [wb0, wb1]
        xbufs = [big[:, 0:4096], big[:, 4096:8192]]
        S.dma("sp", [], [("gsgu",)], lambda q: q.dma_start(out=gsgu[:], in_=io.gsgu[l]))
        S.op("dve", [], [("ss", i) for i in range(16)], lambda v: v.memset(C.ss[:], 0.0))
        _make_AB(S, C, l, 0, 1, io.g1T[l], C.A, C.B, C.gt)
        blocks = [("F",) + b for b in P1_FB] + [("T",) + b for b in P1_TB]

        def load_w(bi):
            c0 = blocks[bi][1]
            t = wb[bi % 2]
            ap, k = _wslice(W, "w_in", l, c0, 512)
            S.dma("pool", [k], [("wb", bi % 2)], lambda q: q.dma_start(
                out=t[:], in_=ap.rearrange("(kc p) n -> p kc n", p=128)))
        load_w(0)
        load_w(1)
        _norm_to_hT(S, nc, x_src, TPC, xbufs, junk, C.ss, C.rstd, C.ident, C.A, C.B, hT, ("hT",), 0)
        hTdv = hT_d.rearrange("(kc p) t -> p kc t", p=128)
        for k4 in range(8):
            S.dma("sp", [("hT",)], [("hT_d",)], lambda q: q.dma_start(
                out=hTdv[:, k4 * 4:(k4 + 1) * 4, :], in_=hT[:, k4 * 4:(k4 + 1) * 4, :]))
        vg = big
        evi = 0
        for bi, (kind, c0, hh, o0, is_gelu) in enumerate(blocks):
            t = wb[bi % 2]
            wk = ("wb", bi % 2)
            if kind == "F":
                for j in range(4):
                    for t5 in range(2):
                        ps, pk = S.psum()
                        _mm_group(S, ps[:, :], pk,
                                  [(t[:, kc, j * 128:(j + 1) * 128], hT[:, kc, t5 * 512:(t5 + 1) * 512])
                                   for kc in range(32)], [wk, ("hT",)])
                        st = stF_[:, evi % 4, :]
                        sk = ("stF", evi % 4)
                        if is_gelu:
                            S.op("act", [pk], [sk], lambda a: a.activation(out=st, in_=ps[:, :], func=AF.Gelu_apprx_tanh))
                        elif evi % 2 == 0:
                            S.op("act", [pk], [sk], lambda a: a.copy(out=st, in_=ps[:, :]))
                        else:
                            S.op("dve", [pk], [sk], lambda v: v.tensor_copy(out=st, in_=ps[:, :]))
                        r0 = o0 + j * 128
                        S.dma("sp", [sk], [("zF_d",)], lambda q: q.dma_start(
                            out=C.zF_d[hh, th, r0:r0 + 128, t5 * 512:(t5 + 1) * 512], in_=st))
                        evi += 1
            else:
                for tt in range(8):
                    ps, pk = S.psum()
                    _mm_group(S, ps[:, :], pk,
                              [(hT[:, kc, tt * 128:(tt + 1) * 128], t[:, kc, :]) for kc in range(32)],
                              [wk, ("hT",)])
                    if is_gelu:
                        half = hh
                        vdst = vg[:, tt * 1024 + half * 512: tt * 1024 + half * 512 + 512]
                        S.op("act", [pk], [("vg", tt)], lambda a: a.activation(out=vdst, in_=ps[:, :], func=AF.Gelu_apprx_tanh))
                        if half == 1:
                            vt = vg[:, tt * 1024:(tt + 1) * 1024]
                            S.op("act", [("vg", tt)], [("junk",), ("ss", 8 + tt)],
                                 lambda a: a.activation(out=junk[:, 0:1024], in_=vt, func=AF.Square,
                                                        accum_out=C.ss[:, 8 + tt:9 + tt]))
                            _rstd(S, C.ss[:, 8 + tt:9 + tt], C.rstd[:, 8 + tt:9 + tt], 1.0 / 1024,
                                  ("ss", 8 + tt), ("rstd", 8 + tt))
                            for h2 in range(2):
                                st = stF_[:, evi % 4, :]
                                sk = ("stF", evi % 4)
                                S.op("dve", [("vg", tt), ("rstd", 8 + tt), ("gsgu",)], [sk],
                                     lambda v: v.scalar_tensor_tensor(
                                         out=st, in0=vt[:, h2 * 512:(h2 + 1) * 512],
                                         scalar=C.rstd[:, 8 + tt:9 + tt], in1=gsgu[:, h2 * 512:(h2 + 1) * 512],
                                         op0=ALU.mult, op1=ALU.mult))
                                S.dma("sp", [sk], [("zT_d",)], lambda q: q.dma_start(
                                    out=C.zT_d[h2, th, tt * 128:(tt + 1) * 128, 0:512], in_=st))
                                evi += 1
                    else:
                        st = stF_[:, evi % 4, :]
                        sk = ("stF", evi % 4)
                        if evi % 2 == 0:
                            S.op("act", [pk], [sk], lambda a: a.copy(out=st, in_=ps[:, :]))
                        else:
                            S.op("dve", [pk], [sk], lambda v: v.tensor_copy(out=st, in_=ps[:, :]))
                        S.dma("sp", [sk], [("zT_d",)], lambda q: q.dma_start(
                            out=C.zT_d[hh, th, tt * 128:(tt + 1) * 128, o0:o0 + 512], in_=st))
                        evi += 1
            if bi + 2 < len(blocks):
                load_w(bi + 2)
        s_barrier(S)


def _phase2(S, nc, io, C, l, hh):
    yT_d = C.y_d[hh]
    L = f"{l}_{hh}"
    zF_my = C.zF_d[hh]
    zT_my = C.zT_d[hh].rearrange("hf t c -> (hf t) c")
    zT5 = zT_my.rearrange("(hf n p) c -> hf p n c", hf=2, n=8, p=128)
    zT4 = zT_my.rearrange("(hf n2 j r) c -> hf r j n2 c", hf=2, n2=2, j=128, r=4)
    zT16 = zT_my.rearrange("(hf j2 r) c -> hf j2 r c", hf=2, j2=64, r=16)
    T = io.tabs
    srot = [0]

    def s_tile():
        i = srot[0] % 4
        srot[0] += 1
        return S._ps[i], ("ps", i)

    def ldF(dst, r0, key):
        for hf in range(2):
            S.dma("sp", [("zFg",)], [key], lambda q, hf=hf: q.dma_start(
                out=dst[:, hf * 1024:(hf + 1) * 1024], in_=zF_my[hf, r0:r0 + 128, :]))

    def ldV1(dst, c0, key):
        for hf in range(2):
            S.dma("sp", [("zTg",)], [key], lambda q, hf=hf: q.dma_start(
                out=dst[:, hf * 8:(hf + 1) * 8, :], in_=zT5[hf, :, :, c0:c0 + 128]))

    from contextlib import ExitStack
    es_ = ExitStack()
    fa = es_.enter_context(nc.sbuf_tensor(f"p2a_{L}", [128, 4, 2048], BF16))
    fb = es_.enter_context(nc.sbuf_tensor(f"p2b_{L}", [128, 4, 2048], BF16))
    vn = es_.enter_context(nc.sbuf_tensor(f"p2v_{L}", [128, 16, 512], BF16))
    wtmp = es_.enter_context(nc.sbuf_tensor(f"p2w_{L}", [128, 4, 128], F32))
    WcT = es_.enter_context(nc.sbuf_tensor(f"p2wc_{L}", [128, 4, 128], BF16))
    bsrep = es_.enter_context(nc.sbuf_tensor(f"p2bs_{L}", [128, 4, 512], F32))
    tmpf = es_.enter_context(nc.sbuf_tensor(f"p2t_{L}", [128, 2, 512], F32))
    Pbf = es_.enter_context(nc.sbuf_tensor(f"p2p_{L}", [128, 2, 512], BF16))
    wp = es_.enter_context(nc.sbuf_tensor(f"p2wp_{L}", [128, 2, 2, 256], BF16))
    poolP = es_.enter_context(nc.sbuf_tensor(f"p2pp_{L}", [128, 2, 3, 128], BF16))
    zW = es_.enter_context(nc.sbuf_tensor(f"p2zw_{L}", [128, 17, 2, 256], BF16))
    poolsc = es_.enter_context(nc.sbuf_tensor(f"p2ps_{L}", [128, 4], F32))
    acc = es_.enter_context(nc.sbuf_tensor(f"p2acc_{L}", [128, 2, 2048], F32))
    Vt = es_.enter_context(nc.sbuf_tensor(f"p2V_{L}", [128, 3, 16, 128], BF16))
    d0dil = es_.enter_context(nc.sbuf_tensor(f"p2d0_{L}", [128, 256], F32))
    d0dil4 = es_.enter_context(nc.sbuf_tensor(f"p2d4_{L}", [128, 512], F32))
    d0moba = es_.enter_context(nc.sbuf_tensor(f"p2dm_{L}", [128, 5, 512], F32))
    slopetab = es_.enter_context(nc.sbuf_tensor(f"p2st_{L}", [128, 16], F32))
    mobabias = es_.enter_context(nc.sbuf_tensor(f"p2mb_{L}", [128, 4 * NOFF], F32))
    validbias = es_.enter_context(nc.sbuf_tensor(f"p2vb_{L}", [128, 128], F32))
    valid01 = es_.enter_context(nc.sbuf_tensor(f"p2v1_{L}", [128, 128], F32))
    selrows = es_.enter_context(nc.sbuf_tensor(f"p2sr_{L}", [8, 8, 128], BF16))
    selT = es_.enter_context(nc.sbuf_tensor(f"p2sT_{L}", [8, 2048], BF16))
    gsb = es_.enter_context(nc.sbuf_tensor(f"p2g_{L}", [128, 4, 128], F32))
    kmf = es_.enter_context(nc.sbuf_tensor(f"p2km_{L}", [128, 4, 8], F32))
    kmhl = es_.enter_context(nc.sbuf_tensor(f"p2kh_{L}", [128, 2, 8], BF16))
    tri = es_.enter_context(nc.sbuf_tensor(f"p2tri_{L}", [128, 128], F32))
    with es_:
        for dst, nm in ((d0dil, "d0dil"), (d0dil4, "d0dil4"), (slopetab, "slopetab"), (mobabias, "mobabias"),
                        (validbias, "validbias"), (valid01, "valid01"), (selrows, "selrows"),
                        (poolP, "poolP"), (tri, "tri")):
            S.dma("sp", [], [("c", nm)], lambda q, dst=dst, nm=nm: q.dma_start(out=dst[:], in_=(T[nm][hh] if nm in HH_TABS else T[nm])))
        S.dma("sp", [], [("c", "d0moba")], lambda q: q.dma_start(
            out=d0moba[:], in_=T["d0moba"].rearrange("v p q -> p v q")))
        S.dma("sp", [], [("wtmp",)], lambda q: q.dma_start(out=wtmp[:], in_=io.sguwT[l, hh]))
        S.dma("sp", [], [("bsrep",)], lambda q: q.dma_start(out=bsrep[:], in_=io.sgub[l, hh]))
        S.dma("pool", [], [("wp",)], lambda q: q.dma_start(out=wp[:], in_=io.poolw[l, hh]))
        S.dma("sp", [], [("poolsc",)], lambda q: q.dma_start(out=poolsc[:], in_=io.poolsc[l, hh]))
        for g4 in range(4):
            S.op("dve", [("wtmp",), ("c", "tri")], [("WcT",)], lambda v, g4=g4: v.tensor_tensor(
                out=WcT[:, g4, :], in0=wtmp[:, g4, :], in1=tri[:], op=ALU.mult))

        for g4 in range(4):
            ldF(fa[:, g4, :], g4 * 128, ("fa", g4))
        for hf in range(2):
            S.dma("sp", [("zTg",)], [("vn",)], lambda q, hf=hf: q.dma_start(
                out=vn[:, hf * 8:(hf + 1) * 8, :], in_=zT5[hf, :, :, 0:512]))
        for g4 in range(4):
            for tg in range(4):
                ps, pk = S.psum()

                def mm(pe, g4=g4, tg=tg, ps=ps):
                    ins = None
                    for i in range(4):
                        ins = pe.matmul(ps[:, i * 128:(i + 1) * 128], vn[:, tg * 4 + i, g4 * 128:(g4 + 1) * 128],
                                        WcT[:, g4, :], start=True, stop=True)
                    return ins
                S.op("pe", [("vn",), ("WcT",)], [pk], mm)
                tk = ("tmpf", tg % 2)
                S.op("dve", [pk, ("bsrep",)], [tk], lambda v, g4=g4, tg=tg, ps=ps: v.tensor_tensor(
                    out=tmpf[:, tg % 2, :], in0=ps[:, :], in1=bsrep[:, g4, :], op=ALU.add))
                S.op("dve", [tk, ("fa", g4)], [("fb", g4)], lambda v, g4=g4, tg=tg: v.tensor_tensor(
                    out=fb[:, g4, tg * 512:(tg + 1) * 512], in0=tmpf[:, tg % 2, :],
                    in1=fa[:, g4, tg * 512:(tg + 1) * 512], op=ALU.mult))
            S.dma("sp", [("fb", g4)], [("yT_d",)], lambda q, g4=g4: q.dma_start(
                out=yT_d[g4 * 128:(g4 + 1) * 128, :], in_=fb[:, g4, :]))

        for i in range(4):
            ldF(fa[:, i, :], 512 + i * 128, ("fa", i))
        S.op("dve", [], [("zW", 0)], lambda v: v.memset(zW[:, 0, :, :], 0.0))
        for pg2 in range(2):
            for t2 in range(8):
                ps, pk = S.psum()

                def mm(pe, pg2=pg2, t2=t2, ps=ps):
                    ins = None
                    for i in range(2):
                        tt = t2 * 2 + i
                        for cc in range(2):
                            ins = pe.matmul(ps[:, i * 256:(i + 1) * 256],
                                            fa[:, pg2 * 2 + cc, tt * 128:(tt + 1) * 128], wp[:, pg2, cc, :],
                                            start=(cc == 0), stop=(cc == 1))
                    return ins
                S.op("pe", [("fa", pg2 * 2), ("fa", pg2 * 2 + 1), ("wp",)], [pk], mm)
                S.op("act", [pk], [("zW", 1)], lambda a, pg2=pg2, t2=t2, ps=ps: a.copy(
                    out=zW[:, 1 + t2 * 2: 3 + t2 * 2, pg2, :], in_=ps[:, :].rearrange("p (i e) -> p i e", i=2)))
        for pg2 in range(2):
            for ec in range(2):
                oc = pg2 * 2 + ec
                for tg in range(4):
                    ps, pk = S.psum()

                    def mm(pe, pg2=pg2, ec=ec, tg=tg, ps=ps):
                        ins = None
                        for i in range(4):
                            tt = tg * 4 + i
                            pe.matmul(ps[:, i * 128:(i + 1) * 128], zW[:, tt + 1, pg2, ec * 128:(ec + 1) * 128],
                                      poolP[:, pg2, (1 if tt == 0 else 0), :], start=True, stop=False)
                            ins = pe.matmul(ps[:, i * 128:(i + 1) * 128], zW[:, tt, pg2, ec * 128:(ec + 1) * 128],
                                            poolP[:, pg2, 2, :], start=False, stop=True)
                        return ins
                    S.op("pe", [("zW", 0), ("zW", 1), ("c", "poolP")], [pk], mm)
                    S.op("dve", [pk, ("poolsc",)], [("fb", oc)], lambda v, oc=oc, tg=tg, ps=ps: v.tensor_scalar(
                        out=fb[:, oc, tg * 512:(tg + 1) * 512], in0=ps[:, :], scalar1=poolsc[:, oc:oc + 1],
                        scalar2=None, op0=ALU.mult))
                S.dma("sp", [("fb", oc)], [("yT_d",)], lambda q, oc=oc: q.dma_start(
                    out=yT_d[512 + oc * 128: 512 + (oc + 1) * 128, :], in_=fb[:, oc, :]))

        accO = acc[:, 0, :]
        accD = acc[:, 1, :]
        segn = [0]

        def od_tiles():
            i = 4 + 2 * (segn[0] % 2)
            segn[0] += 1
            return (S._ps[i], ("ps", i)), (S._ps[i + 1], ("ps", i + 1))

        def softmax_pv(ps_s, sk, lo, hi, dtab, ccol, bias_ap, pv_list, extra_reads):
            flush_pv()
            pi = segn[1] % 2
            segn[1] += 1
            xk = ("tmpf", pi)
            S.op("dve", [sk, ("c", "d0dil"), ("c", "d0dil4"), ("c", "d0moba"), ("c", "slopetab")], [xk], lambda v: v.scalar_tensor_tensor(
                out=tmpf[:, pi, lo:hi], in0=dtab, scalar=slopetab[:, ccol:ccol + 1], in1=ps_s[:, lo:hi],
                op0=ALU.mult, op1=ALU.add))
            pk_ = ("Pbf", pi)
            if bias_ap is None:
                S.op("act", [xk], [pk_], lambda a: a.activation(out=Pbf[:, pi, lo:hi], in_=tmpf[:, pi, lo:hi],
                                                                func=AF.Exp, scale=SCALE))
            else:
                S.op("act", [xk, ("c", "mobabias")], [pk_], lambda a: a.activation(
                    out=Pbf[:, pi, lo:hi], in_=tmpf[:, pi, lo:hi], func=AF.Exp, scale=SCALE, bias=bias_ap))
            def emit_pv():
                for (otile, okey, ocol, ncol, pcol, lhsT, start, stop) in pv_list:
                    S.op("pe", [pk_] + extra_reads, [okey], lambda pe: pe.matmul(
                        otile[:, ocol:ocol + ncol], lhsT, Pbf[:, pi, pcol:pcol + ncol], start=start, stop=stop))
            pend.append(emit_pv)
        segn.append(0)
        pend = []

        def flush_pv():
            for f in pend:
                f()
            pend.clear()

        for h4 in range(4):
            for gi in range(3):
                ldF(fa[:, 0, :], 1024 + gi * 512 + h4 * 128, ("fa", 0))
                ldF(fa[:, 1, :], 2560 + gi * 512 + h4 * 128, ("fa", 1))
                qT = fa[:, 0, :]
                kT = fa[:, 1, :]
                c0 = 512 + gi * 512 + h4 * 128
                V = Vt[:, gi, :, :]
                vk = ("Vt", gi)
                if gi == 0:
                    ldV1(V, c0, vk)
                elif gi == 1:
                    for hf in range(2):
                        for r in range(4):
                            S.dma("sp", [("zTg",)], [vk], lambda q, hf=hf, r=r: q.dma_start(
                                out=V[:, r * 4 + hf * 2: r * 4 + hf * 2 + 2, :],
                                in_=zT4[hf, r, :, :, c0:c0 + 128]))
                else:
                    for hf in range(2):
                        S.dma("sp", [("zTg",)], [vk], lambda q, hf=hf: q.dma_start(
                            out=Vt[hf * 64:(hf + 1) * 64, gi, :, :], in_=zT16[hf, :, :, c0:c0 + 128]))
                ccol = gi * 4 + h4
                rd = [("fa", 0), ("fa", 1)]
                for seg in range(4):
                    (psO, ok), (psD, dk) = od_tiles()
                    if gi < 2:
                        d = DILS[gi]
                        a = seg if gi == 0 else 0
                        base = 0 if gi == 0 else seg
                        nlo = 4 * a - 1 if (gi == 0 and a > 0) else 4 * a

                        def tok(n, cnt):
                            if gi == 0:
                                return slice(n * 128, n * 128 + cnt)
                            return slice(512 * n + base, 512 * n + base + 4 * (cnt - 1) + 1, 4)

                        def vidx(n):
                            return n if gi == 0 else base * 4 + n
                        for n in range(nlo, 4 * a + 4):
                            cur_ok = n >= 4 * a
                            nxt_ok = n + 1 <= 4 * a + 3
                            ps_s, sk = s_tile()
                            if cur_ok and nxt_ok:
                                lo, hi = 0, 256
                                S.op("pe", rd, [sk], lambda pe: pe.matmul(
                                    ps_s[:, 0:256], kT[:, tok(n, 128)], qT[:, tok(n, 256)], start=True, stop=True))
                            elif nxt_ok:
                                lo, hi = 128, 256
                                S.op("pe", rd, [sk], lambda pe: pe.matmul(
                                    ps_s[:, 128:256], kT[:, tok(n, 128)], qT[:, tok(n + 1, 128)], start=True, stop=True))
                            else:
                                lo, hi = 0, 128
                                S.op("pe", rd, [sk], lambda pe: pe.matmul(
                                    ps_s[:, 0:128], kT[:, tok(n, 128)], qT[:, tok(n, 128)], start=True, stop=True))
                            pv = []
                            if cur_ok:
                                oc = (n - 4 * a) * 128
                                st = (n == 0)
                                pv.append((psO, ok, oc, 128, 0, V[:, vidx(n), :], st, True))
                                pv.append((psD, dk, oc, 128, 0, C.ones_bf[:], st, True))
                            if nxt_ok:
                                oc = (n + 1 - 4 * a) * 128
                                pv.append((psO, ok, oc, 128, 128, V[:, vidx(n), :], True, False))
                                pv.append((psD, dk, oc, 128, 128, C.ones_bf[:], True, False))
                            softmax_pv(ps_s, sk, lo, hi, d0dil[:, lo:hi], ccol, None, pv, [vk, ("ones_bf",)])
                    else:
                        ps_s, sk = s_tile()

                        def mm(pe, seg=seg, ps_s=ps_s):
                            ins = None
                            for i in range(4):
                                r = seg * 4 + i
                                sl = slice(r, r + 16 * 127 + 1, 16)
                                ins = pe.matmul(ps_s[:, i * 128:(i + 1) * 128], kT[:, sl], qT[:, sl],
                                                start=True, stop=True)
                            return ins
                        S.op("pe", rd, [sk], mm)
                        pv = []
                        for i in range(4):
                            r = seg * 4 + i
                            pv.append((psO, ok, i * 128, 128, i * 128, V[:, r, :], True, True))
                            pv.append((psD, dk, i * 128, 128, i * 128, C.ones_bf[:], True, True))
                        softmax_pv(ps_s, sk, 0, 512, d0dil4[:, :], ccol, None, pv, [vk, ("ones_bf",)])
                    flush_pv()
                    if gi == 0:
                        S.op("act", [ok], [("accO",)], lambda a_: a_.copy(out=accO[:, seg * 512:(seg + 1) * 512], in_=psO[:, :]))
                        S.op("dve", [dk], [("accD",)], lambda v: v.tensor_copy(out=accD[:, seg * 512:(seg + 1) * 512], in_=psD[:, :]))
                    elif gi == 1:
                        sl = slice(seg, 2048, 4)
                        S.op("dve", [ok], [("accO",)], lambda v: v.tensor_tensor(out=accO[:, sl], in0=accO[:, sl], in1=psO[:, :], op=ALU.add))
                        S.op("dve", [dk], [("accD",)], lambda v: v.tensor_tensor(out=accD[:, sl], in0=accD[:, sl], in1=psD[:, :], op=ALU.add))
                    else:
                        for (at, ak, pt, pk2) in ((accO, ("accO",), psO, ok), (accD, ("accD",), psD, dk)):
                            av = at.rearrange("p (j r) -> p r j", r=16)[:, seg * 4:(seg + 1) * 4, :]
                            pv_ = pt[:, :].rearrange("p (i j) -> p i j", i=4)
                            S.op("dve", [pk2], [ak], lambda v, av=av, pv_=pv_: v.tensor_tensor(out=av, in0=av, in1=pv_, op=ALU.add))
            S.op("dve", [("accD",)], [("accD",)], lambda v: v.reciprocal(out=accD, in_=accD))
            S.op("dve", [("accD",), ("accO",)], [("fb", h4)], lambda v: v.tensor_tensor(
                out=fb[:, h4, :], in0=accO, in1=accD, op=ALU.mult))
            S.dma("sp", [("fb", h4)], [("yT_d",)], lambda q: q.dma_start(
                out=yT_d[1024 + h4 * 128: 1024 + (h4 + 1) * 128, :], in_=fb[:, h4, :]))

        gm = gsb[:, 0, :]
        top8 = gsb[:, 1, :]
        sel = gsb[:, 2, :]
        for h4 in range(4):
            ldF(fa[:, 2, :], 4096 + h4 * 128, ("fa", 2))
            ldF(fa[:, 3, :], 4608 + h4 * 128, ("fa", 3))
            qT = fa[:, 2, :]
            kT = fa[:, 3, :]
            V = Vt[:, 0, :, :]
            vk = ("Vt", 0)
            ldV1(V, 2048 + h4 * 128, vk)
            rd = [("fa", 2), ("fa", 3)]
            S.op("dve", [("fa", 3)], [("kmf",)], lambda v: v.reduce_sum(
                out=kmf[:, 0, :], in_=kT.rearrange("p (n t) -> p n t", n=8), axis=AX.X))
            S.op("dve", [("kmf",)], [("kmf",)], lambda v: v.tensor_scalar(
                out=kmf[:, 0, :], in0=kmf[:, 0, :], scalar1=1.0 / 256, scalar2=None, op0=ALU.mult))
            S.op("dve", [("kmf",)], [("kmhl",)], lambda v: v.tensor_copy(out=kmhl[:, 0, :], in_=kmf[:, 0, :]))
            S.op("dve", [("kmhl",)], [("kmf",)], lambda v: v.tensor_copy(out=kmf[:, 1, :], in_=kmhl[:, 0, :]))
            S.op("dve", [("kmf",)], [("kmf",)], lambda v: v.tensor_tensor(
                out=kmf[:, 2, :], in0=kmf[:, 0, :], in1=kmf[:, 1, :], op=ALU.subtract))
            S.op("dve", [("kmf",)], [("kmhl",)], lambda v: v.tensor_copy(out=kmhl[:, 1, :], in_=kmf[:, 2, :]))
            psg, gk = s_tile()

            def gate(pe, psg=psg):
                ins = None
                for tt in range(16):
                    pe.matmul(psg[:, tt * 8:(tt + 1) * 8], qT[:, tt * 128:(tt + 1) * 128], kmhl[:, 0, :],
                              start=True, stop=False)
                    ins = pe.matmul(psg[:, tt * 8:(tt + 1) * 8], qT[:, tt * 128:(tt + 1) * 128], kmhl[:, 1, :],
                                    start=False, stop=True)
                return ins
            S.op("pe", rd + [("kmhl",)], [gk], gate)
            S.op("dve", [gk, ("c", "validbias")], [("gm",)], lambda v: v.tensor_tensor(
                out=gm, in0=psg[:, 0:128], in1=validbias[:], op=ALU.add))
            for tt in range(16):
                S.op("dve", [("gm",)], [("top8",)], lambda v, tt=tt: v.max(
                    out=top8[:, tt * 8:(tt + 1) * 8], in_=gm[:, tt * 8:(tt + 1) * 8]))
            for tt in range(16):
                S.op("dve", [("gm",), ("top8",)], [("sel",)], lambda v, tt=tt: v.tensor_scalar(
                    out=sel[:, tt * 8:(tt + 1) * 8], in0=gm[:, tt * 8:(tt + 1) * 8],
                    scalar1=top8[:, tt * 8 + 2: tt * 8 + 3], scalar2=None, op0=ALU.is_ge))
            S.op("dve", [("sel",)], [("sel",)], lambda v: v.tensor_scalar(
                out=sel, in0=sel, scalar1=BIG, scalar2=-BIG, op0=ALU.mult, op1=ALU.add))
            S.op("dve", [("sel",), ("c", "valid01")], [("sel",)], lambda v: v.tensor_tensor(
                out=sel, in0=sel, in1=valid01[:], op=ALU.mult))
            for g in range(4):
                pst, tk = s_tile()

                def trs(pe, g=g, pst=pst):
                    ins = None
                    for i in range(4):
                        tt = g * 4 + i
                        ins = pe.transpose(out=pst[:8, i * 128:(i + 1) * 128], in_=sel[:, tt * 8:(tt + 1) * 8],
                                           identity=C.ident[:, :])
                    return ins
                S.op("pe", [("sel",), ("ident",)], [tk], trs)
                S.op("act", [tk], [("selT",)], lambda a, g=g, pst=pst: a.copy(
                    out=selT[:8, g * 512:(g + 1) * 512], in_=pst[:8, :]))
            for qr in range(4):
                (psO, ok), (psD, dk) = od_tiles()
                nks = 4 * qr + 4
                for ks in range(nks):
                    ps_s, sk = s_tile()

                    def mm(pe, ks=ks, qr=qr, ps_s=ps_s):
                        pe.matmul(ps_s[:, :], kT[:, ks * 128:(ks + 1) * 128], qT[:, qr * 512:(qr + 1) * 512],
                                  start=True, stop=False)
                        return pe.matmul(ps_s[:, :], selrows[:8, ks // 2, :], selT[:8, qr * 512:(qr + 1) * 512],
                                         start=False, stop=True)
                    S.op("pe", rd + [("selT",), ("c", "selrows")], [sk], mm)
                    j = ks - 4 * qr
                    dv = 0 if j < 0 else 1 + j
                    oi = (4 * qr - ks) + 3
                    bias_ap = mobabias[:, h4 * NOFF + oi: h4 * NOFF + oi + 1]
                    pv = [(psO, ok, 0, 512, 0, V[:, ks, :], ks == 0, ks == nks - 1),
                          (psD, dk, 0, 512, 0, C.ones_bf[:], ks == 0, ks == nks - 1)]
                    softmax_pv(ps_s, sk, 0, 512, d0moba[:, dv, :], 12 + h4, bias_ap, pv, [vk, ("ones_bf",)])
                flush_pv()
                rk = ("tmpf", qr % 2)
                S.op("dve", [dk], [rk], lambda v: v.reciprocal(out=tmpf[:, qr % 2, :], in_=psD[:, :]))
                S.op("dve", [rk, ok], [("fb", h4)], lambda v: v.tensor_tensor(
                    out=fb[:, h4, qr * 512:(qr + 1) * 512], in0=psO[:, :], in1=tmpf[:, qr % 2, :], op=ALU.mult))
            S.dma("sp", [("fb", h4)], [("yT_d",)], lambda q: q.dma_start(
                out=yT_d[1536 + h4 * 128: 1536 + (h4 + 1) * 128, :], in_=fb[:, h4, :]))
        s_barrier(S)


def _rep_piece(S, C, l, m, eb, dst, dkey):
    ps, pk = S.psum()
    for j in range(4):
        kc = eb * 4 + j
        dg = C.diag[kc % 2]
        dk = ("diag", kc % 2)
        S.op("dve", [("modcols",), ("ident",)], [dk], lambda v: v.tensor_scalar(
            out=dg[:], in0=C.ident[:], scalar1=_mod_ap(C, l, m, kc), scalar2=None, op0=ALU.mult))
        S.op("pe", [dk, ("onesf",)], [pk], lambda pe: pe.matmul(
            ps[:, j * 128:(j + 1) * 128], C.onesf[:], dg[:], start=True, stop=True))
    S.op("act", [pk], [dkey], lambda a: a.copy(out=dst, in_=ps[:, :]))


def _phase3a(S, nc, io, C, W, l, th):
    mT_d = C.mT_d[th]
    L = f"{l}_{th}"
    from contextlib import ExitStack
    es_ = ExitStack()
    hT = es_.enter_context(nc.sbuf_tensor(f"p3h_{L}", [128, 32, TPC], BF16))
    yT = es_.enter_context(nc.sbuf_tensor(f"p3y_{L}", [128, 32, TPC], BF16))
    mwb = es_.enter_context(nc.sbuf_tensor(f"p3mw_{L}", [128, 2, 32, 256], BF16))
    bwb = es_.enter_context(nc.sbuf_tensor(f"p3bw_{L}", [128, 2, 8, 256], BF16))
    macc = es_.enter_context(nc.sbuf_tensor(f"p3ma_{L}", [128, 4, 512], F32))
    gsig = es_.enter_context(nc.sbuf_tensor(f"p3gs_{L}", [128, 2, 512], F32))
    tmp = es_.enter_context(nc.sbuf_tensor(f"p3tm_{L}", [128, 512], F32))
    stg = es_.enter_context(nc.sbuf_tensor(f"p3st_{L}", [128, 2, 512], BF16))
    mb = es_.enter_context(nc.sbuf_tensor(f"p3mb_{L}", [128, 4, 32], F32))
    with es_:
        S.dma("sp", [], [("mb",)], lambda q: q.dma_start(out=mb[:], in_=io.mbT[l]))
        hv = C.hT_d[th].rearrange("(kc p) t -> p kc t", p=128)
        for k4 in range(8):
            S.dma("sp", [("hT_d",)], [("hT",)], lambda q: q.dma_start(
                out=hT[:, k4 * 4:(k4 + 1) * 4, :], in_=hv[:, k4 * 4:(k4 + 1) * 4, :]))
        for br in range(4):
            for hs in range(2):
                S.dma("sp", [("yT_d",)], [("yT",)], lambda q: q.dma_start(
                    out=yT[:, br * 8 + hs * 4: br * 8 + hs * 4 + 4, :],
                    in_=C.y_d[hs, br * 512:(br + 1) * 512, th * TPC:(th + 1) * TPC].rearrange("(k p) t -> p k t", p=128)))
        wi = 0
        gi_ = 0
        for eg in range(16):
            for br in range(4):
                mt = mwb[:, wi % 2, :, :]
                bt = bwb[:, wi % 2, :, :]
                mk = ("mwb", wi % 2)
                bk = ("bwb", wi % 2)
                wi += 1
                ap, k = _wslice(W, "merge_w", l, br * 4096 + eg * 256, 256)
                S.dma("pool", [k], [mk], lambda q: q.dma_start(out=mt, in_=ap.rearrange("(kc p) n -> p kc n", p=128)))
                ap2, k2 = _wslice(W, "branch_w", l, eg * 256, 256)
                S.dma("pool", [k2], [bk], lambda q: q.dma_start(
                    out=bt, in_=ap2[br * 1024:(br + 1) * 1024, :].rearrange("(kc p) n -> p kc n", p=128)))
                for e2 in range(2):
                    ec = eg * 2 + e2
                    for t5 in range(2):
                        ai = e2 * 2 + t5
                        psg, gk = S.psum()
                        _mm_group(S, psg[:, :], gk, [(mt[:, kc, e2 * 128:(e2 + 1) * 128], hT[:, kc, t5 * 512:(t5 + 1) * 512])
                                                     for kc in range(32)], [mk, ("hT",)])
                        gs = gsig[:, gi_ % 2, :]
                        gsk = ("gsig", gi_ % 2)
                        gi_ += 1
                        S.op("act", [gk, ("mb",)], [gsk], lambda a: a.activation(
                            out=gs, in_=psg[:, :], func=AF.Sigmoid, bias=mb[:, br, ec:ec + 1]))
                        psp, ppk = S.psum()
                        _mm_group(S, psp[:, :], ppk, [(bt[:, k8, e2 * 128:(e2 + 1) * 128], yT[:, br * 8 + k8, t5 * 512:(t5 + 1) * 512])
                                                      for k8 in range(8)], [bk, ("yT",)])
                        if br == 0:
                            S.op("dve", [gsk, ppk], [("macc", ai)], lambda v: v.tensor_tensor(
                                out=macc[:, ai, :], in0=gs, in1=psp[:, :], op=ALU.mult))
                        else:
                            S.op("dve", [gsk, ppk], [("tmp3",)], lambda v: v.tensor_tensor(
                                out=tmp[:], in0=gs, in1=psp[:, :], op=ALU.mult))
                            S.op("dve", [("tmp3",), ("macc", ai)], [("macc", ai)], lambda v: v.tensor_tensor(
                                out=macc[:, ai, :], in0=macc[:, ai, :], in1=tmp[:], op=ALU.add))
                        if br == 3:
                            st = stg[:, ai % 2, :]
                            sk = ("stg", ai % 2)
                            S.op("act", [("macc", ai)], [sk], lambda a: a.copy(out=st, in_=macc[:, ai, :]))
                            S.dma("sp", [sk], [("mT_d",)], lambda q: q.dma_start(
                                out=mT_d[ec * 128:(ec + 1) * 128, t5 * 512:(t5 + 1) * 512], in_=st))
        s_barrier(S)


def _epilogue_tile(S, C, ps, pk, grep, gkey, x_ap, out_ap, okey, xt, ot, ei):
    xk = ("xt", ei % 2)
    ok_ = ("ot", ei % 2)
    x_ = xt[:, ei % 2, :]
    o_ = ot[:, ei % 2, :]
    S.dma("sp", [("xsrc",)], [xk], lambda q: q.dma_start(out=x_, in_=x_ap))
    S.op("dve", [pk, gkey], [ok_], lambda v: v.tensor_tensor(out=o_, in0=ps[:, :], in1=grep, op=ALU.mult))
    S.op("dve", [ok_, xk], [ok_], lambda v: v.tensor_tensor(out=o_, in0=o_, in1=x_, op=ALU.add))
    S.dma("sp", [ok_], [okey], lambda q: q.dma_start(out=out_ap, in_=o_))


def _phase3b(S, nc, io, C, W, l, x_src, x_dst, th):
    xmid_d = C.xmid_d[th * TPC:(th + 1) * TPC, :]
    L = f"{l}_{th}"
    h2_d = nc.dram_tensor(f"h2T{L}", [D, TPC + 2], BF16).ap()
    mT_d = C.mT_d[th]
    if th == 0:
        wd_src = io.ffn_wd[l]
        C.wd_bf = nc.dram_tensor(f"wdbf{l}", [DFF, D], BF16).ap()
        C.wd_keys = [("wdbf", l, bi) for bi in range(16)]
        for bi in range(16):
            S.dma("pool", [], [C.wd_keys[bi]], lambda q: q.dma_start(
                out=C.wd_bf[bi * 688:(bi + 1) * 688, :], in_=wd_src[bi * 688:(bi + 1) * 688, :]))
    with (nc.sbuf_tensor(f"p4m_{L}", [128, 32, TPC], BF16) as mT,
          nc.sbuf_tensor(f"p4w_{L}", [128, 2, 32, 512], BF16) as owb,
          nc.sbuf_tensor(f"p4g_{L}", [128, 2, 512], F32) as grep,
          nc.sbuf_tensor(f"p4x_{L}", [128, 2, 512], F32) as xt,
          nc.sbuf_tensor(f"p4o_{L}", [128, 2, 512], F32) as ot):
        mv = mT_d.rearrange("(kc p) t -> p kc t", p=128)
        for k4 in range(8):
            S.dma("sp", [("mT_d",)], [("mT",)], lambda q: q.dma_start(
                out=mT[:, k4 * 4:(k4 + 1) * 4, :], in_=mv[:, k4 * 4:(k4 + 1) * 4, :]))
        ei = 0
        for eb in range(8):
            wt = owb[:, eb % 2, :, :]
            wk = ("owb", eb % 2)
            ap, k = _wslice(W, "out_w", l, eb * 512, 512)
            S.dma("pool", [k], [wk], lambda q: q.dma_start(out=wt, in_=ap.rearrange("(kc p) n -> p kc n", p=128)))
            gr = grep[:, eb % 2, :]
            gk = ("grep", eb % 2)
            _rep_piece(S, C, l, 2, eb, gr, gk)
            for tt in range(8):
                ps, pk = S.psum()
                _mm_group(S, ps[:, :], pk, [(mT[:, kc, tt * 128:(tt + 1) * 128], wt[:, kc, :]) for kc in range(32)],
                          [wk, ("mT",)])
                _epilogue_tile(S, C, ps, pk, gr, gk, x_src[tt * 128:(tt + 1) * 128, eb * 512:(eb + 1) * 512],
                               xmid_d[tt * 128:(tt + 1) * 128, eb * 512:(eb + 1) * 512], ("xmid_d",), xt, ot, ei)
                ei += 1
        s_barrier(S)
    with (nc.sbuf_tensor(f"p5h_{L}", [128, 32, TPC + 2], BF16) as h2T,
          nc.sbuf_tensor(f"p5x_{L}", [128, 8192], F32) as big,
          nc.sbuf_tensor(f"p5j_{L}", [128, 4096], BF16) as junk):
        xbufs = [big[:, 0:4096], big[:, 4096:8192]]
        _make_AB(S, C, l, 3, 4, io.g2T[l], C.A, C.B, C.gt)
        S.op("dve", [], [("ss", i) for i in range(16)], lambda v: v.memset(C.ss[:], 0.0))
        _norm_to_hT(S, nc, xmid_d, TPC, xbufs, junk, C.ss, C.rstd, C.ident, C.A, C.B, h2T, ("h2T",), 2)
        S.op("dve", [("h2T",)], [("h2T",)], lambda v: v.memset(h2T[:, :, 0:2], 0.0))
        hv = h2_d.rearrange("(kc p) t -> p kc t", p=128)
        for k4 in range(8):
            S.dma("sp", [("h2T",)], [("h2_d",)], lambda q: q.dma_start(
                out=hv[:, k4 * 4:(k4 + 1) * 4, :], in_=h2T[:, k4 * 4:(k4 + 1) * 4, :]))
        s_barrier(S)
    fT_d = nc.dram_tensor(f"fT{L}", [DFF, TPC], BF16).ap()
    from contextlib import ExitStack
    es_ = ExitStack()
    h2h = es_.enter_context(nc.sbuf_tensor(f"p6h_{L}", [128, 32, TPC + 2], BF16))
    wgb = es_.enter_context(nc.sbuf_tensor(f"p6wg_{L}", [128, 2, 32, 128], BF16))
    wub = es_.enter_context(nc.sbuf_tensor(f"p6wu_{L}", [128, 2, 32, 128], BF16))
    gsb = es_.enter_context(nc.sbuf_tensor(f"p6gs_{L}", [128, 2, 514], F32))
    ab = es_.enter_context(nc.sbuf_tensor(f"p6a_{L}", [128, 2, 512], F32))
    fst = es_.enter_context(nc.sbuf_tensor(f"p6fs_{L}", [128, 4, 512], BF16))
    cw = es_.enter_context(nc.sbuf_tensor(f"p6cw_{L}", [128, 3, NFC], F32))
    cb = es_.enter_context(nc.sbuf_tensor(f"p6cb_{L}", [128, NFC], F32))
    with es_:
        S.dma("sp", [], [("cw",)], lambda q: q.dma_start(out=cw[:], in_=io.convw[l]))
        S.dma("sp", [], [("cb",)], lambda q: q.dma_start(out=cb[:], in_=io.convb[l]))
        hv = h2_d.rearrange("(kc p) t -> p kc t", p=128)
        for k4 in range(8):
            S.dma("sp", [("h2_d",)], [("h2h",)], lambda q: q.dma_start(
                out=h2h[:, k4 * 4:(k4 + 1) * 4, :], in_=hv[:, k4 * 4:(k4 + 1) * 4, :]))
        it = 0
        for fc in range(NFC):
            wg = wgb[:, fc % 2, :, :]
            wu = wub[:, fc % 2, :, :]
            gk_, uk_ = ("wgb", fc % 2), ("wub", fc % 2)
            ap, k = _wslice(W, "ffn_wg", l, fc * 128, 128)
            S.dma("pool", [k], [gk_], lambda q: q.dma_start(out=wg, in_=ap.rearrange("(kc p) n -> p kc n", p=128)))
            ap, k = _wslice(W, "ffn_wu", l, fc * 128, 128)
            S.dma("pool", [k], [uk_], lambda q: q.dma_start(out=wu, in_=ap.rearrange("(kc p) n -> p kc n", p=128)))
            for t2 in range(2):
                c0 = 2 + t2 * 512
                psg, pgk = S.psum()
                _mm_group(S, psg[:, :], pgk, [(wg[:, kc, :], h2h[:, kc, c0:c0 + 512]) for kc in range(32)], [gk_, ("h2h",)])
                psu, puk = S.psum()
                _mm_group(S, psu[:, :], puk, [(wu[:, kc, :], h2h[:, kc, c0:c0 + 512]) for kc in range(32)], [uk_, ("h2h",)])
                gs = gsb[:, it % 2, :]
                gsk = ("gsb", it % 2)
                a_ = ab[:, it % 2, :]
                ak = ("ab", it % 2)
                fs = fst[:, it % 4, :]
                fk = ("fst", it % 4)
                it += 1
                S.op("act", [pgk], [gsk], lambda a: a.copy(out=gs[:, 2:514], in_=psg[:, :]))
                S.op("dve", [("glast",)], [gsk], lambda v: v.tensor_copy(out=gs[:, 0:2], in_=C.glast[:, fc, :]))
                S.op("dve", [gsk], [("glast",)], lambda v: v.tensor_copy(out=C.glast[:, fc, :], in_=gs[:, 512:514]))
                S.op("dve", [gsk, ("cw",), ("cb",)], [ak], lambda v: v.tensor_scalar(
                    out=a_, in0=gs[:, 2:514], scalar1=cw[:, 2, fc:fc + 1], scalar2=cb[:, fc:fc + 1],
                    op0=ALU.mult, op1=ALU.add))
                S.op("dve", [gsk, ak], [ak], lambda v: v.scalar_tensor_tensor(
                    out=a_, in0=gs[:, 1:513], scalar=cw[:, 1, fc:fc + 1], in1=a_, op0=ALU.mult, op1=ALU.add))
                S.op("dve", [gsk, ak], [ak], lambda v: v.scalar_tensor_tensor(
                    out=a_, in0=gs[:, 0:512], scalar=cw[:, 0, fc:fc + 1], in1=a_, op0=ALU.mult, op1=ALU.add))
                S.op("act", [ak], [ak], lambda a: a.activation(out=a_, in_=a_, func=AF.Gelu_apprx_tanh))
                S.op("dve", [ak, puk], [fk], lambda v: v.tensor_tensor(out=fs, in0=a_, in1=psu[:, :], op=ALU.mult))
                S.dma("sp", [fk], [("fT_d",)], lambda q: q.dma_start(
                    out=fT_d[fc * 128:(fc + 1) * 128, t2 * 512:(t2 + 1) * 512], in_=fs))
        s_barrier(S)
    es_ = ExitStack()
    fT = es_.enter_context(nc.sbuf_tensor(f"p7f_{L}", [128, NFC, 512], BF16))
    wdb = es_.enter_context(nc.sbuf_tensor(f"p7wd_{L}", [128, 2, 4, 512], BF16))
    grep = es_.enter_context(nc.sbuf_tensor(f"p7g_{L}", [128, 2, 512], F32))
    xt = es_.enter_context(nc.sbuf_tensor(f"p7x_{L}", [128, 2, 512], F32))
    ot = es_.enter_context(nc.sbuf_tensor(f"p7o_{L}", [128, 2, 512], F32))
    with es_:
        ei = 0
        wdi = 0
        fv = fT_d.rearrange("(fc p) t -> p fc t", p=128)
        for t2 in range(2):
            for f4 in range(0, NFC, 8):
                n8 = min(8, NFC - f4)
                S.dma("sp", [("fT_d",)], [("fT",)], lambda q: q.dma_start(
                    out=fT[:, f4:f4 + n8, :], in_=fv[:, f4:f4 + n8, t2 * 512:(t2 + 1) * 512]))
            for eb in range(8):
                banks = [(S._ps[(eb % 2) * 4 + i], ("ps", (eb % 2) * 4 + i)) for i in range(4)]
                gr = grep[:, eb % 2, :]
                gk = ("grep", eb % 2)
                _rep_piece_fixed(S, C, l, 5, eb, gr, gk, banks)
                for fg in range(22):
                    nf = 4 if fg < 21 else 2
                    wd = wdb[:, wdi % 2, :, :]
                    wk = ("wdb", wdi % 2)
                    wdi += 1
                    S.dma("sp", C.wd_keys, [wk], lambda q: q.dma_start(
                        out=wd[:, 0:nf, :],
                        in_=C.wd_bf[fg * 512: fg * 512 + nf * 128, eb * 512:(eb + 1) * 512].rearrange("(k p) n -> p k n", p=128)))

                    def mm(pe, fg=fg, nf=nf, wd=wd, banks=banks):
                        ins = None
                        for k_ in range(nf):
                            fc = fg * 4 + k_
                            for t4 in range(4):
                                ins = pe.matmul(banks[t4][0][:, :], fT[:, fc, t4 * 128:(t4 + 1) * 128], wd[:, k_, :],
                                                start=(fc == 0), stop=(fc == NFC - 1))
                        return ins
                    S.op("pe", [wk, ("fT",)], [b[1] for b in banks], mm)
                for t4 in range(4):
                    r0 = t2 * 512 + t4 * 128
                    _epilogue_tile(S, C, banks[t4][0], banks[t4][1], gr, gk,
                                   xmid_d[r0:r0 + 128, eb * 512:(eb + 1) * 512],
                                   x_dst[r0:r0 + 128, eb * 512:(eb + 1) * 512], ("x_dst",), xt, ot, ei)
                    ei += 1
        s_barrier(S)


def _rep_piece_fixed(S, C, l, m, eb, dst, dkey, banks):
    ps, pk = banks[0]
    for j in range(4):
        kc = eb * 4 + j
        dg = C.diag[kc % 2]
        dk = ("diag", kc % 2)
        S.op("dve", [("modcols",), ("ident",)], [dk], lambda v: v.tensor_scalar(
            out=dg[:], in0=C.ident[:], scalar1=_mod_ap(C, l, m, kc), scalar2=None, op0=ALU.mult))
        S.op("pe", [dk, ("onesf",)], [pk], lambda pe: pe.matmul(
            ps[:, j * 128:(j + 1) * 128], C.onesf[:], dg[:], start=True, stop=True))
    S.op("act", [pk], [dkey], lambda a: a.copy(out=dst, in_=ps[:, :]))


def _final_norm(S, nc, io, C, x_src):
    with (nc.sbuf_tensor("pfx", [128, 2, 4096], F32) as xb,
          nc.sbuf_tensor("pfj", [128, 4096], BF16) as junk,
          nc.sbuf_tensor("pfg", [128, 4096], F32) as gf):
        S.dma("sp", [], [("gf",)], lambda q: q.dma_start(out=gf[:], in_=io.gfin))
        S.op("dve", [], [("ss", i) for i in range(16)], lambda v: v.memset(C.ss[:], 0.0))
        for tt in range(16):
            xt = xb[:, tt % 2, :]
            xk = ("xb", tt % 2)
            S.dma("sp", [("x_dst",)], [xk], lambda q: q.dma_start(out=xt, in_=x_src[tt * 128:(tt + 1) * 128, :]))
            S.op("act", [xk], [("junk",), ("ss", tt)], lambda a: a.activation(
                out=junk[:], in_=xt, func=AF.Square, accum_out=C.ss[:, tt:tt + 1]))
            _rstd(S, C.ss[:, tt:tt + 1], C.rstd[:, tt:tt + 1], 1.0 / D, ("ss", tt), ("rstd", tt))
            S.op("dve", [xk, ("rstd", tt), ("gf",)], [xk], lambda v: v.scalar_tensor_tensor(
                out=xt, in0=xt, scalar=C.rstd[:, tt:tt + 1], in1=gf[:], op0=ALU.mult, op1=ALU.mult))
            S.dma("sp", [xk], [("out",)], lambda q: q.dma_start(out=io.out[tt * 128:(tt + 1) * 128, :], in_=xt))


NCORES = 4


def build_fused(debug=False, n_layers=2, fake=False, stop_after=None):
    nc = bass.Bass("TRN2", target_bir_lowering=False)
    io = _declare_io(nc, fake, n_layers)
    dbg = {}
    if debug:
        for nm, shp, dt in (("mod", [128, 384], F32), ("p1", [4 * 5120, TPC], BF16), ("p1t", [4 * TPC, 2560], BF16),
                            ("p2", [2 * 2048, S_LEN], BF16),
                            ("p3a", [2 * D, TPC], BF16), ("p3b", [S_LEN, D], F32)):
            dbg[nm] = nc.dram_tensor("dbg_" + nm, shp, dt, kind="ExternalOutput").ap()
    S = Sched(nc)
    _sched_ext(S)
    S.psum_init()
    C = Ctx()
    C.n_layers = n_layers
    C.ident = nc.alloc_sbuf_tensor("ident_sb", [128, 128], F32)
    C.onesf = nc.alloc_sbuf_tensor("onesf", [128, 128], F32)
    C.ones_bf = nc.alloc_sbuf_tensor("ones_bf", [128, 128], BF16)
    C.modcols = nc.alloc_sbuf_tensor("modcols", [128, 2, 192], F32)
    C.A = nc.alloc_sbuf_tensor("A", [128, 32], F32)
    C.B = nc.alloc_sbuf_tensor("B", [128, 32], F32)
    C.gt = nc.alloc_sbuf_tensor("gt", [128, 32], F32)
    C.ss = nc.alloc_sbuf_tensor("ss", [128, 16], F32)
    C.rstd = nc.alloc_sbuf_tensor("rstd", [128, 16], F32)
    C.diag = [nc.alloc_sbuf_tensor(f"diag{i}", [128, 128], F32) for i in range(2)]
    C.glast = nc.alloc_sbuf_tensor("glast", [128, NFC, 2], F32)
    S.dma("sp", [], [("ident",)], lambda q: q.dma_start(out=C.ident[:], in_=io.tabs["ident"]))
    S.dma("sp", [], [("ones_bf",)], lambda q: q.dma_start(out=C.ones_bf[:], in_=io.tabs["ones_bf"]))
    S.op("dve", [], [("onesf",)], lambda v: v.memset(C.onesf[:], 1.0))
    W = {}
    x1_d = nc.dram_tensor("x1_d", [S_LEN, D], F32).ap()
    x2_d = nc.dram_tensor("x2_d", [S_LEN, D], F32).ap()
    C.xmid_d = nc.dram_tensor("xmid_d", [S_LEN, D], F32).ap()
    C.hT_d = nc.dram_tensor("hT_d", [2, D, TPC], BF16).ap()
    C.mT_d = nc.dram_tensor("mT_d", [2, D, TPC], BF16).ap()
    C.zF_d = nc.dram_tensor("zF_d", [2, 2, 5120, TPC], BF16).ap()
    C.zT_d = nc.dram_tensor("zT_d", [2, 2, TPC, 2560], BF16).ap()
    C.y_d = nc.dram_tensor("y_d", [2, 2048, S_LEN], BF16).ap()

    def dump(nm, src):
        if debug:
            S.dma("sp", [], [("dbg", nm)], lambda q: q.dma_start(out=dbg[nm], in_=src))

    def stop():
        S.dma("sp", [], [("out",)], lambda q: q.dma_start(out=io.out[0:128, 0:128], in_=io.x[0:128, 0:128]))
        S.finish("sp")
        return nc
    _mod_phase(S, nc, io, C)
    if stop_after == "mod":
        return stop()
    if debug:
        S.dma("sp", [("modcols",)], [("dbg", "mod")], lambda q: q.dma_start(
            out=dbg["mod"], in_=C.modcols[:].rearrange("p a b -> p (a b)")))
    x_src = io.x
    for l in range(n_layers):
        _weights_phase(S, nc, io, l, W)
        S.op("dve", [("glast",)], [("glast",)], lambda v: v.memset(C.glast[:], 0.0))
        x_dst = x1_d if l == 0 else x2_d
        for th in range(2):
            _phase1(S, nc, io, C, W, l, x_src[th * TPC:(th + 1) * TPC, :], th)
        if stop_after == "p1":
            return stop()
        dump("p1", C.zF_d.rearrange("a b r t -> (a b r) t"))
        dump("p1t", C.zT_d.rearrange("a b t c -> (a b t) c"))
        for hh in range(2):
            _phase2(S, nc, io, C, l, hh)
        if stop_after == "p2":
            return stop()
        dump("p2", C.y_d.rearrange("a r t -> (a r) t"))
        for th in range(2):
            _phase3a(S, nc, io, C, W, l, th)
        if stop_after == "p3a":
            return stop()
        dump("p3a", C.mT_d.rearrange("a r t -> (a r) t"))
        for th in range(2):
            _phase3b(S, nc, io, C, W, l, x_src[th * TPC:(th + 1) * TPC, :], x_dst[th * TPC:(th + 1) * TPC, :], th)
        if stop_after == "p3b":
            return stop()
        dump("p3b", x_dst)
        x_src = x_dst
    _final_norm(S, nc, io, C, x_src)
    S.finish("sp")
    print("instr counts", S.ccnt, flush=True)
    return nc


def _prep_inputs(inp, NL=2):
    ca = np.ascontiguousarray
    x = inp["x"]
    g1T = ca(inp["norm1_g"].reshape(2, 32, 128).transpose(0, 2, 1))
    g2T = ca(inp["norm2_g"].reshape(2, 32, 128).transpose(0, 2, 1))
    gfin = ca(np.broadcast_to(inp["final_g"][None, :], (128, D)))
    gsgu = ca(np.broadcast_to(inp["sgu_norm_g"][:, None, :], (2, 128, 1024)))
    mbT = ca(inp["merge_b"].reshape(2, 4, 32, 128).transpose(0, 3, 1, 2))
    convw = ca(inp["conv_w"].reshape(2, 3, NFC, 128).transpose(0, 3, 1, 2))
    convb = ca(inp["conv_b"].reshape(2, NFC, 128).transpose(0, 2, 1))
    sguwT = ca(np.stack([inp["sgu_w"][:, 4 * hh:4 * hh + 4].transpose(0, 3, 1, 2) for hh in range(2)], axis=1))
    sgub = ca(np.stack([np.broadcast_to(np.tile(inp["sgu_b"][:, 4 * hh:4 * hh + 4, :], (1, 1, 4))[:, None, :, :],
                                        (2, 128, 4, 512)) for hh in range(2)], axis=1))
    poolw = ca(np.stack([inp["pool_w"][:, 2 * hh:2 * hh + 2].reshape(2, 2, 2, 128, 256).transpose(0, 3, 1, 2, 4)
                         for hh in range(2)], axis=1))
    poolsc = ca(np.stack([inp["pool_scale"][:, hh * 512:(hh + 1) * 512].reshape(2, 4, 128).transpose(0, 2, 1)
                          for hh in range(2)], axis=1))
    shared = {"w_ada": ca(inp["w_ada"][:NL]), "b_adaT": ca(inp["b_ada"][:NL].reshape(NL, 192, 128).transpose(0, 2, 1)),
              "g1T": g1T, "g2T": g2T, "gfin": gfin, "gsgu": gsgu, "mbT": mbT, "convw": convw, "convb": convb,
              "sguwT": sguwT, "sgub": sgub, "poolw": poolw, "poolsc": poolsc,
              "w_in": ca(inp["w_in"][:NL]), "merge_w": ca(inp["merge_w"].reshape(2, D, 4 * D)[:NL]),
              "branch_w": ca(inp["branch_w"].reshape(2, D, D)[:NL]), "out_w": ca(inp["out_w"][:NL]),
              "ffn_wg": ca(inp["ffn_wg"][:NL]), "ffn_wu": ca(inp["ffn_wu"][:NL]), "ffn_wd": ca(inp["ffn_wd"][:NL])}
    for k, v in _const_tables_all().items():
        shared["t_" + k] = v
    maps = []
    for b in range(NCORES):
        m = dict(shared)
        m["x"] = ca(x[b])
        m["cT"] = ca(inp["c"][b].reshape(32, 128).T)
        maps.append(m)
    return maps


_NC_CACHE = {}


def kernel(**inputs):
    inp = {k: np.asarray(v) for k, v in inputs.items()}
    if "nc" not in _NC_CACHE:
        _NC_CACHE["nc"] = build_fused()
    maps = _prep_inputs(inp)
    res = run_bass_kernel_spmd(_NC_CACHE["nc"], maps, core_ids=list(range(NCORES)))
    out = np.empty((NB, S_LEN, D), np.float32)
    for b in range(NCORES):
        out[b] = np.asarray(res.results[b]["out"], dtype=np.float32)
    return out
```
